# Optimizing a Trainium2 kernel written in Bass

```python
import math
import jax, jax.numpy as jnp
from jax import lax
import numpy as np

D_MODEL = 1024
BATCH = 4
SEQ = 4096
DEPTH = 2

D_CONV = D_MODEL // 2
CONV_WIDTH = 31
D_SSM = D_MODEL // 2
SSM_GROUP = 16
N_SSM_GROUPS = D_SSM // SSM_GROUP
SSM_STATE = 64
N_DIR = 2
DT_MIN = 1e-3
DT_MAX = 1e-1
D_IN = 2 * D_CONV + D_SSM + 2 * D_MODEL
D_FF = ((8 * D_MODEL // 3 + 127) // 128) * 128
N_EXPERTS = 8
TOP_K = 2
N_DENSE = (DEPTH + 1) // 2
N_MOE = DEPTH // 2
RMS_EPS = 1e-6
LN_EPS = 1e-5

kernel_name = "hybrid_conv_s5_moe_encoder"


def rms_norm(x, g):
    xf = x.astype(jnp.float32)
    y = xf * lax.rsqrt(jnp.mean(xf * xf, axis=-1, keepdims=True) + RMS_EPS)
    return (y * g.astype(jnp.float32)).astype(x.dtype)


def layer_norm(x, g, b):
    xf = x.astype(jnp.float32)
    mu = jnp.mean(xf, axis=-1, keepdims=True)
    var = jnp.mean(jnp.square(xf - mu), axis=-1, keepdims=True)
    y = (xf - mu) * lax.rsqrt(var + LN_EPS)
    return (y * g.astype(jnp.float32) + b.astype(jnp.float32)).astype(x.dtype)


def conformer_conv(v, gate, dw_w, dw_b, ln_g, ln_b, w_proj):
    h = v * jax.nn.sigmoid(gate)
    half = CONV_WIDTH // 2
    h = lax.conv_general_dilated(
        h, dw_w[:, None, :], window_strides=(1,), padding=[(half, half)],
        dimension_numbers=('NWC', 'WIO', 'NWC'),
        feature_group_count=D_CONV) + dw_b
    h = jax.nn.silu(layer_norm(h, ln_g, ln_b))
    return h @ w_proj


def zoh_discretise(a_re, a_im, log_dt, b_re, b_im):
    dt = jnp.exp(log_dt)[:, None]
    mag = jnp.exp(a_re * dt)
    ab_re = mag * jnp.cos(a_im * dt)
    ab_im = mag * jnp.sin(a_im * dt)
    n_re = ab_re - 1.0
    n_im = ab_im
    den = a_re * a_re + a_im * a_im
    q_re = ((n_re * a_re + n_im * a_im) / den)[..., None]
    q_im = ((n_im * a_re - n_re * a_im) / den)[..., None]
    bb_re = q_re * b_re - q_im * b_im
    bb_im = q_re * b_im + q_im * b_re
    return ab_re, ab_im, bb_re, bb_im


def linear_recurrence_combine(left, right):
    a1r, a1i, b1r, b1i = left
    a2r, a2i, b2r, b2i = right
    ar = a2r * a1r - a2i * a1i
    ai = a2r * a1i + a2i * a1r
    br = a2r * b1r - a2i * b1i + b2r
    bi = a2r * b1i + a2i * b1r + b2i
    return ar, ai, br, bi


def s5_bidirectional(u, a_re, a_im, log_dt, b_re, b_im, c_re, c_im, d_skip):
    bsz, seq_len, _ = u.shape
    uf = u.astype(jnp.float32)
    ug = uf.reshape(bsz, seq_len, N_SSM_GROUPS, SSM_GROUP)
    y = d_skip.astype(jnp.float32) * uf
    b_re32 = b_re.astype(jnp.float32)
    b_im32 = b_im.astype(jnp.float32)
    for direction in range(N_DIR):
        ab_re, ab_im, bb_re, bb_im = zoh_discretise(
            a_re[direction].astype(jnp.float32), a_im[direction].astype(jnp.float32),
            log_dt[direction].astype(jnp.float32), b_re32, b_im32)
        bu_re = jnp.einsum('blgc,gpc->blgp', ug, bb_re)
        bu_im = jnp.einsum('blgc,gpc->blgp', ug, bb_im)
        a_seq_re = jnp.broadcast_to(ab_re, (1, seq_len, N_SSM_GROUPS, SSM_STATE))
        a_seq_im = jnp.broadcast_to(ab_im, (1, seq_len, N_SSM_GROUPS, SSM_STATE))
        _, _, s_re, s_im = lax.associative_scan(
            linear_recurrence_combine, (a_seq_re, a_seq_im, bu_re, bu_im),
            reverse=(direction == 1), axis=1)
        cr = c_re[direction].astype(jnp.float32)
        ci = c_im[direction].astype(jnp.float32)
        y_dir = (jnp.einsum('blgp,gcp->blgc', s_re, cr)
                 - jnp.einsum('blgp,gcp->blgc', s_im, ci))
        y = y + y_dir.reshape(bsz, seq_len, D_SSM)
    return y.astype(u.dtype)


def swiglu(h, w_gate, w_up, w_down):
    return (jax.nn.silu(h @ w_gate) * (h @ w_up)) @ w_down


def top2_moe(h, router_w, router_b, w_gate, w_up, w_down):
    bsz, seq_len, d = h.shape
    t = h.reshape(bsz * seq_len, d)
    logits = (t @ router_w + router_b).astype(jnp.float32)
    top_val, top_idx = lax.top_k(logits, TOP_K)
    top_w = jax.nn.softmax(top_val, axis=-1)
    gates = jnp.sum(jax.nn.one_hot(top_idx, N_EXPERTS, dtype=jnp.float32)
                    * top_w[..., None], axis=1)
    out = jnp.zeros((bsz * seq_len, d), jnp.float32)
    for e in range(N_EXPERTS):
        out = out + gates[:, e:e + 1] * swiglu(t, w_gate[e], w_up[e], w_down[e]).astype(jnp.float32)
    return out.astype(h.dtype).reshape(bsz, seq_len, d)


def setup_inputs(seed: int = 0) -> dict:
    key = jax.random.key(seed)
    ks = iter(jax.random.split(key, 40))
    f32 = jnp.float32

    def nrm(shape, scale):
        return jax.random.normal(next(ks), shape, f32) * scale

    x = jax.random.normal(next(ks), (BATCH, SEQ, D_MODEL), f32)
    norm_mix_g = 1.0 + nrm((DEPTH, D_MODEL), 0.02)
    w_in = nrm((DEPTH, D_MODEL, D_IN), D_MODEL ** -0.5)
    conv_dw_w = nrm((DEPTH, CONV_WIDTH, D_CONV), CONV_WIDTH ** -0.5)
    conv_dw_b = nrm((DEPTH, D_CONV), 0.02)
    conv_ln_g = 1.0 + nrm((DEPTH, D_CONV), 0.02)
    conv_ln_b = nrm((DEPTH, D_CONV), 0.02)
    w_conv_proj = nrm((DEPTH, D_CONV, D_MODEL), D_CONV ** -0.5)
    n_idx = jnp.arange(SSM_STATE, dtype=f32)
    ssm_a_re = -0.5 + nrm((DEPTH, N_DIR, N_SSM_GROUPS, SSM_STATE), 0.01)
    ssm_a_im = math.pi * n_idx + nrm((DEPTH, N_DIR, N_SSM_GROUPS, SSM_STATE), 0.01)
    ssm_log_dt = jax.random.uniform(next(ks), (DEPTH, N_DIR, N_SSM_GROUPS), f32,
                                    math.log(DT_MIN), math.log(DT_MAX))
    b_scale = (2.0 * SSM_GROUP) ** -0.5
    ssm_b_re = nrm((DEPTH, N_SSM_GROUPS, SSM_STATE, SSM_GROUP), b_scale)
    ssm_b_im = nrm((DEPTH, N_SSM_GROUPS, SSM_STATE, SSM_GROUP), b_scale)
    c_scale = (2.0 * SSM_STATE) ** -0.5
    ssm_c_re = nrm((DEPTH, N_DIR, N_SSM_GROUPS, SSM_GROUP, SSM_STATE), c_scale)
    ssm_c_im = nrm((DEPTH, N_DIR, N_SSM_GROUPS, SSM_GROUP, SSM_STATE), c_scale)
    ssm_d = nrm((DEPTH, D_SSM), 1.0)
    ssm_w_glu = nrm((DEPTH, D_SSM, D_SSM), D_SSM ** -0.5)
    ssm_b_glu = nrm((DEPTH, D_SSM), 0.02)
    w_ssm_proj = nrm((DEPTH, D_SSM, D_MODEL), D_SSM ** -0.5)
    w_out = nrm((DEPTH, D_MODEL, D_MODEL), D_MODEL ** -0.5)
    norm_ffn_g = 1.0 + nrm((DEPTH, D_MODEL), 0.02)
    ffn_w_gate = nrm((N_DENSE, D_MODEL, D_FF), D_MODEL ** -0.5)
    ffn_w_up = nrm((N_DENSE, D_MODEL, D_FF), D_MODEL ** -0.5)
    ffn_w_down = nrm((N_DENSE, D_FF, D_MODEL), D_FF ** -0.5)
    router_w = nrm((N_MOE, D_MODEL, N_EXPERTS), D_MODEL ** -0.5)
    router_b = nrm((N_MOE, N_EXPERTS), 0.01)
    moe_w_gate = nrm((N_MOE, N_EXPERTS, D_MODEL, D_FF), D_MODEL ** -0.5)
    moe_w_up = nrm((N_MOE, N_EXPERTS, D_MODEL, D_FF), D_MODEL ** -0.5)
    moe_w_down = nrm((N_MOE, N_EXPERTS, D_FF, D_MODEL), D_FF ** -0.5)
    final_norm_g = 1.0 + nrm((D_MODEL,), 0.02)
    return {
        "x": x, "norm_mix_g": norm_mix_g, "w_in": w_in,
        "conv_dw_w": conv_dw_w, "conv_dw_b": conv_dw_b,
        "conv_ln_g": conv_ln_g, "conv_ln_b": conv_ln_b, "w_conv_proj": w_conv_proj,
        "ssm_a_re": ssm_a_re, "ssm_a_im": ssm_a_im, "ssm_log_dt": ssm_log_dt,
        "ssm_b_re": ssm_b_re, "ssm_b_im": ssm_b_im,
        "ssm_c_re": ssm_c_re, "ssm_c_im": ssm_c_im, "ssm_d": ssm_d,
        "ssm_w_glu": ssm_w_glu, "ssm_b_glu": ssm_b_glu, "w_ssm_proj": w_ssm_proj,
        "w_out": w_out, "norm_ffn_g": norm_ffn_g,
        "ffn_w_gate": ffn_w_gate, "ffn_w_up": ffn_w_up, "ffn_w_down": ffn_w_down,
        "router_w": router_w, "router_b": router_b,
        "moe_w_gate": moe_w_gate, "moe_w_up": moe_w_up, "moe_w_down": moe_w_down,
        "final_norm_g": final_norm_g,
    }


def reference(x, norm_mix_g, w_in, conv_dw_w, conv_dw_b, conv_ln_g, conv_ln_b, w_conv_proj,
              ssm_a_re, ssm_a_im, ssm_log_dt, ssm_b_re, ssm_b_im, ssm_c_re, ssm_c_im, ssm_d,
              ssm_w_glu, ssm_b_glu, w_ssm_proj, w_out, norm_ffn_g,
              ffn_w_gate, ffn_w_up, ffn_w_down, router_w, router_b,
              moe_w_gate, moe_w_up, moe_w_down, final_norm_g):
    split_at = [D_CONV, 2 * D_CONV, 2 * D_CONV + D_SSM, 2 * D_CONV + D_SSM + D_MODEL]
    for layer in range(DEPTH):
        h = rms_norm(x, norm_mix_g[layer])
        z = h @ w_in[layer]
        conv_v, conv_g, ssm_u, gate_conv, gate_ssm = jnp.split(z, split_at, axis=-1)
        br_conv = conformer_conv(conv_v, conv_g, conv_dw_w[layer], conv_dw_b[layer],
                                 conv_ln_g[layer], conv_ln_b[layer], w_conv_proj[layer])
        y = s5_bidirectional(ssm_u, ssm_a_re[layer], ssm_a_im[layer], ssm_log_dt[layer],
                             ssm_b_re[layer], ssm_b_im[layer], ssm_c_re[layer],
                             ssm_c_im[layer], ssm_d[layer])
        y = jax.nn.gelu(y)
        y = y * jax.nn.sigmoid(y @ ssm_w_glu[layer] + ssm_b_glu[layer])
        br_ssm = y @ w_ssm_proj[layer]
        merged = jax.nn.sigmoid(gate_conv) * br_conv + jax.nn.sigmoid(gate_ssm) * br_ssm
        x = x + merged @ w_out[layer]
        h = rms_norm(x, norm_ffn_g[layer])
        if layer % 2 == 0:
            i = layer // 2
            x = x + swiglu(h, ffn_w_gate[i], ffn_w_up[i], ffn_w_down[i])
        else:
            i = layer // 2
            x = x + top2_moe(h, router_w[i], router_b[i], moe_w_gate[i],
                             moe_w_up[i], moe_w_down[i])
    return rms_norm(x, final_norm_g)
```

```python
import contextlib
import math
import numpy as np
import concourse.bass as bass
import concourse.mybir as mybir
from concourse.bass_utils import run_bass_kernel_spmd

F32 = mybir.dt.float32
BF16 = mybir.dt.bfloat16
AF = mybir.ActivationFunctionType
ALU = mybir.AluOpType
AX = mybir.AxisListType

ENGS = ["tensor", "vector", "scalar", "gpsimd", "sync"]


def _flat(keys):
    out = []
    for k in keys:
        if isinstance(k, (list, tuple)) and not (len(k) == 2 and k[0] == "dma"):
            out.extend(_flat(k))
        else:
            out.append(k)
    return out


class Sched:
    def __init__(self, nc):
        self.nc = nc
        self.q = {e: [] for e in ENGS}
        self.cnt = {e: 0 for e in ENGS}
        self.last_w = {}
        self.readers = {}
        self.waited = {e: {} for e in ENGS}
        self.dma_cnt = {}
        self.semkeys = list(ENGS)
        self.out_tokens = []

    def _deps(self, reads, writes):
        deps = {}
        reads = _flat(reads)
        writes = _flat(writes)

        def add(tok):
            if tok is None:
                return
            k, v = tok
            if deps.get(k, 0) < v:
                deps[k] = v

        for b in reads:
            add(self.last_w.get(b))
        for b in writes:
            add(self.last_w.get(b))
            for t in self.readers.get(b, ()):
                add(t)
        return deps

    def _emit_waits(self, eng, deps):
        w = self.waited[eng]
        todo = []
        for k, v in deps.items():
            if w.get(k, 0) >= v:
                continue
            w[k] = v
            todo.append((k, v))
        return todo

    def _commit(self, tok, reads, writes):
        reads = _flat(reads)
        writes = _flat(writes)
        for b in reads:
            self.readers.setdefault(b, []).append(tok)
        for b in writes:
            self.last_w[b] = tok
            self.readers[b] = []

    def op(self, eng, fn, reads=(), writes=()):
        deps = self._deps(reads, writes)
        todo = self._emit_waits(eng, deps)
        self.cnt[eng] += 1
        n = self.cnt[eng]
        tok = (eng, n)
        self.waited[eng][eng] = max(self.waited[eng].get(eng, 0), 0)
        self.q[eng].append(("op", todo, fn, eng))
        self._commit(tok, reads, writes)
        return tok

    def dma(self, eng, fn, reads=(), writes=(), key=None, is_output=False):
        deps = self._deps(reads, writes)
        todo = self._emit_waits(eng, deps)
        if key is None:
            key = ("dma", _flat(writes)[0])
        else:
            key = ("dma", key)
        if key not in self.dma_cnt:
            self.dma_cnt[key] = 0
            self.semkeys.append(key)
        self.dma_cnt[key] += 16
        tok = (key, self.dma_cnt[key])
        self.q[eng].append(("dma", todo, fn, key))
        self._commit(tok, reads, writes)
        if is_output:
            self.out_tokens.append(tok)
        return tok

    def barrier(self):
        deps = {e: self.cnt[e] for e in ENGS if self.cnt[e] > 0}
        for k, v in self.dma_cnt.items():
            deps[k] = v
        for e in ENGS:
            todo = self._emit_waits(e, dict(deps))
            self.q[e].append(("wait", todo, None, None))

    def finish(self):
        deps = {}
        for k, v in self.out_tokens:
            deps[k] = max(deps.get(k, 0), v)
        todo = self._emit_waits("sync", deps)
        self.q["sync"].append(("wait", todo, None, None))

    def flush(self, stack):
        nc = self.nc
        if not hasattr(self, "sems"):
            self.sems = {}
        sems = self.sems
        for k in self.semkeys:
            if k not in sems:
                sems[k] = stack.enter_context(nc.semaphore("s%d" % len(sems)))
        q = self.q
        self.q = {e: [] for e in ENGS}
        with nc.Block() as block:
            def run(engname, e):
                for kind, todo, fn, key in q[engname]:
                    for k, v in todo:
                        e.wait_ge(sems[k], v)
                    if kind == "op":
                        fn(e).then_inc(sems[key], 1)
                    elif kind == "dma":
                        fn(e).then_inc(sems[key], 16)

            @block.tensor
            def _(e):
                run("tensor", e)

            @block.vector
            def _(e):
                run("vector", e)

            @block.scalar
            def _(e):
                run("scalar", e)

            @block.gpsimd
            def _(e):
                run("gpsimd", e)

            @block.sync
            def _(e):
                run("sync", e)

    def emit(self):
        self._st = contextlib.ExitStack()
        self.flush(self._st)
        self._st.close()
import contextlib

NT = 2048
D = 1024
KB = D // 128
TT = 512


def build_A(d_in=3584, eps=1e-6):
    nc = bass.Bass("TRN2", target_bir_lowering=False)
    nfo = d_in // 128
    xT = nc.dram_tensor("xT", [128, KB, NT], F32, kind="ExternalInput").ap()
    gcol = nc.dram_tensor("gcol", [128, KB], F32, kind="ExternalInput").ap()
    w = nc.dram_tensor("w", [D, d_in], F32, kind="ExternalInput").ap()
    zT = nc.dram_tensor("zT", [nfo, 128, NT], F32, kind="ExternalOutput").ap()
    wv = w.rearrange("(kb p) n -> p kb n", p=128)
    S = Sched(nc)
    with contextlib.ExitStack() as st:
        sb = lambda name, shape, dt: st.enter_context(nc.sbuf_tensor(name, shape, dt))
        ps = lambda name, shape, dt: st.enter_context(nc.psum_tensor(name, shape, dt))
        wsb = sb("wsb", [128, KB, d_in], BF16)
        g_sb = sb("g_sb", [128, KB], F32)
        ones = sb("ones", [128, 128], F32)
        xt = [sb("xt%d" % i, [128, KB, TT], F32) for i in range(2)]
        sq = sb("sq", [128, KB, TT], F32)
        rstd = sb("rstd", [128, TT], F32)
        hT = [sb("hT%d" % i, [128, KB, TT], BF16) for i in range(2)]
        zo = [sb("zo%d" % i, [128, TT], F32) for i in range(4)]
        pss = ps("pss", [128, TT], F32)
        pz = [ps("pz%d" % i, [128, TT], F32) for i in range(4)]

        S.op("vector", lambda e: e.memset(ones[:], 1.0), writes=["ones"])
        S.dma("sync", lambda e: e.dma_start(out=g_sb[:], in_=gcol), writes=["g_sb"])
        WCH = 512
        nwch = d_in // WCH
        for c in range(nwch):
            S.dma("gpsimd", lambda e, c=c: e.dma_start(out=wsb[:, :, c * WCH:(c + 1) * WCH],
                                                       in_=wv[:, :, c * WCH:(c + 1) * WCH]),
                  writes=["w%d" % c])
        nt = NT // TT
        oi = 0
        for t in range(nt):
            xb = xt[t % 2]
            hb = hT[t % 2]
            S.dma("sync", lambda e, xb=xb, t=t: e.dma_start(out=xb[:], in_=xT[:, :, t * TT:(t + 1) * TT]),
                  writes=["xt%d" % (t % 2)])
            S.op("scalar", lambda e, xb=xb: e.activation(out=sq[:], in_=xb[:], func=AF.Square),
                 reads=["xt%d" % (t % 2)], writes=["sq"])

            def mm_ss(e):
                for kb in range(KB):
                    r = e.matmul(pss[:], lhsT=ones[:], rhs=sq[:, kb, :], start=(kb == 0), stop=(kb == KB - 1))
                return r
            S.op("tensor", mm_ss, reads=["ones", "sq"], writes=["pss"])
            S.op("vector", lambda e: e.tensor_scalar(out=rstd[:], in0=pss[:], scalar1=1.0 / D, scalar2=eps,
                                                     op0=ALU.mult, op1=ALU.add),
                 reads=["pss"], writes=["rstd"])
            S.op("scalar", lambda e: e.sqrt(out=rstd[:], in_=rstd[:]), reads=["rstd"], writes=["rstd"])
            S.op("vector", lambda e: e.reciprocal(out=rstd[:], in_=rstd[:]), reads=["rstd"], writes=["rstd"])
            for kb in range(KB):
                S.op("vector", lambda e, kb=kb, xb=xb, hb=hb: e.scalar_tensor_tensor(
                    out=hb[:, kb, :], in0=xb[:, kb, :], scalar=g_sb[:, kb:kb + 1], in1=rstd[:],
                    op0=ALU.mult, op1=ALU.mult),
                    reads=["xt%d" % (t % 2), "rstd", "g_sb"], writes=["hT%d_%d" % (t % 2, kb)])
            for fo in range(nfo):
                pb = pz[oi % 4]
                ob = zo[oi % 4]
                pk = "pz%d" % (oi % 4)
                ok = "zo%d" % (oi % 4)

                def mm(e, fo=fo, pb=pb, hb=hb):
                    for kb in range(KB):
                        r = e.matmul(pb[:], lhsT=wsb[:, kb, fo * 128:(fo + 1) * 128], rhs=hb[:, kb, :],
                                     start=(kb == 0), stop=(kb == KB - 1))
                    return r
                S.op("tensor", mm, reads=["w%d" % (fo * 128 // WCH)] + ["hT%d_%d" % (t % 2, kb) for kb in range(KB)],
                     writes=[pk])
                eng = "scalar" if oi % 2 == 0 else "vector"
                if eng == "scalar":
                    S.op("scalar", lambda e, pb=pb, ob=ob: e.copy(out=ob[:], in_=pb[:]), reads=[pk], writes=[ok])
                else:
                    S.op("vector", lambda e, pb=pb, ob=ob: e.tensor_copy(out=ob[:], in_=pb[:]), reads=[pk], writes=[ok])
                S.dma("sync", lambda e, fo=fo, t=t, ob=ob: e.dma_start(out=zT[fo, :, t * TT:(t + 1) * TT], in_=ob[:]),
                      reads=[ok], writes=["zT"], is_output=True)
                oi += 1
        S.finish()
        S.emit()
    return nc


from concourse.ap import AP

L = 4096
NK = L // 8
G = 16
TWO_PI = 2.0 * math.pi


def rev_ap(ap, dim):
    pat = [list(x) for x in ap.ap]
    step, cnt = pat[dim]
    off = ap.offset + step * (cnt - 1)
    pat[dim] = [-step, cnt]
    return AP(ap.tensor, off, pat)


def build_B():
    nc = bass.Bass("TRN2", target_bir_lowering=False)
    din = lambda name, shape: nc.dram_tensor(name, shape, F32, kind="ExternalInput").ap()
    U_d = din("U", [128, G, NK])
    vT_d = din("vT", [128, 2, L])
    gT_d = din("gT", [128, 2, L])
    dww_d = din("dww", [128, 2, 31])
    dwb_d = din("dwb", [128, 2])
    are_d = din("a_re", [128, G])
    aim_d = din("a_im", [128, G])
    ldt_d = din("log_dt", [128, G])
    bre_d = din("b_re", [128, G, 16])
    bim_d = din("b_im", [128, G, 16])
    cre_d = din("c_re", [128, G, 16])
    cim_d = din("c_im", [128, G, 16])
    dbc_d = din("dbc", [128, G, 128])
    mf_d = din("mask_f", [128, 128])
    mb_d = din("mask_b", [128, 128])
    convT_d = nc.dram_tensor("convT", [128, 2, L], F32, kind="ExternalOutput").ap()
    Y_d = nc.dram_tensor("Y", [4, 128, G, 128], F32, kind="ExternalOutput").ap()
    S = Sched(nc)
    V = "vector"
    with contextlib.ExitStack() as st:
        def mk(stack):
            return (lambda name, shape, dt=F32: stack.enter_context(nc.sbuf_tensor("sb_" + name, shape, dt)),
                    lambda name, shape, dt=F32: stack.enter_context(nc.psum_tensor("ps_" + name, shape, dt)))
        sb, ps = mk(st)
        ident = sb("ident", [128, 128])
        identb = sb("identb", [128, 128], BF16)
        dww = sb("dww", [128, 2, 31]); dwb = sb("dwb", [128, 2])
        WstT = sb("WstT", [128, G, 2, 128], BF16)
        Kloc = sb("Kloc", [128, G, 128], BF16)
        WoR = sb("WoR", [128, G, 128], BF16); WoI = sb("WoI", [128, G, 128], BF16)
        AR2 = sb("AR2", [128, 2, G]); AI2 = sb("AI2", [128, 2, G])
        S.dma("sync", lambda e: e.dma_start(out=dww[:], in_=dww_d), writes=["dww"])
        S.dma("sync", lambda e: e.dma_start(out=dwb[:], in_=dwb_d), writes=["dwb"])
        S.op("gpsimd", lambda e: e.memset(ident[:], 1.0), writes=["ident"])
        S.op("gpsimd", lambda e: e.affine_select(out=ident[:], in_=ident[:], pattern=[[-1, 128]], compare_op=ALU.is_equal,
                                                 fill=0.0, base=0, channel_multiplier=1), reads=["ident"], writes=["ident"])
        S.op(V, lambda e: e.tensor_copy(out=identb[:], in_=ident[:]), reads=["ident"], writes=["identb"])

        with contextlib.ExitStack() as s1:
            sb1, ps1 = mk(s1)

            def ld(name, d, shape):
                t = sb1(name, shape)
                S.dma("sync", lambda e: e.dma_start(out=t[:], in_=d), writes=[name])
                return t
            are = ld("are", are_d, [128, G]); aim = ld("aim", aim_d, [128, G]); ldt = ld("ldt", ldt_d, [128, G])
            bre = ld("bre", bre_d, [128, G, 16]); bim = ld("bim", bim_d, [128, G, 16])
            cre = ld("cre", cre_d, [128, G, 16]); cim = ld("cim", cim_d, [128, G, 16])
            dbc = ld("dbc", dbc_d, [128, G, 128]); mf = ld("mf", mf_d, [128, 128]); mb = ld("mb", mb_d, [128, 128])
            cnt = [0]
            pools = {}

            def tmp(shape=(128, G), dt=F32, persist=True):
                shape = tuple(shape)
                if persist:
                    cnt[0] += 1
                    nm = "t%d" % cnt[0]
                    return sb1(nm, list(shape), dt), nm
                pl = pools.setdefault(shape, {"i": 0, "bufs": []})
                npool = 8
                if len(pl["bufs"]) < npool:
                    cnt[0] += 1
                    nm = "tp%d" % cnt[0]
                    pl["bufs"].append((sb1(nm, list(shape), dt), nm))
                r = pl["bufs"][pl["i"] % npool]
                pl["i"] += 1
                return r

            def tt(o, ok, a, ak, b, bk, op):
                S.op(V, lambda e: e.tensor_tensor(out=o, in0=a, in1=b, op=op), reads=[ak, bk], writes=[ok])

            def ts(o, ok, a, ak, s1_, s2_, op0, op1=None):
                if op1 is None:
                    S.op(V, lambda e: e.tensor_single_scalar(out=o, in_=a, scalar=s1_, op=op0), reads=[ak], writes=[ok])
                else:
                    S.op(V, lambda e: e.tensor_scalar(out=o, in0=a, scalar1=s1_, scalar2=s2_, op0=op0, op1=op1), reads=[ak], writes=[ok])

            def act(o, ok, a, ak, f):
                S.op("scalar", lambda e: e.activation(out=o, in_=a, func=f), reads=[ak], writes=[ok])

            def new_tt(a, ak, b, bk, op, shape=(128, G), persist=True):
                o, ok = tmp(shape, persist=persist)
                tt(o[:], ok, a, ak, b, bk, op)
                return o, ok

            def cmul_into(ore, orek, oim, oimk, ar_, ark, ai_, aik, br_, brk, bi_, bik, shape, sign=1.0):
                t1, k1 = new_tt(ar_, ark, br_, brk, ALU.mult, shape, False)
                t2, k2 = new_tt(ai_, aik, bi_, bik, ALU.mult, shape, False)
                tt(ore, orek, t1[:], k1, t2[:], k2, ALU.subtract)
                t3, k3 = new_tt(ar_, ark, bi_, bik, ALU.mult, shape, False)
                t4, k4 = new_tt(ai_, aik, br_, brk, ALU.mult, shape, False)
                tt(oim, oimk, t3[:], k3, t4[:], k4, ALU.add)

            def cmul(ar_, ark, ai_, aik, br_, brk, bi_, bik, shape):
                re, rk = tmp(shape); im, ik = tmp(shape)
                cmul_into(re[:], rk, im[:], ik, ar_, ark, ai_, aik, br_, brk, bi_, bik, shape)
                return re, rk, im, ik

            dt_, dtk = tmp(); act(dt_[:], dtk, ldt[:], "ldt", AF.Exp)
            adr, adrk = new_tt(are[:], "are", dt_[:], dtk, ALU.mult)
            adi, adik = new_tt(aim[:], "aim", dt_[:], dtk, ALU.mult)
            mag, magk = tmp(); act(mag[:], magk, adr[:], adrk, AF.Exp)

            def reduced(shift):
                r, rk = tmp(); ts(r[:], rk, adi[:], adik, 1.0 / TWO_PI, shift, ALU.mult, ALU.add)
                ni, nik = tmp((128, G), mybir.dt.int32)
                S.op(V, lambda e: e.tensor_copy(out=ni[:], in_=r[:]), reads=[rk], writes=[nik])
                nf, nfk = tmp()
                S.op(V, lambda e: e.tensor_copy(out=nf[:], in_=ni[:]), reads=[nik], writes=[nfk])
                fr, frk = new_tt(r[:], rk, nf[:], nfk, ALU.subtract)
                ng, ngk = tmp(); ts(ng[:], ngk, fr[:], frk, 0.0, None, ALU.is_lt)
                fr2, fr2k = new_tt(fr[:], frk, ng[:], ngk, ALU.add)
                th, thk = tmp(); ts(th[:], thk, fr2[:], fr2k, TWO_PI, -math.pi, ALU.mult, ALU.add)
                th2, th2k = tmp(); ts(th2[:], th2k, th[:], thk, 3.1415925, -3.1415925, ALU.min, ALU.max)
                o, ok = tmp(); act(o[:], ok, th2[:], th2k, AF.Sin)
                return o, ok
            sn, snk = reduced(0.5)
            cs, csk = reduced(0.75)
            abr, abrk = new_tt(mag[:], magk, cs[:], csk, ALU.mult)
            abi, abik = new_tt(mag[:], magk, sn[:], snk, ALU.mult)
            nr, nrk = tmp(); ts(nr[:], nrk, abr[:], abrk, -1.0, None, ALU.add)
            d1, d1k = new_tt(are[:], "are", are[:], "are", ALU.mult)
            d2, d2k = new_tt(aim[:], "aim", aim[:], "aim", ALU.mult)
            den, denk = new_tt(d1[:], d1k, d2[:], d2k, ALU.add)
            rden, rdenk = tmp(); S.op(V, lambda e: e.reciprocal(out=rden[:], in_=den[:]), reads=[denk], writes=[rdenk])
            u1, u1k = new_tt(nr[:], nrk, are[:], "are", ALU.mult)
            u2, u2k = new_tt(abi[:], abik, aim[:], "aim", ALU.mult)
            u3, u3k = new_tt(u1[:], u1k, u2[:], u2k, ALU.add)
            qre, qrek = new_tt(u3[:], u3k, rden[:], rdenk, ALU.mult)
            u4, u4k = new_tt(abi[:], abik, are[:], "are", ALU.mult)
            u5, u5k = new_tt(nr[:], nrk, aim[:], "aim", ALU.mult)
            u6, u6k = new_tt(u4[:], u4k, u5[:], u5k, ALU.subtract)
            qim, qimk = new_tt(u6[:], u6k, rden[:], rdenk, ALU.mult)
            m1, m1k = new_tt(abr[:], abrk, abr[:], abrk, ALU.mult)
            m2, m2k = new_tt(abi[:], abik, abi[:], abik, ALU.mult)
            m3, m3k = new_tt(m1[:], m1k, m2[:], m2k, ALU.add)
            rm, rmk = tmp(); S.op(V, lambda e: e.reciprocal(out=rm[:], in_=m3[:]), reads=[m3k], writes=[rmk])
            ibr, ibrk = new_tt(abr[:], abrk, rm[:], rmk, ALU.mult)
            ibi0, ibi0k = new_tt(abi[:], abik, rm[:], rmk, ALU.mult)
            ibi, ibik = tmp(); ts(ibi[:], ibik, ibi0[:], ibi0k, -1.0, None, ALU.mult)
            one, onek = tmp(); S.op(V, lambda e: e.memset(one[:], 1.0), writes=[onek])
            zero, zerok = tmp(); S.op(V, lambda e: e.memset(zero[:], 0.0), writes=[zerok])
            P = [(one, onek, zero, zerok), (abr, abrk, abi, abik)]
            for k in range(2, 9):
                pr, prk, pi, pik = P[-1]
                P.append(cmul(pr[:], prk, pi[:], pik, abr[:], abrk, abi[:], abik, (128, G)))
            N = [(one, onek, zero, zerok), (ibr, ibrk, ibi, ibik)]
            for k in range(2, 8):
                pr, prk, pi, pik = N[-1]
                N.append(cmul(pr[:], prk, pi[:], pik, ibr[:], ibrk, ibi[:], ibik, (128, G)))

            def bc16(t):
                return t[:].unsqueeze(2).to_broadcast([128, G, 16])
            sh3 = (128, G, 16)
            Bbr, Bbrk, Bbi, Bbik = cmul(bc16(qre), qrek, bc16(qim), qimk, bre[:], "bre", bim[:], "bim", sh3)
            Yr = sb1("Yr", [128, G, 8, 16]); Yi = sb1("Yi", [128, G, 8, 16])
            Xr = sb1("Xr", [128, G, 8, 16]); nXi = sb1("nXi", [128, G, 8, 16])
            YK = []; XK = []

            def put(dst, nm, s, src, srck, neg=False):
                for lo, pos in ((0, s), (64, 7 - s)):
                    key = "%s_%d_%d" % (nm, lo, pos)
                    if neg:
                        S.op(V, lambda e, lo=lo, pos=pos: e.tensor_single_scalar(out=dst[lo:lo + 64, :, pos, :], in_=src[lo:lo + 64],
                                                                                  scalar=-1.0, op=ALU.mult), reads=[srck], writes=[key])
                    else:
                        S.op(V, lambda e, lo=lo, pos=pos: e.tensor_copy(out=dst[lo:lo + 64, :, pos, :], in_=src[lo:lo + 64]),
                             reads=[srck], writes=[key])
                    (YK if nm[0] == "Y" else XK).append(key)
            for s in range(8):
                nr_, nrk_, ni_, nik_ = N[s]
                r, rk = tmp(sh3, persist=False); i, ik = tmp(sh3, persist=False)
                cmul_into(r[:], rk, i[:], ik, bc16(nr_), nrk_, bc16(ni_), nik_, Bbr[:], Bbrk, Bbi[:], Bbik, sh3)
                put(Yr, "Yr", s, r, rk); put(Yi, "Yi", s, i, ik)
                pr_, prk_, pi_, pik_ = P[s]
                r, rk = tmp(sh3, persist=False); i, ik = tmp(sh3, persist=False)
                cmul_into(r[:], rk, i[:], ik, bc16(pr_), prk_, bc16(pi_), pik_, cre[:], "cre", cim[:], "cim", sh3)
                put(Xr, "Xr", s, r, rk); put(nXi, "nXi", s, i, ik, neg=True)
            sh4 = (128, G, 128)

            def bc128(t):
                return t[:].unsqueeze(2).to_broadcast([128, G, 128])
            f3 = lambda t: t[:].rearrange("p g s c -> p g (s c)")
            p7r, p7rk, p7i, p7ik = P[7]
            Wr = sb1("Wr", [128, G, 128]); Wi = sb1("Wi", [128, G, 128])
            tA = sb1("tA", [128, G, 128]); tB = sb1("tB", [128, G, 128])
            tt(tA[:], "tA", bc128(p7r), p7rk, f3(Yr), YK, ALU.mult)
            tt(tB[:], "tB", bc128(p7i), p7ik, f3(Yi), YK, ALU.mult)
            tt(Wr[:], "Wr", tA[:], "tA", tB[:], "tB", ALU.subtract)
            tt(tA[:], "tA", bc128(p7r), p7rk, f3(Yi), YK, ALU.mult)
            tt(tB[:], "tB", bc128(p7i), p7ik, f3(Yr), YK, ALU.mult)
            tt(Wi[:], "Wi", tA[:], "tA", tB[:], "tB", ALU.add)
            p1r, p1rk, p1i, p1ik = P[1]
            tt(tA[:], "tA", bc128(p1r), p1rk, f3(Xr), XK, ALU.mult)
            tt(tB[:], "tB", bc128(p1i), p1ik, f3(nXi), XK, ALU.mult)
            tt(WoR[:], "WoR", tA[:], "tA", tB[:], "tB", ALU.add)
            tt(tA[:], "tA", bc128(p1r), p1rk, f3(nXi), XK, ALU.mult)
            tt(tB[:], "tB", bc128(p1i), p1ik, f3(Xr), XK, ALU.mult)
            tt(WoI[:], "WoI", tA[:], "tA", tB[:], "tB", ALU.subtract)
            ptr = [ps1("ptr%d" % i, [128, 128]) for i in range(2)]
            pkf = ps1("pkf", [128, 128]); pkb = ps1("pkb", [128, 128])
            k1 = sb1("k1", [128, 128]); k2 = sb1("k2", [128, 128]); k3 = sb1("k3", [128, 128])
            ti = 0
            for g in range(G):
                for ri, (src, nm) in enumerate(((Wr, "Wr"), (Wi, "Wi"))):
                    pb = ptr[ti % 2]; pk = "ptr%d" % (ti % 2); ti += 1
                    S.op("tensor", lambda e, pb=pb, src=src, g=g: e.transpose(pb[:], src[:, g, :], ident[:]),
                         reads=[nm, "ident"], writes=[pk])
                    S.op("scalar", lambda e, pb=pb, g=g, ri=ri: e.copy(out=WstT[:, g, ri, :], in_=pb[:]), reads=[pk], writes=["WstT"])

                def mmk(e, g=g):
                    fl = lambda t, lo: t[lo:lo + 64, g].rearrange("p s c -> p (s c)")
                    e.matmul(pkf[:], lhsT=fl(Yr, 0), rhs=fl(Xr, 0), start=True, stop=False)
                    e.matmul(pkf[:], lhsT=fl(Yi, 0), rhs=fl(nXi, 0), start=False, stop=True)
                    e.matmul(pkb[:], lhsT=fl(Yr, 64), rhs=fl(Xr, 64), start=True, stop=False)
                    return e.matmul(pkb[:], lhsT=fl(Yi, 64), rhs=fl(nXi, 64), start=False, stop=True)
                S.op("tensor", mmk, reads=YK + XK, writes=["pkf", "pkb"])
                S.op(V, lambda e: e.tensor_tensor(out=k1[:], in0=pkf[:], in1=mf[:], op=ALU.mult), reads=["pkf", "mf"], writes=["k1"])
                S.op(V, lambda e: e.tensor_tensor(out=k2[:], in0=pkb[:], in1=mb[:], op=ALU.mult), reads=["pkb", "mb"], writes=["k2"])
                S.op(V, lambda e, g=g: e.tensor_tensor(out=k3[:], in0=ident[:], in1=dbc[:, g, :], op=ALU.mult), reads=["ident", "dbc"], writes=["k3"])
                S.op(V, lambda e: e.tensor_tensor(out=k1[:], in0=k1[:], in1=k2[:], op=ALU.add), reads=["k1", "k2"], writes=["k1"])
                S.op(V, lambda e, g=g: e.tensor_tensor(out=Kloc[:, g, :], in0=k1[:], in1=k3[:], op=ALU.add), reads=["k1", "k3"], writes=["Kloc"])
            p8r, p8rk, p8i, p8ik = P[8]
            S.op(V, lambda e: e.tensor_copy(out=AR2[:, 0, :], in_=p8r[:]), reads=[p8rk], writes=["AR2a"])
            S.op(V, lambda e: e.tensor_copy(out=AR2[:, 1, :], in_=p8r[:]), reads=[p8rk], writes=["AR2b"])
            S.op(V, lambda e: e.tensor_single_scalar(out=AI2[:, 0, :], in_=p8i[:], scalar=-1.0, op=ALU.mult), reads=[p8ik], writes=["AI2a"])
            S.op(V, lambda e: e.tensor_copy(out=AI2[:, 1, :], in_=p8i[:]), reads=[p8ik], writes=["AI2b"])
            S.barrier()
            S.flush(st)
        AK = ["AR2a", "AR2b", "AI2a", "AI2b"]

        Ub = sb("Ub", [128, G, NK], BF16)
        St = sb("St", [128, 2, NK, G])
        for g4 in range(4):
            S.dma("gpsimd", lambda e, g4=g4: e.dma_start(out=Ub[:, g4 * 4:(g4 + 1) * 4, :], in_=U_d[:, g4 * 4:(g4 + 1) * 4, :]),
                  writes=["Ub%d" % g4])
        with contextlib.ExitStack() as s2:
            sb2, ps2 = mk(s2)
            hc = sb2("hc", [128, 2, L + 30], BF16)
            S.op("gpsimd", lambda e: e.memset(hc[:, :, 0:15], 0.0), writes=["hcp0"])
            S.op("gpsimd", lambda e: e.memset(hc[:, :, L + 15:L + 30], 0.0), writes=["hcp1"])
            CW = 512
            vt = [sb2("vt%d" % i, [128, CW]) for i in range(2)]
            gt = [sb2("gt%d" % i, [128, CW]) for i in range(2)]
            ci = 0
            HCK = []
            for blk in range(2):
                for q in range(L // CW):
                    vb = vt[ci % 2]; gb = gt[ci % 2]; vk = "vt%d" % (ci % 2); gk = "gt%d" % (ci % 2)
                    sl = slice(q * CW, (q + 1) * CW)
                    S.dma("sync", lambda e, vb=vb, blk=blk, sl=sl: e.dma_start(out=vb[:], in_=vT_d[:, blk, sl]), writes=[vk])
                    S.dma("sync", lambda e, gb=gb, blk=blk, sl=sl: e.dma_start(out=gb[:], in_=gT_d[:, blk, sl]), writes=[gk])
                    S.op("scalar", lambda e, gb=gb: e.activation(out=gb[:], in_=gb[:], func=AF.Sigmoid), reads=[gk], writes=[gk])
                    hk = "hc%d_%d" % (blk, q)
                    S.op(V, lambda e, vb=vb, gb=gb, blk=blk, q=q: e.tensor_tensor(out=hc[:, blk, 15 + q * CW:15 + (q + 1) * CW],
                                                                                 in0=vb[:], in1=gb[:], op=ALU.mult),
                         reads=[vk, gk], writes=[hk])
                    HCK.append(hk)
                    ci += 1
            dg = sb2("dg", [128, 2, 31, 128], BF16)
            for blk in range(2):
                for k in range(31):
                    S.op("gpsimd", lambda e, blk=blk, k=k: e.tensor_scalar(out=dg[:, blk, k, :], in0=ident[:], scalar1=dww[:, blk, k:k + 1],
                                                                           scalar2=None, op0=ALU.mult),
                         reads=["ident", "dww"], writes=["dg%d" % blk])
            pst = [ps2("pst%d" % i, [128, NK]) for i in range(2)]
            si = 0
            STK = []
            for g in range(G):
                for ri in range(2):
                    pb = pst[si % 2]; pk = "pst%d" % (si % 2)
                    S.op("tensor", lambda e, pb=pb, g=g, ri=ri: e.matmul(pb[:], lhsT=WstT[:, g, ri, :], rhs=Ub[:, g, :], start=True, stop=True),
                         reads=["WstT", "Ub%d" % (g // 4)], writes=[pk])
                    sk = "St%d_%d" % (ri, g)
                    if si % 2 == 0:
                        S.op("scalar", lambda e, pb=pb, g=g, ri=ri: e.copy(out=St[:, ri, :, g], in_=pb[:]), reads=[pk], writes=[sk])
                    else:
                        S.op(V, lambda e, pb=pb, g=g, ri=ri: e.tensor_copy(out=St[:, ri, :, g], in_=pb[:]), reads=[pk], writes=[sk])
                    STK.append(sk)
                    si += 1
            sc = {nm: (sb2("sc1" + nm, [128, 2, G]), sb2("sc2" + nm, [128, 2, G])) for nm in ("f", "b")}

            def scan(eng, nm, lo, order):
                t1, t2 = sc[nm]
                hk = "H" + nm
                first = True
                for kprev, kcur in order:
                    rd = (STK if first else []) + [hk] + AK
                    first = False
                    prev = St[lo:lo + 64, :, kprev, :]
                    prev_sw = rev_ap(St[lo:lo + 64, :, kprev, :], 1)
                    cur = St[lo:lo + 64, :, kcur, :]
                    S.op(eng, lambda e, prev=prev: e.tensor_tensor(out=t1[lo:lo + 64], in0=prev, in1=AR2[lo:lo + 64], op=ALU.mult),
                         reads=rd, writes=["sc1" + nm])
                    S.op(eng, lambda e, prev_sw=prev_sw: e.tensor_tensor(out=t2[lo:lo + 64], in0=prev_sw, in1=AI2[lo:lo + 64], op=ALU.mult),
                         reads=rd, writes=["sc2" + nm])
                    S.op(eng, lambda e, cur=cur: e.tensor_tensor(out=cur, in0=cur, in1=t1[lo:lo + 64], op=ALU.add),
                         reads=["sc1" + nm] + rd, writes=[hk])
                    S.op(eng, lambda e, cur=cur: e.tensor_tensor(out=cur, in0=cur, in1=t2[lo:lo + 64], op=ALU.add),
                         reads=["sc2" + nm], writes=[hk])
            scan("vector", "f", 0, [(k - 1, k) for k in range(1, NK)])
            scan("gpsimd", "b", 64, [(k + 1, k) for k in range(NK - 2, -1, -1)])
            pc = [ps2("pc%d" % i, [128, 512]) for i in range(2)]
            co = [sb2("co%d" % i, [128, 512]) for i in range(2)]
            ti = 0
            for blk in range(2):
                for t in range(L // 512):
                    pb = pc[ti % 2]; pk = "pc%d" % (ti % 2); ob = co[ti % 2]; ok = "co%d" % (ti % 2)

                    def mmc(e, pb=pb, blk=blk, t=t):
                        for k in range(31):
                            r = e.matmul(pb[:], lhsT=dg[:, blk, k, :], rhs=hc[:, blk, t * 512 + k:t * 512 + k + 512],
                                         start=(k == 0), stop=(k == 30))
                        return r
                    S.op("tensor", mmc, reads=["dg%d" % blk, "hcp0", "hcp1"] + HCK, writes=[pk])
                    S.op("scalar", lambda e, pb=pb, ob=ob, blk=blk: e.activation(out=ob[:], in_=pb[:], func=AF.Identity, bias=dwb[:, blk:blk + 1]),
                         reads=[pk, "dwb"], writes=[ok])
                    S.dma("sync", lambda e, ob=ob, blk=blk, t=t: e.dma_start(out=convT_d[:, blk, t * 512:(t + 1) * 512], in_=ob[:]),
                          reads=[ok], writes=["convT_d"], is_output=True)
                    ti += 1
            S.barrier()
            S.flush(st)
        with contextlib.ExitStack() as s3:
            sb3, ps3 = mk(s3)
            Hin = sb3("Hin", [128, 2, G, NK], BF16)
            S.op("gpsimd", lambda e: e.memset(Hin[:], 0.0), writes=["Hin"])
            for ri in range(2):
                S.op(V, lambda e, ri=ri: e.tensor_copy(out=Hin[0:64, ri, :, 1:NK], in_=St[0:64, ri, 0:NK - 1, :].rearrange("p k g -> p g k")),
                     reads=["Hf"], writes=["Hin"])
                S.op("gpsimd", lambda e, ri=ri: e.tensor_copy(out=Hin[64:128, ri, :, 0:NK - 1], in_=St[64:128, ri, 1:NK, :].rearrange("p k g -> p g k")),
                     reads=["Hb"], writes=["Hin"])
            py = [ps3("py%d" % i, [128, 128]) for i in range(2)]
            Yo = [sb3("Yo%d" % i, [128, G, 128]) for i in range(2)]
            yi = 0
            for kb in range(4):
                yo = Yo[kb % 2]; yok = "Yo%d" % (kb % 2)
                for g in range(G):
                    pb = py[yi % 2]; pk = "py%d" % (yi % 2)

                    def mmy(e, pb=pb, g=g, kb=kb):
                        ks = slice(kb * 128, (kb + 1) * 128)
                        e.matmul(pb[:], lhsT=Ub[:, g, ks], rhs=Kloc[:, g, :], start=True, stop=False)
                        e.matmul(pb[:], lhsT=Hin[:, 0, g, ks], rhs=WoR[:, g, :], start=False, stop=False)
                        return e.matmul(pb[:], lhsT=Hin[:, 1, g, ks], rhs=WoI[:, g, :], start=False, stop=True)
                    S.op("tensor", mmy, reads=["Ub%d" % (g // 4), "Kloc", "Hin", "WoR", "WoI"], writes=[pk])
                    if yi % 2 == 0:
                        S.op("scalar", lambda e, pb=pb, g=g, yo=yo: e.copy(out=yo[:, g, :], in_=pb[:]), reads=[pk], writes=[yok])
                    else:
                        S.op(V, lambda e, pb=pb, g=g, yo=yo: e.tensor_copy(out=yo[:, g, :], in_=pb[:]), reads=[pk], writes=[yok])
                    yi += 1
                S.dma("sync", lambda e, kb=kb, yo=yo: e.dma_start(out=Y_d[kb], in_=yo[:]), reads=[yok], writes=["Y_d"], is_output=True)
            S.finish()
            S.flush(st)
    return nc


def prep_B(core, z_u, z_v, z_g, p):
    b, hf = core // 2, core % 2
    gs = slice(hf * G, (hf + 1) * G)
    cs = slice(hf * 256, (hf + 1) * 256)
    u = z_u[b][:, cs].reshape(NK, 8, G, 16)
    U = np.ascontiguousarray(u.transpose(1, 3, 2, 0).reshape(128, G, NK))
    fm = lambda a: np.ascontiguousarray(a[b][:, cs].T.reshape(2, 128, L).transpose(1, 0, 2))
    dp = lambda a: np.ascontiguousarray(a[:, gs, :].transpose(0, 2, 1).reshape(128, G))
    ldt = np.ascontiguousarray(np.broadcast_to(p["ssm_log_dt"][:, gs][:, None, :], (2, 64, G)).reshape(128, G))
    bb = lambda a: np.ascontiguousarray(np.broadcast_to(a[gs].transpose(1, 0, 2)[None], (2, 64, G, 16)).reshape(128, G, 16))
    cc = lambda a: np.ascontiguousarray(a[:, gs].transpose(0, 3, 1, 2).reshape(128, G, 16))
    dsk = p["ssm_d"][cs].reshape(G, 16)
    dbc = np.ascontiguousarray(np.broadcast_to(dsk[None, :, None, :], (128, G, 8, 16)).reshape(128, G, 128))
    s_idx = np.arange(128) // 16
    mf = (s_idx[None, :] >= s_idx[:, None]).astype(np.float32)
    mb = (s_idx[None, :] <= s_idx[:, None]).astype(np.float32)
    return {
        "U": U, "vT": fm(z_v), "gT": fm(z_g),
        "dww": np.ascontiguousarray(p["conv_dw_w"][:, cs].T.reshape(2, 128, 31).transpose(1, 0, 2)),
        "dwb": np.ascontiguousarray(p["conv_dw_b"][cs].reshape(2, 128).T),
        "a_re": dp(p["ssm_a_re"]), "a_im": dp(p["ssm_a_im"]), "log_dt": ldt,
        "b_re": bb(p["ssm_b_re"]), "b_im": bb(p["ssm_b_im"]),
        "c_re": cc(p["ssm_c_re"]), "c_im": cc(p["ssm_c_im"]),
        "dbc": dbc, "mask_f": mf, "mask_b": mb,
    }


def post_B(results):
    conv = np.zeros((4, L, 512), np.float32)
    y = np.zeros((4, L, 512), np.float32)
    for core in range(8):
        b, hf = core // 2, core % 2
        cT = results[core]["convT"]
        conv[b][:, hf * 256:(hf + 1) * 256] = cT.transpose(1, 0, 2).reshape(256, L).T
        Y = results[core]["Y"]
        yy = Y.reshape(NK, G, 8, 16).transpose(0, 2, 1, 3).reshape(L, 256)
        y[b][:, hf * 256:(hf + 1) * 256] = yy
    return conv, y


NT = 2048
D = 1024
DFF = 2816
NFB = DFF // 128
TT = 512
LN_EPS = 1e-5
RMS_EPS = 1e-6


def build_C(n_exp, moe, last, ffn=True):
    nc = bass.Bass("TRN2", target_bir_lowering=False)
    din = lambda name, shape: nc.dram_tensor(name, shape, F32, kind="ExternalInput").ap()
    convT_d = din("convT", [128, 4, NT]); yT_d = din("yT", [128, 4, NT])
    gcT_d = din("gcT", [128, 8, NT]); gsT_d = din("gsT", [128, 8, NT])
    x_d = din("x_tok", [128, 16, D])
    lng_d = din("lng", [128, 4]); lnb_d = din("lnb", [128, 4]); bglu_d = din("bglu", [128, 4])
    wcp_d = din("wcp", [512, D]); wglu_d = din("wglu", [512, 512]); wsp_d = din("wsp", [512, D]); wout_d = din("wout", [D, D])
    nfg_d = din("nfg", [128, D])
    if ffn:
        wg_d = din("wg", [n_exp, D, DFF]); wu_d = din("wu", [n_exp, D, DFF]); wd_d = din("wd", [n_exp, DFF, D])
    else:
        n_exp = 0
        h2T_o = nc.dram_tensor("h2T_out", [128, 8, NT], F32, kind="ExternalOutput").ap()
        gates_o = nc.dram_tensor("gates_out", [128, 16, 8], F32, kind="ExternalOutput").ap()
    if moe:
        rw_d = din("rw", [128, 8, 8]); rb_d = din("rb", [128, 8])
    if last:
        fg_d = din("fg", [128, D])
    out_d = nc.dram_tensor("out", [128, 16, D], F32, kind="ExternalOutput").ap()
    S = Sched(nc)
    V = "vector"; A = "scalar"; T = "tensor"
    with contextlib.ExitStack() as st:
        def mk(stack):
            return (lambda name, shape, dt=F32: stack.enter_context(nc.sbuf_tensor("sb_" + name, shape, dt)),
                    lambda name, shape, dt=F32: stack.enter_context(nc.psum_tensor("ps_" + name, shape, dt)))
        sb, ps = mk(st)
        xm = sb("xm", [128, 16, D])
        ident = sb("ident", [128, 128])
        onesm = sb("onesm", [128, 128])
        S.op("gpsimd", lambda e: e.memset(ident[:], 1.0), writes=["ident"])
        S.op("gpsimd", lambda e: e.affine_select(out=ident[:], in_=ident[:], pattern=[[-1, 128]], compare_op=ALU.is_equal,
                                                 fill=0.0, base=0, channel_multiplier=1), reads=["ident"], writes=["ident"])
        S.op("gpsimd", lambda e: e.memset(onesm[:], 1.0 / 512.0), writes=["onesm"])

        def ldsm(sbf, name, d, shape):
            t = sbf(name, shape)
            S.dma("sync", lambda e: e.dma_start(out=t[:], in_=d), writes=[name])
            return t
        nfg = ldsm(sb, "nfg", nfg_d, [128, D])
        if last:
            fg = ldsm(sb, "fg", fg_d, [128, D])
        if moe:
            rw = ldsm(sb, "rw", rw_d, [128, 8, 8]); rb = ldsm(sb, "rb", rb_d, [128, 8])
            gates = sb("gates", [128, 16, 8])

        with contextlib.ExitStack() as s1:
            sb1, ps1 = mk(s1)
            lng = ldsm(sb1, "lng", lng_d, [128, 4]); lnb = ldsm(sb1, "lnb", lnb_d, [128, 4]); bglu = ldsm(sb1, "bglu", bglu_d, [128, 4])
            wcp = sb1("wcp", [128, 4, D], BF16); wglu = sb1("wglu", [128, 4, 512], BF16)
            wsp = sb1("wsp", [128, 4, D], BF16); wout = sb1("wout", [128, 8, D], BF16)
            for t_, d_, nm in ((wcp, wcp_d, "wcp"), (wglu, wglu_d, "wglu"), (wsp, wsp_d, "wsp"), (wout, wout_d, "wout")):
                S.dma("gpsimd", lambda e, t_=t_, d_=d_: e.dma_start(out=t_[:], in_=d_.rearrange("(kb p) n -> p kb n", p=128)), writes=[nm])
            cv = sb1("cv", [128, 4, TT]); sq = sb1("sq", [128, 4, TT])
            yv = sb1("yv", [128, 4, TT]); ygb = sb1("ygb", [128, 4, TT], BF16)
            mean = sb1("mean", [128, TT]); var = sb1("var", [128, TT]); rstd = sb1("rstd", [128, TT])
            ca = sb1("ca", [128, 4, TT], BF16)
            gl = sb1("gl", [128, 4, TT]); y2 = sb1("y2", [128, 4, TT], BF16)
            gcb = [sb1("gcb%d" % i, [128, TT]) for i in range(2)]
            gsb = [sb1("gsb%d" % i, [128, TT]) for i in range(2)]
            m1 = [sb1("m1_%d" % i, [128, TT]) for i in range(2)]
            m2 = [sb1("m2_%d" % i, [128, TT]) for i in range(2)]
            mg = sb1("mg", [128, 8, TT], BF16)
            xin = [sb1("xin%d" % i, [128, D]) for i in range(2)]
            pmean = ps1("pmean", [128, TT]); pex2 = ps1("pex2", [128, TT])
            pA = [ps1("pA%d" % i, [128, TT]) for i in range(2)]
            pB = [ps1("pB%d" % i, [128, TT]) for i in range(2)]
            pW = [ps1("pW%d" % i, [128, TT]) for i in range(2)]
            fi = 0
            xi = 0
            for t in range(NT // TT):
                ts_ = slice(t * TT, (t + 1) * TT)
                S.dma("sync", lambda e, ts_=ts_: e.dma_start(out=cv[:], in_=convT_d[:, :, ts_]), writes=["cv"])
                S.dma("sync", lambda e, ts_=ts_: e.dma_start(out=yv[:], in_=yT_d[:, :, ts_]), writes=["yv"])
                S.op(A, lambda e: e.activation(out=sq[:], in_=cv[:], func=AF.Square), reads=["cv"], writes=["sq"])

                def mm_stats(e):
                    for kb in range(4):
                        e.matmul(pmean[:], lhsT=onesm[:], rhs=cv[:, kb, :], start=(kb == 0), stop=(kb == 3))
                    for kb in range(4):
                        r = e.matmul(pex2[:], lhsT=onesm[:], rhs=sq[:, kb, :], start=(kb == 0), stop=(kb == 3))
                    return r
                S.op(T, mm_stats, reads=["onesm", "cv", "sq"], writes=["pmean", "pex2"])
                S.op(V, lambda e: e.tensor_copy(out=mean[:], in_=pmean[:]), reads=["pmean"], writes=["mean"])
                S.op(V, lambda e: e.tensor_tensor(out=var[:], in0=mean[:], in1=mean[:], op=ALU.mult), reads=["mean"], writes=["var"])
                S.op(V, lambda e: e.tensor_tensor(out=var[:], in0=pex2[:], in1=var[:], op=ALU.subtract), reads=["pex2", "var"], writes=["var"])
                S.op(V, lambda e: e.tensor_scalar(out=var[:], in0=var[:], scalar1=LN_EPS, scalar2=None, op0=ALU.add), reads=["var"], writes=["var"])
                S.op(A, lambda e: e.sqrt(out=var[:], in_=var[:]), reads=["var"], writes=["var"])
                S.op(V, lambda e: e.reciprocal(out=rstd[:], in_=var[:]), reads=["var"], writes=["rstd"])
                for kb in range(4):
                    S.op(V, lambda e, kb=kb: e.tensor_tensor(out=cv[:, kb, :], in0=cv[:, kb, :], in1=mean[:], op=ALU.subtract),
                         reads=["cv", "mean"], writes=["cv"])
                    S.op(V, lambda e, kb=kb: e.tensor_tensor(out=cv[:, kb, :], in0=cv[:, kb, :], in1=rstd[:], op=ALU.mult),
                         reads=["cv", "rstd"], writes=["cv"])
                    S.op(A, lambda e, kb=kb: e.activation(out=ca[:, kb, :], in_=cv[:, kb, :], func=AF.Silu,
                                                         scale=lng[:, kb:kb + 1], bias=lnb[:, kb:kb + 1]),
                         reads=["cv", "lng", "lnb"], writes=["ca"])
                S.op(A, lambda e: e.activation(out=yv[:], in_=yv[:], func=AF.Gelu_apprx_tanh), reads=["yv"], writes=["yv"])
                S.op(V, lambda e: e.tensor_copy(out=ygb[:], in_=yv[:]), reads=["yv"], writes=["ygb"])
                for fo in range(4):
                    pb = pA[fi % 2]; pk = "pA%d" % (fi % 2); fi += 1

                    def mm_glu(e, pb=pb, fo=fo):
                        for kb in range(4):
                            r = e.matmul(pb[:], lhsT=wglu[:, kb, fo * 128:(fo + 1) * 128], rhs=ygb[:, kb, :], start=(kb == 0), stop=(kb == 3))
                        return r
                    S.op(T, mm_glu, reads=["wglu", "ygb"], writes=[pk])
                    S.op(A, lambda e, pb=pb, fo=fo: e.activation(out=gl[:, fo, :], in_=pb[:], func=AF.Sigmoid, bias=bglu[:, fo:fo + 1]),
                         reads=[pk, "bglu"], writes=["gl"])
                    S.op(V, lambda e, fo=fo: e.tensor_tensor(out=y2[:, fo, :], in0=yv[:, fo, :], in1=gl[:, fo, :], op=ALU.mult),
                         reads=["yv", "gl"], writes=["y2"])
                for fo in range(8):
                    pa = pA[fi % 2]; pak = "pA%d" % (fi % 2)
                    pb = pB[fi % 2]; pbk = "pB%d" % (fi % 2)
                    gc = gcb[fi % 2]; gck = "gcb%d" % (fi % 2)
                    gs = gsb[fi % 2]; gsk = "gsb%d" % (fi % 2)
                    ma = m1[fi % 2]; mak = "m1_%d" % (fi % 2)
                    mb_ = m2[fi % 2]; mbk = "m2_%d" % (fi % 2)
                    fi += 1
                    S.dma("sync", lambda e, gc=gc, fo=fo, ts_=ts_: e.dma_start(out=gc[:], in_=gcT_d[:, fo, ts_]), writes=[gck])
                    S.dma("sync", lambda e, gs=gs, fo=fo, ts_=ts_: e.dma_start(out=gs[:], in_=gsT_d[:, fo, ts_]), writes=[gsk])
                    S.op(A, lambda e, gc=gc: e.activation(out=gc[:], in_=gc[:], func=AF.Sigmoid), reads=[gck], writes=[gck])
                    S.op(A, lambda e, gs=gs: e.activation(out=gs[:], in_=gs[:], func=AF.Sigmoid), reads=[gsk], writes=[gsk])

                    def mm_cp(e, pa=pa, fo=fo):
                        for kb in range(4):
                            r = e.matmul(pa[:], lhsT=wcp[:, kb, fo * 128:(fo + 1) * 128], rhs=ca[:, kb, :], start=(kb == 0), stop=(kb == 3))
                        return r
                    S.op(T, mm_cp, reads=["wcp", "ca"], writes=[pak])

                    def mm_sp(e, pb=pb, fo=fo):
                        for kb in range(4):
                            r = e.matmul(pb[:], lhsT=wsp[:, kb, fo * 128:(fo + 1) * 128], rhs=y2[:, kb, :], start=(kb == 0), stop=(kb == 3))
                        return r
                    S.op(T, mm_sp, reads=["wsp", "y2"], writes=[pbk])
                    S.op(V, lambda e, pa=pa, gc=gc, ma=ma: e.tensor_tensor(out=ma[:], in0=pa[:], in1=gc[:], op=ALU.mult), reads=[pak, gck], writes=[mak])
                    S.op(V, lambda e, pb=pb, gs=gs, mb_=mb_: e.tensor_tensor(out=mb_[:], in0=pb[:], in1=gs[:], op=ALU.mult), reads=[pbk, gsk], writes=[mbk])
                    S.op(V, lambda e, ma=ma, mb_=mb_, fo=fo: e.tensor_tensor(out=mg[:, fo, :], in0=ma[:], in1=mb_[:], op=ALU.add),
                         reads=[mak, mbk], writes=["mg%d" % fo])
                for sub in range(TT // 128):
                    tt_ = t * (TT // 128) + sub
                    xb = xin[xi % 2]; xk = "xin%d" % (xi % 2); xi += 1
                    S.dma("sync", lambda e, xb=xb, tt_=tt_: e.dma_start(out=xb[:], in_=x_d[:, tt_, :]), writes=[xk])

                    def mm_wo(e, sub=sub):
                        for h in range(2):
                            for kb in range(8):
                                r = e.matmul(pW[h][:], lhsT=mg[:, kb, sub * 128:(sub + 1) * 128], rhs=wout[:, kb, h * 512:(h + 1) * 512],
                                             start=(kb == 0), stop=(kb == 7))
                        return r
                    S.op(T, mm_wo, reads=["wout"] + ["mg%d" % k for k in range(8)], writes=["pW0", "pW1"])
                    for h in range(2):
                        S.op(V, lambda e, h=h, xb=xb, tt_=tt_: e.tensor_tensor(out=xm[:, tt_, h * 512:(h + 1) * 512], in0=pW[h][:],
                                                                               in1=xb[:, h * 512:(h + 1) * 512], op=ALU.add),
                             reads=["pW%d" % h, xk], writes=["xm%d" % tt_])
            S.barrier()
            S.flush(st)

        with contextlib.ExitStack() as s2:
            sb2, ps2 = mk(s2)
            HT = NT // 2
            NSUB = HT // 128
            h2T = sb2("h2T", [128, 8, HT], BF16)
            h2f = sb2("h2f", [128, D])
            ssq = sb2("ssq", [128, 1]); rs = sb2("rs", [128, 1])
            if ffn:
                act = sb2("act", [128, NFB, HT], BF16)
                wdb = sb2("wdb", [128, NFB, D], BF16)
            WC = 256
            NCH = DFF // WC
            wgb = [sb2("wgb%d" % i, [128, 8, WC], BF16) for i in range(2)]
            wub = [sb2("wub%d" % i, [128, 8, WC], BF16) for i in range(2)]
            sg = [sb2("sg%d" % i, [128, TT], BF16) for i in range(2)]
            ptr = ps2("ptr", [128, 8 * 128])
            pg = [ps2("pg%d" % i, [128, TT]) for i in range(2)]
            pu = [ps2("pu%d" % i, [128, TT]) for i in range(2)]
            pd = ps2("pd", [128, D])
            if not moe:
                ob = sb2("ob", [128, D]); ob_ap = ob[:]; obk = "ob"
            if moe:
                h2T32 = sb2("h2T32", [128, 8, 128])
                ob_ap = h2T32[:].rearrange("p k t -> p (k t)"); obk = "h2T32"
                lg = sb2("lg", [128, 8]); l2 = sb2("l2", [128, 8]); eq1 = sb2("eq1", [128, 8]); eq2 = sb2("eq2", [128, 8])
                mx1 = sb2("mx1", [128, 1]); mx2 = sb2("mx2", [128, 1]); dd = sb2("dd", [128, 1]); w1 = sb2("w1", [128, 1]); w2 = sb2("w2", [128, 1])
                g1 = sb2("g1", [128, 8])
            wi = 0
            gi = 0
            for th in range(2):
                for sub in range(NSUB):
                    tt_ = th * NSUB + sub
                    xk = "xm%d" % tt_
                    S.op(A, lambda e, tt_=tt_: e.activation(out=ob_ap, in_=xm[:, tt_, :], func=AF.Square, accum_out=ssq[:]),
                         reads=[xk], writes=[obk, "ssq"])
                    S.op(V, lambda e: e.tensor_scalar(out=rs[:], in0=ssq[:], scalar1=1.0 / D, scalar2=RMS_EPS, op0=ALU.mult, op1=ALU.add),
                         reads=["ssq"], writes=["rs"])
                    S.op(A, lambda e: e.sqrt(out=rs[:], in_=rs[:]), reads=["rs"], writes=["rs"])
                    S.op(V, lambda e: e.reciprocal(out=rs[:], in_=rs[:]), reads=["rs"], writes=["rs"])
                    S.op(V, lambda e, tt_=tt_: e.scalar_tensor_tensor(out=h2f[:], in0=xm[:, tt_, :], scalar=rs[:, 0:1], in1=nfg[:],
                                                                      op0=ALU.mult, op1=ALU.mult),
                         reads=[xk, "rs", "nfg"], writes=["h2f"])

                    def tr8(e):
                        for kb in range(8):
                            r = e.transpose(ptr[:, kb * 128:(kb + 1) * 128], h2f[:, kb * 128:(kb + 1) * 128], ident[:])
                        return r
                    S.op(T, tr8, reads=["h2f", "ident"], writes=["ptr"])
                    S.op(V, lambda e, sub=sub: e.tensor_copy(out=h2T[:, :, sub * 128:(sub + 1) * 128], in_=ptr[:].rearrange("p (k t) -> p k t", t=128)),
                         reads=["ptr"], writes=["h2T_%d" % sub])
                    if moe:
                        S.op(A, lambda e: e.copy(out=h2T32[:], in_=ptr[:].rearrange("p (k t) -> p k t", t=128)),
                             reads=["ptr", "h2T_%d" % sub], writes=["h2T32"])
                        if not ffn:
                            S.dma("sync", lambda e, tt_=tt_: e.dma_start(out=h2T_o[:, :, tt_ * 128:(tt_ + 1) * 128], in_=h2T32[:]),
                                  reads=["h2T32"], writes=["h2T_o"], is_output=True)

                        def mm_r(e):
                            for kb in range(8):
                                r = e.matmul(pg[0][:, 0:8], lhsT=h2T32[:, kb, :], rhs=rw[:, kb, :], start=(kb == 0), stop=(kb == 7))
                            return r
                        S.op(T, mm_r, reads=["h2T32", "rw"], writes=["pg0"])
                        S.op(V, lambda e: e.tensor_tensor(out=lg[:], in0=pg[0][:, 0:8], in1=rb[:], op=ALU.add), reads=["pg0", "rb"], writes=["lg"])
                        S.op(V, lambda e: e.reduce_max(out=mx1[:], in_=lg[:], axis=AX.X), reads=["lg"], writes=["mx1"])
                        S.op(V, lambda e: e.tensor_scalar(out=eq1[:], in0=lg[:], scalar1=mx1[:, 0:1], scalar2=None, op0=ALU.is_equal),
                             reads=["lg", "mx1"], writes=["eq1"])
                        S.op(V, lambda e: e.scalar_tensor_tensor(out=l2[:], in0=eq1[:], scalar=-1e30, in1=lg[:], op0=ALU.mult, op1=ALU.add),
                             reads=["eq1", "lg"], writes=["l2"])
                        S.op(V, lambda e: e.reduce_max(out=mx2[:], in_=l2[:], axis=AX.X), reads=["l2"], writes=["mx2"])
                        S.op(V, lambda e: e.tensor_scalar(out=eq2[:], in0=l2[:], scalar1=mx2[:, 0:1], scalar2=None, op0=ALU.is_equal),
                             reads=["l2", "mx2"], writes=["eq2"])
                        S.op(V, lambda e: e.tensor_tensor(out=dd[:], in0=mx2[:], in1=mx1[:], op=ALU.subtract), reads=["mx1", "mx2"], writes=["dd"])
                        S.op(A, lambda e: e.activation(out=w2[:], in_=dd[:], func=AF.Sigmoid), reads=["dd"], writes=["w2"])
                        S.op(V, lambda e: e.tensor_scalar(out=w1[:], in0=w2[:], scalar1=-1.0, scalar2=1.0, op0=ALU.mult, op1=ALU.add),
                             reads=["w2"], writes=["w1"])
                        S.op(V, lambda e: e.tensor_scalar(out=g1[:], in0=eq1[:], scalar1=w1[:, 0:1], scalar2=None, op0=ALU.mult),
                             reads=["eq1", "w1"], writes=["g1"])
                        S.op(V, lambda e, tt_=tt_: e.scalar_tensor_tensor(out=gates[:, tt_, :], in0=eq2[:], scalar=w2[:, 0:1], in1=g1[:],
                                                                          op0=ALU.mult, op1=ALU.add),
                             reads=["eq2", "w2", "g1"], writes=["gates%d" % tt_])
                H2K = ["h2T_%d" % s_ for s_ in range(NSUB)]
                for ex in range(n_exp):
                    for q in range(2):
                        S.dma("gpsimd", lambda e, ex=ex, q=q: e.dma_start(
                            out=wdb[:, q * 11:(q + 1) * 11, :],
                            in_=wd_d[ex].rearrange("(fb p) n -> p fb n", p=128)[:, q * 11:(q + 1) * 11, :]), writes=["wdb%d" % q])
                    for c in range(NCH):
                        wgt = wgb[wi % 2]; wgk = "wgb%d" % (wi % 2)
                        wut = wub[wi % 2]; wuk = "wub%d" % (wi % 2)
                        wi += 1
                        cs = slice(c * WC, (c + 1) * WC)
                        S.dma("gpsimd", lambda e, wgt=wgt, ex=ex, cs=cs: e.dma_start(
                            out=wgt[:], in_=wg_d[ex].rearrange("(kb p) n -> p kb n", p=128)[:, :, cs]), writes=[wgk])
                        S.dma("gpsimd", lambda e, wut=wut, ex=ex, cs=cs: e.dma_start(
                            out=wut[:], in_=wu_d[ex].rearrange("(kb p) n -> p kb n", p=128)[:, :, cs]), writes=[wuk])
                        for fl in range(WC // 128):
                            fb = c * (WC // 128) + fl
                            for tt2 in range(HT // TT):
                                pgb = pg[gi % 2]; pgk = "pg%d" % (gi % 2)
                                pub = pu[gi % 2]; puk = "pu%d" % (gi % 2)
                                sgb = sg[gi % 2]; sgk = "sg%d" % (gi % 2)
                                gi += 1
                                tsl = slice(tt2 * TT, (tt2 + 1) * TT)

                                def mm_g(e, pgb=pgb, wgt=wgt, fl=fl, tsl=tsl):
                                    for kb in range(8):
                                        r = e.matmul(pgb[:], lhsT=wgt[:, kb, fl * 128:(fl + 1) * 128], rhs=h2T[:, kb, tsl], start=(kb == 0), stop=(kb == 7))
                                    return r
                                S.op(T, mm_g, reads=[wgk] + H2K, writes=[pgk])

                                def mm_u(e, pub=pub, wut=wut, fl=fl, tsl=tsl):
                                    for kb in range(8):
                                        r = e.matmul(pub[:], lhsT=wut[:, kb, fl * 128:(fl + 1) * 128], rhs=h2T[:, kb, tsl], start=(kb == 0), stop=(kb == 7))
                                    return r
                                S.op(T, mm_u, reads=[wuk] + H2K, writes=[puk])
                                S.op(A, lambda e, pgb=pgb, sgb=sgb: e.activation(out=sgb[:], in_=pgb[:], func=AF.Silu), reads=[pgk], writes=[sgk])
                                S.op(V, lambda e, pub=pub, sgb=sgb, fb=fb, tsl=tsl: e.tensor_tensor(out=act[:, fb, tsl], in0=pub[:], in1=sgb[:], op=ALU.mult),
                                     reads=[puk, sgk], writes=["act%d" % fb])
                    ACTK = ["act%d" % fb for fb in range(NFB)]
                    for sub in range(NSUB):
                        tt_ = th * NSUB + sub

                        def mm_d(e, sub=sub):
                            for h in range(2):
                                for fb in range(NFB):
                                    r = e.matmul(pd[:, h * 512:(h + 1) * 512], lhsT=act[:, fb, sub * 128:(sub + 1) * 128],
                                                 rhs=wdb[:, fb, h * 512:(h + 1) * 512], start=(fb == 0), stop=(fb == NFB - 1))
                            return r
                        S.op(T, mm_d, reads=ACTK + ["wdb0", "wdb1"], writes=["pd"])
                        if moe:
                            S.op(V, lambda e, tt_=tt_, ex=ex: e.scalar_tensor_tensor(out=xm[:, tt_, :], in0=pd[:], scalar=gates[:, tt_, ex:ex + 1],
                                                                                     in1=xm[:, tt_, :], op0=ALU.mult, op1=ALU.add),
                                 reads=["pd", "gates%d" % tt_, "xm%d" % tt_], writes=["xm%d" % tt_])
                        else:
                            S.op(V, lambda e, tt_=tt_: e.tensor_tensor(out=xm[:, tt_, :], in0=pd[:], in1=xm[:, tt_, :], op=ALU.add),
                                 reads=["pd", "xm%d" % tt_], writes=["xm%d" % tt_])
                if moe and not ffn and th == 1:
                    S.dma("sync", lambda e: e.dma_start(out=gates_o, in_=gates[:]), reads=["gates%d" % k for k in range(16)],
                          writes=["gates_o"], is_output=True)
                for sub in range(NSUB):
                    tt_ = th * NSUB + sub
                    xk = "xm%d" % tt_
                    if last:
                        S.op(A, lambda e, tt_=tt_: e.activation(out=h2f[:], in_=xm[:, tt_, :], func=AF.Square, accum_out=ssq[:]),
                             reads=[xk], writes=["h2f", "ssq"])
                        S.op(V, lambda e: e.tensor_scalar(out=rs[:], in0=ssq[:], scalar1=1.0 / D, scalar2=RMS_EPS, op0=ALU.mult, op1=ALU.add),
                             reads=["ssq"], writes=["rs"])
                        S.op(A, lambda e: e.sqrt(out=rs[:], in_=rs[:]), reads=["rs"], writes=["rs"])
                        S.op(V, lambda e: e.reciprocal(out=rs[:], in_=rs[:]), reads=["rs"], writes=["rs"])
                        S.op(V, lambda e, tt_=tt_: e.scalar_tensor_tensor(out=ob_ap, in0=xm[:, tt_, :], scalar=rs[:, 0:1], in1=fg[:],
                                                                          op0=ALU.mult, op1=ALU.mult),
                             reads=[xk, "rs", "fg"], writes=[obk])
                        S.dma("sync", lambda e, tt_=tt_: e.dma_start(out=out_d[:, tt_, :], in_=ob_ap), reads=[obk], writes=["out_d"], is_output=True)
                    else:
                        S.dma("sync", lambda e, tt_=tt_: e.dma_start(out=out_d[:, tt_, :], in_=xm[:, tt_, :]), reads=[xk], writes=["out_d"], is_output=True)
            S.finish()
            S.flush(st)
    return nc


def prep_C(core, layer, inp, conv, y, z, x, moe, last, ffn=True):
    b, hf = core // 2, core % 2
    tk = slice(hf * NT, (hf + 1) * NT)
    fm = lambda a, nb: np.ascontiguousarray(a.T.reshape(nb, 128, NT).transpose(1, 0, 2))
    col = lambda v, nb: np.ascontiguousarray(v.reshape(nb, 128).T)
    rep = lambda v: np.ascontiguousarray(np.broadcast_to(v[None, :], (128, v.shape[0])))
    m = {
        "convT": fm(conv[b, tk], 4), "yT": fm(y[b, tk], 4),
        "gcT": fm(z[b, tk, 1536:2560], 8), "gsT": fm(z[b, tk, 2560:3584], 8),
        "x_tok": np.ascontiguousarray(x[b, tk].reshape(16, 128, D).transpose(1, 0, 2)),
        "lng": col(inp["conv_ln_g"][layer], 4), "lnb": col(inp["conv_ln_b"][layer], 4), "bglu": col(inp["ssm_b_glu"][layer], 4),
        "wcp": inp["w_conv_proj"][layer], "wglu": inp["ssm_w_glu"][layer], "wsp": inp["w_ssm_proj"][layer], "wout": inp["w_out"][layer],
        "nfg": rep(inp["norm_ffn_g"][layer]),
    }
    i = layer // 2
    if moe:
        if ffn:
            m["wg"] = inp["moe_w_gate"][i]; m["wu"] = inp["moe_w_up"][i]; m["wd"] = inp["moe_w_down"][i]
        m["rw"] = np.ascontiguousarray(inp["router_w"][i].reshape(8, 128, 8).transpose(1, 0, 2))
        m["rb"] = rep(inp["router_b"][i])
    else:
        m["wg"] = inp["ffn_w_gate"][i][None]; m["wu"] = inp["ffn_w_up"][i][None]; m["wd"] = inp["ffn_w_down"][i][None]
    if last:
        m["fg"] = rep(inp["final_norm_g"])
    return m


def post_C(results):
    out = np.zeros((4, 4096, D), np.float32)
    for core in range(8):
        b, hf = core // 2, core % 2
        o = results[core]["out"]
        out[b, hf * NT:(hf + 1) * NT] = o.transpose(1, 0, 2).reshape(NT, D)
    return out


import contextlib

D = 1024
DFF = 2816
NFB = DFF // 128
NTOK = 16384
TB = 2048
TT = 512
RMS_EPS = 1e-6


def build_E():
    nc = bass.Bass("TRN2", target_bir_lowering=False)
    din = lambda name, shape: nc.dram_tensor(name, shape, F32, kind="ExternalInput").ap()
    hT_d = din("hT", [128, 8, NTOK])
    g_d = din("gate", [128, NTOK // 128])
    wg_d = din("wg", [D, DFF]); wu_d = din("wu", [D, DFF]); wd_d = din("wd", [DFF, D])
    out_d = nc.dram_tensor("part", [128, NTOK // 128, D], F32, kind="ExternalOutput").ap()
    S = Sched(nc)
    V = "vector"; A = "scalar"; T = "tensor"
    with contextlib.ExitStack() as st:
        sb = lambda name, shape, dt=F32: st.enter_context(nc.sbuf_tensor("sb_" + name, shape, dt))
        ps = lambda name, shape, dt=F32: st.enter_context(nc.psum_tensor("ps_" + name, shape, dt))
        gt = sb("gt", [128, NTOK // 128])
        S.dma("sync", lambda e: e.dma_start(out=gt[:], in_=g_d), writes=["gt"])
        h2T = sb("h2T", [128, 8, TB], BF16)
        act = sb("act", [128, NFB, TB], BF16)
        wdb = sb("wdb", [128, NFB, D], BF16)
        WC = 256
        NCH = DFF // WC
        wgb = [sb("wgb%d" % i, [128, 8, WC], BF16) for i in range(2)]
        wub = [sb("wub%d" % i, [128, 8, WC], BF16) for i in range(2)]
        sg = [sb("sg%d" % i, [128, TT], BF16) for i in range(2)]
        ob = [sb("ob%d" % i, [128, D]) for i in range(2)]
        pg = [ps("pg%d" % i, [128, TT]) for i in range(2)]
        pu = [ps("pu%d" % i, [128, TT]) for i in range(2)]
        pd = [ps("pd%d" % i, [128, D]) for i in range(2)]
        for q in range(2):
            S.dma("gpsimd", lambda e, q=q: e.dma_start(out=wdb[:, q * 11:(q + 1) * 11, :],
                                                       in_=wd_d.rearrange("(fb p) n -> p fb n", p=128)[:, q * 11:(q + 1) * 11, :]),
                  writes=["wdb%d" % q])
        wi = 0; gi = 0; oi = 0
        for tb in range(NTOK // TB):
            for q in range(4):
                S.dma("gpsimd", lambda e, tb=tb, q=q: e.dma_start(out=h2T[:, :, q * 512:(q + 1) * 512],
                                                                  in_=hT_d[:, :, tb * TB + q * 512: tb * TB + (q + 1) * 512]),
                      writes=["h2T_%d" % q])
            for c in range(NCH):
                wgt = wgb[wi % 2]; wgk = "wgb%d" % (wi % 2)
                wut = wub[wi % 2]; wuk = "wub%d" % (wi % 2)
                wi += 1
                cs = slice(c * WC, (c + 1) * WC)
                S.dma("gpsimd", lambda e, wgt=wgt, cs=cs: e.dma_start(out=wgt[:], in_=wg_d.rearrange("(kb p) n -> p kb n", p=128)[:, :, cs]), writes=[wgk])
                S.dma("gpsimd", lambda e, wut=wut, cs=cs: e.dma_start(out=wut[:], in_=wu_d.rearrange("(kb p) n -> p kb n", p=128)[:, :, cs]), writes=[wuk])
                for fl in range(WC // 128):
                    fb = c * (WC // 128) + fl
                    for tt2 in range(TB // TT):
                        pgb = pg[gi % 2]; pgk = "pg%d" % (gi % 2)
                        pub = pu[gi % 2]; puk = "pu%d" % (gi % 2)
                        sgb = sg[gi % 2]; sgk = "sg%d" % (gi % 2)
                        gi += 1
                        tsl = slice(tt2 * TT, (tt2 + 1) * TT)

                        def mm_g(e, pgb=pgb, wgt=wgt, fl=fl, tsl=tsl):
                            for kb in range(8):
                                r = e.matmul(pgb[:], lhsT=wgt[:, kb, fl * 128:(fl + 1) * 128], rhs=h2T[:, kb, tsl], start=(kb == 0), stop=(kb == 7))
                            return r
                        S.op(T, mm_g, reads=[wgk, "h2T_%d" % tt2], writes=[pgk])

                        def mm_u(e, pub=pub, wut=wut, fl=fl, tsl=tsl):
                            for kb in range(8):
                                r = e.matmul(pub[:], lhsT=wut[:, kb, fl * 128:(fl + 1) * 128], rhs=h2T[:, kb, tsl], start=(kb == 0), stop=(kb == 7))
                            return r
                        S.op(T, mm_u, reads=[wuk, "h2T_%d" % tt2], writes=[puk])
                        S.op(A, lambda e, pgb=pgb, sgb=sgb: e.activation(out=sgb[:], in_=pgb[:], func=AF.Silu), reads=[pgk], writes=[sgk])
                        S.op(V, lambda e, pub=pub, sgb=sgb, fb=fb, tsl=tsl: e.tensor_tensor(out=act[:, fb, tsl], in0=pub[:], in1=sgb[:], op=ALU.mult),
                             reads=[puk, sgk], writes=["act%d_%d" % (fb, tt2)])
            for sub in range(TB // 128):
                tt_ = tb * (TB // 128) + sub
                pdb = pd[oi % 2]; pdk = "pd%d" % (oi % 2)
                obb = ob[oi % 2]; obk = "ob%d" % (oi % 2)
                oi += 1

                def mm_d(e, sub=sub, pdb=pdb):
                    for h in range(2):
                        for fb in range(NFB):
                            r = e.matmul(pdb[:, h * 512:(h + 1) * 512], lhsT=act[:, fb, sub * 128:(sub + 1) * 128],
                                         rhs=wdb[:, fb, h * 512:(h + 1) * 512], start=(fb == 0), stop=(fb == NFB - 1))
                    return r
                S.op(T, mm_d, reads=["act%d_%d" % (fb, sub // 4) for fb in range(NFB)] + ["wdb0", "wdb1"], writes=[pdk])
                S.op(V, lambda e, pdb=pdb, obb=obb, tt_=tt_: e.tensor_scalar(out=obb[:], in0=pdb[:], scalar1=gt[:, tt_:tt_ + 1], scalar2=None, op0=ALU.mult),
                     reads=[pdk, "gt"], writes=[obk])
                S.dma("sync", lambda e, obb=obb, tt_=tt_: e.dma_start(out=out_d[:, tt_, :], in_=obb[:]), reads=[obk], writes=["out_d"], is_output=True)
        S.finish()
        S.emit()
    return nc


def build_F():
    nc = bass.Bass("TRN2", target_bir_lowering=False)
    din = lambda name, shape: nc.dram_tensor(name, shape, F32, kind="ExternalInput").ap()
    x_d = din("x_tok", [128, 16, D])
    p_d = din("parts", [8, 128, 16, D])
    fg_d = din("fg", [128, D])
    out_d = nc.dram_tensor("out", [128, 16, D], F32, kind="ExternalOutput").ap()
    S = Sched(nc)
    V = "vector"; A = "scalar"
    with contextlib.ExitStack() as st:
        sb = lambda name, shape, dt=F32: st.enter_context(nc.sbuf_tensor("sb_" + name, shape, dt))
        fg = sb("fg", [128, D])
        S.dma("sync", lambda e: e.dma_start(out=fg[:], in_=fg_d), writes=["fg"])
        xb = [sb("xb%d" % i, [128, D]) for i in range(2)]
        pb = [sb("pb%d" % i, [128, 8, D]) for i in range(2)]
        junk = sb("junk", [128, D])
        ssq = sb("ssq", [128, 1]); rs = sb("rs", [128, 1])
        ob = [sb("ob%d" % i, [128, D]) for i in range(2)]
        for tt_ in range(16):
            x = xb[tt_ % 2]; xk = "xb%d" % (tt_ % 2)
            p = pb[tt_ % 2]; pk = "pb%d" % (tt_ % 2)
            o = ob[tt_ % 2]; ok = "ob%d" % (tt_ % 2)
            S.dma("sync", lambda e, x=x, tt_=tt_: e.dma_start(out=x[:], in_=x_d[:, tt_, :]), writes=[xk])
            S.dma("sync", lambda e, p=p, tt_=tt_: e.dma_start(out=p[:], in_=p_d[:, :, tt_, :].rearrange("e p d -> p e d")), writes=[pk])
            for ex in range(8):
                eng = V if ex % 2 == 0 else "gpsimd"
                S.op(V, lambda e, x=x, p=p, ex=ex: e.tensor_tensor(out=x[:], in0=x[:], in1=p[:, ex, :], op=ALU.add), reads=[xk, pk], writes=[xk])
            S.op(A, lambda e, x=x: e.activation(out=junk[:], in_=x[:], func=AF.Square, accum_out=ssq[:]), reads=[xk], writes=["junk", "ssq"])
            S.op(V, lambda e: e.tensor_scalar(out=rs[:], in0=ssq[:], scalar1=1.0 / D, scalar2=RMS_EPS, op0=ALU.mult, op1=ALU.add), reads=["ssq"], writes=["rs"])
            S.op(A, lambda e: e.sqrt(out=rs[:], in_=rs[:]), reads=["rs"], writes=["rs"])
            S.op(V, lambda e: e.reciprocal(out=rs[:], in_=rs[:]), reads=["rs"], writes=["rs"])
            S.op(V, lambda e, x=x, o=o: e.scalar_tensor_tensor(out=o[:], in0=x[:], scalar=rs[:, 0:1], in1=fg[:], op0=ALU.mult, op1=ALU.mult),
                 reads=[xk, "rs", "fg"], writes=[ok])
            S.dma("sync", lambda e, o=o, tt_=tt_: e.dma_start(out=out_d[:, tt_, :], in_=o[:]), reads=[ok], writes=["out_d"], is_output=True)
        S.finish()
        S.emit()
    return nc


_CACHE = {}


def _get(name, fn):
    if name not in _CACHE:
        _CACHE[name] = fn()
    return _CACHE[name]


def kernel(**inputs):
    inp = {k: np.ascontiguousarray(np.asarray(v, dtype=np.float32)) for k, v in inputs.items()}
    x = inp["x"]
    cores = list(range(8))
    for layer in range(2):
        ncA = _get("A", lambda: build_A(3584))
        gcol = np.ascontiguousarray(inp["norm_mix_g"][layer].reshape(8, 128).T)
        in_maps = []
        for c in cores:
            b, hf = c // 2, c % 2
            xs = x[b, hf * 2048:(hf + 1) * 2048]
            in_maps.append({"xT": np.ascontiguousarray(xs.T.reshape(8, 128, 2048).transpose(1, 0, 2)), "gcol": gcol, "w": inp["w_in"][layer]})
        res = run_bass_kernel_spmd(ncA, in_maps, core_ids=cores)
        z = np.zeros((4, 4096, 3584), np.float32)
        for c in cores:
            b, hf = c // 2, c % 2
            z[b, hf * 2048:(hf + 1) * 2048] = res.results[c]["zT"].reshape(3584, 2048).T
        p = {k: inp[k][layer] for k in ["conv_dw_w", "conv_dw_b", "ssm_a_re", "ssm_a_im", "ssm_log_dt", "ssm_b_re", "ssm_b_im",
                                         "ssm_c_re", "ssm_c_im", "ssm_d"]}
        ncB = _get("B", build_B)
        z_v = z[..., 0:512]; z_g = z[..., 512:1024]; z_u = z[..., 1024:1536]
        res = run_bass_kernel_spmd(ncB, [prep_B(c, z_u, z_v, z_g, p) for c in cores], core_ids=cores)
        conv, y = post_B(res.results)
        if layer % 2 == 0:
            ncC = _get("C0", lambda: build_C(1, False, False))
            res = run_bass_kernel_spmd(ncC, [prep_C(c, layer, inp, conv, y, z, x, False, False) for c in cores], core_ids=cores)
            x = post_C(res.results)
        else:
            ncC = _get("C1", lambda: build_C(0, True, False, ffn=False))
            res = run_bass_kernel_spmd(ncC, [prep_C(c, layer, inp, conv, y, z, x, True, False, ffn=False) for c in cores], core_ids=cores)
            xmid = [res.results[c]["out"] for c in cores]
            hT_all = np.ascontiguousarray(np.concatenate([res.results[c]["h2T_out"] for c in cores], axis=2))
            gates_all = np.concatenate([res.results[c]["gates_out"] for c in cores], axis=1)
            i = layer // 2
            ncE = _get("E", build_E)
            in_maps = [{"hT": hT_all, "gate": np.ascontiguousarray(gates_all[:, :, e]),
                        "wg": inp["moe_w_gate"][i, e], "wu": inp["moe_w_up"][i, e], "wd": inp["moe_w_down"][i, e]} for e in cores]
            res = run_bass_kernel_spmd(ncE, in_maps, core_ids=cores)
            parts = [res.results[e]["part"] for e in cores]
            ncF = _get("F", build_F)
            fg = np.ascontiguousarray(np.broadcast_to(inp["final_norm_g"][None, :], (128, 1024)))
            in_maps = []
            for c in cores:
                pc = np.ascontiguousarray(np.stack([parts[e][:, c * 16:(c + 1) * 16, :] for e in cores], axis=0))
                in_maps.append({"x_tok": xmid[c], "parts": pc, "fg": fg})
            res = run_bass_kernel_spmd(ncF, in_maps, core_ids=cores)
            x = post_C(res.results)
    return x
```

```python
import contextlib
import math
import numpy as np
import concourse.bass as bass
import concourse.mybir as mybir
from concourse.bass_utils import run_bass_kernel_spmd

F32 = mybir.dt.float32
BF16 = mybir.dt.bfloat16
AF = mybir.ActivationFunctionType
ALU = mybir.AluOpType
AX = mybir.AxisListType

ENGS = ["tensor", "vector", "scalar", "gpsimd", "sync"]


def _flat(keys):
    out = []
    for k in keys:
        if isinstance(k, (list, tuple)):
            out.extend(_flat(k))
        else:
            out.append(k)
    return out


class Sched:
    def __init__(self, nc):
        self.nc = nc
        self.q = {e: [] for e in ENGS}
        self.cnt = {e: 0 for e in ENGS}
        self.last_w = {}
        self.readers = {}
        self.waited = {e: {} for e in ENGS}
        self.dma_cnt = {}
        self.semkeys = list(ENGS)
        self.out_tokens = []

    def _deps(self, reads, writes):
        deps = {}
        reads = _flat(reads)
        writes = _flat(writes)

        def add(tok):
            if tok is None:
                return
            k, v = tok
            if deps.get(k, 0) < v:
                deps[k] = v

        for b in reads:
            add(self.last_w.get(b))
        for b in writes:
            add(self.last_w.get(b))
            for t in self.readers.get(b, ()):
                add(t)
        return deps

    def _emit_waits(self, eng, deps):
        w = self.waited[eng]
        todo = []
        for k, v in deps.items():
            if w.get(k, 0) >= v:
                continue
            w[k] = v
            todo.append((k, v))
        return todo

    def _commit(self, tok, reads, writes):
        reads = _flat(reads)
        writes = _flat(writes)
        for b in reads:
            self.readers.setdefault(b, []).append(tok)
        for b in writes:
            self.last_w[b] = tok
            self.readers[b] = []

    def op(self, eng, fn, reads=(), writes=()):
        deps = self._deps(reads, writes)
        todo = self._emit_waits(eng, deps)
        self.cnt[eng] += 1
        n = self.cnt[eng]
        tok = (eng, n)
        self.waited[eng][eng] = max(self.waited[eng].get(eng, 0), 0)
        self.q[eng].append(("op", todo, fn, eng))
        self._commit(tok, reads, writes)
        return tok

    def dma(self, eng, fn, reads=(), writes=(), key=None, is_output=False):
        deps = self._deps(reads, writes)
        todo = self._emit_waits(eng, deps)
        if key is None:
            key = _flat(writes)[0]
        if not hasattr(self, "dma_slot"):
            self.dma_slot = {}
            self.slot_val = []
            self.slot_free = []
        if key not in self.dma_slot:
            if self.slot_free:
                slot = self.slot_free.pop()
            else:
                slot = len(self.slot_val)
                self.slot_val.append(0)
                self.semkeys.append(("slot", slot))
            self.dma_slot[key] = slot
        slot = self.dma_slot[key]
        self.slot_val[slot] += 16
        sk = ("slot", slot)
        tok = (sk, self.slot_val[slot])
        self.q[eng].append(("dma", todo, fn, sk))
        self._commit(tok, reads, writes)
        if is_output:
            self.out_tokens.append(tok)
        return tok

    def barrier(self):
        deps = {e: self.cnt[e] for e in ENGS if self.cnt[e] > 0}
        if hasattr(self, "dma_slot"):
            for i, v in enumerate(self.slot_val):
                if v > 0:
                    deps[("slot", i)] = v
        for e in ENGS:
            todo = self._emit_waits(e, dict(deps))
            self.q[e].append(("wait", todo, None, None))
        if hasattr(self, "dma_slot"):
            for k, slot in self.dma_slot.items():
                if slot not in self.slot_free:
                    self.slot_free.append(slot)
            self.dma_slot = {}

    def finish(self):
        deps = {}
        for k, v in self.out_tokens:
            deps[k] = max(deps.get(k, 0), v)
        todo = self._emit_waits("sync", deps)
        self.q["sync"].append(("wait", todo, None, None))

    def flush(self, stack):
        nc = self.nc
        if not hasattr(self, "sems"):
            self.sems = {}
        sems = self.sems
        for k in self.semkeys:
            if k not in sems:
                sems[k] = stack.enter_context(nc.semaphore("s%d" % len(sems)))
        q = self.q
        self.q = {e: [] for e in ENGS}
        with nc.Block() as block:
            def run(engname, e):
                for kind, todo, fn, key in q[engname]:
                    for k, v in todo:
                        e.wait_ge(sems[k], v)
                    if kind == "op":
                        fn(e).then_inc(sems[key], 1)
                    elif kind == "dma":
                        fn(e).then_inc(sems[key], 16)

            @block.tensor
            def _(e):
                run("tensor", e)

            @block.vector
            def _(e):
                run("vector", e)

            @block.scalar
            def _(e):
                run("scalar", e)

            @block.gpsimd
            def _(e):
                run("gpsimd", e)

            @block.sync
            def _(e):
                run("sync", e)

    def emit(self):
        self._st = contextlib.ExitStack()
        self.flush(self._st)
        self._st.close()
from concourse.ap import AP
NT = 2048
D = 1024
KB = 8
TT = 512
L = 4096
NK = L // 8
G = 16
TWO_PI = 2.0 * math.pi


def rev_ap(ap, dim):
    pat = [list(x) for x in ap.ap]
    step, cnt = pat[dim]
    off = ap.offset + step * (cnt - 1)
    pat[dim] = [-step, cnt]
    return AP(ap.tensor, off, pat)


DFF = 2816
NFB = DFF // 128
TT = 512
LN_EPS = 1e-5
RMS_EPS = 1e-6


def emit_A(nc, S, sst, d, uid, ntiles, gate_tiles, d_in=3584, eps=1e-6):
    nfo = d_in // 128
    xT = d["xT"]; gcol = d["gcol"]; w = d["w"]
    wv = w.rearrange("(kb p) n -> p kb n", p=128)
    with contextlib.ExitStack() as st:
        sb = lambda name, shape, dt: st.enter_context(nc.sbuf_tensor("sb%d_" % uid + name, shape, dt))
        ps = lambda name, shape, dt: st.enter_context(nc.psum_tensor("ps%d_" % uid + name, shape, dt))
        wsb = sb("wsb", [128, KB, d_in], BF16)
        g_sb = sb("g_sb", [128, KB], F32)
        ones = sb("ones", [128, 128], F32)
        xt = [sb("xt%d" % i, [128, KB, TT], F32) for i in range(2)]
        sq = sb("sq", [128, KB, TT], F32)
        rstd = sb("rstd", [128, TT], F32)
        hT = [sb("hT%d" % i, [128, KB, TT], BF16) for i in range(2)]
        zo = [sb("zo%d" % i, [128, TT], F32) for i in range(4)]
        pss = ps("pss", [128, TT], F32)
        pz = [ps("pz%d" % i, [128, TT], F32) for i in range(4)]

        S.op("vector", lambda e: e.memset(ones[:], 1.0), writes=["ones"])
        S.dma("sync", lambda e: e.dma_start(out=g_sb[:], in_=gcol), writes=["g_sb"])
        WCH = 512
        nwch = d_in // WCH
        for c in range(nwch):
            S.dma("gpsimd", lambda e, c=c: e.dma_start(out=wsb[:, :, c * WCH:(c + 1) * WCH],
                                                       in_=wv[:, :, c * WCH:(c + 1) * WCH]),
                  writes=["w%d" % c])
        nt = ntiles
        oi = 0

        def prep(t):
            xb = xt[t % 2]
            hb = hT[t % 2]
            S.dma("sync", lambda e, xb=xb, t=t: e.dma_start(out=xb[:], in_=xT[:, :, t * TT:(t + 1) * TT]),
                  writes=["xt%d" % (t % 2)])
            S.op("scalar", lambda e, xb=xb: e.activation(out=sq[:], in_=xb[:], func=AF.Square),
                 reads=["xt%d" % (t % 2)], writes=["sq"])

            def mm_ss(e):
                for kb in range(KB):
                    r = e.matmul(pss[:], lhsT=ones[:], rhs=sq[:, kb, :], start=(kb == 0), stop=(kb == KB - 1))
                return r
            S.op("tensor", mm_ss, reads=["ones", "sq"], writes=["pss"])
            S.op("vector", lambda e: e.tensor_scalar(out=rstd[:], in0=pss[:], scalar1=1.0 / D, scalar2=eps,
                                                     op0=ALU.mult, op1=ALU.add),
                 reads=["pss"], writes=["rstd"])
            S.op("scalar", lambda e: e.sqrt(out=rstd[:], in_=rstd[:]), reads=["rstd"], writes=["rstd"])
            S.op("vector", lambda e: e.reciprocal(out=rstd[:], in_=rstd[:]), reads=["rstd"], writes=["rstd"])
            for kb in range(KB):
                S.op("vector", lambda e, kb=kb, xb=xb, hb=hb: e.scalar_tensor_tensor(
                    out=hb[:, kb, :], in0=xb[:, kb, :], scalar=g_sb[:, kb:kb + 1], in1=rstd[:],
                    op0=ALU.mult, op1=ALU.mult),
                    reads=["xt%d" % (t % 2), "rstd", "g_sb"], writes=["hT%d_%d" % (t % 2, kb)])

        def tile(t):
            nonlocal oi
            hb = hT[t % 2]
            def fo_block(fo, dst):
                nonlocal oi
                pb = pz[oi % 4]
                ob = zo[oi % 4]
                pk = "pz%d" % (oi % 4)
                ok = "zo%d" % (oi % 4)

                def mm(e, fo=fo, pb=pb, hb=hb):
                    for kb in range(KB):
                        r = e.matmul(pb[:], lhsT=wsb[:, kb, fo * 128:(fo + 1) * 128], rhs=hb[:, kb, :],
                                     start=(kb == 0), stop=(kb == KB - 1))
                    return r
                S.op("tensor", mm, reads=["w%d" % (fo * 128 // WCH)] + ["hT%d_%d" % (t % 2, kb) for kb in range(KB)],
                     writes=[pk])
                if oi % 2 == 0:
                    S.op("scalar", lambda e, pb=pb, ob=ob: e.copy(out=ob[:], in_=pb[:]), reads=[pk], writes=[ok])
                else:
                    S.op("vector", lambda e, pb=pb, ob=ob: e.tensor_copy(out=ob[:], in_=pb[:]), reads=[pk], writes=[ok])
                S.dma("sync", lambda e, ob=ob, dst=dst: e.dma_start(out=dst, in_=ob[:]), reads=[ok], writes=["zscr"])
                oi += 1
            tsl = slice(t * TT, (t + 1) * TT)
            for fo in range(4):
                fo_block(fo, d["vT"][:, fo, tsl])
            for fo in range(4, 8):
                fo_block(fo, d["gT"][:, fo - 4, tsl])
            for sub in range(TT // 128):
                pb = pz[oi % 4]; ob = zo[oi % 4]; pk = "pz%d" % (oi % 4); ok = "zo%d" % (oi % 4)

                def mmu(e, pb=pb, hb=hb, sub=sub):
                    for kb in range(KB):
                        r = e.matmul(pb[:], lhsT=hb[:, kb, sub * 128:(sub + 1) * 128], rhs=wsb[:, kb, 1024:1536],
                                     start=(kb == 0), stop=(kb == KB - 1))
                    return r
                S.op("tensor", mmu, reads=["w2"] + ["hT%d_%d" % (t % 2, kb) for kb in range(KB)], writes=[pk])
                if oi % 2 == 0:
                    S.op("scalar", lambda e, pb=pb, ob=ob: e.copy(out=ob[:], in_=pb[:]), reads=[pk], writes=[ok])
                else:
                    S.op("vector", lambda e, pb=pb, ob=ob: e.tensor_copy(out=ob[:], in_=pb[:]), reads=[pk], writes=[ok])
                r0 = t * TT + sub * 128
                S.dma("sync", lambda e, ob=ob, r0=r0: e.dma_start(out=d["zu"][r0:r0 + 128, :], in_=ob[:]), reads=[ok], writes=["zscr"])
                oi += 1
            if t + 1 < nt:
                prep(t + 1)
            if t < gate_tiles:
                for fo in range(12, 20):
                    fo_block(fo, d["gcT"][:, fo - 12, tsl])
                for fo in range(20, 28):
                    fo_block(fo, d["gsT"][:, fo - 20, tsl])
        prep(0)
        for t in range(nt):
            tile(t)
        S.barrier()
        S.flush(sst)


def emit_B(nc, S, sst, d, uid, own_only=False):
    zu_d = d["zu"]; vT_d = d["vT"]; gT_d = d["gT"]; dww_d = d["dww"]; dwb_d = d["dwb"]
    are_d = d["a_re"]; aim_d = d["a_im"]; ldt_d = d["log_dt"]; bre_d = d["b_re"]; bim_d = d["b_im"]
    cre_d = d["c_re"]; cim_d = d["c_im"]; dbc_d = d["dbc"]; mf_d = d["mask_f"]; mb_d = d["mask_b"]
    convT_d = d["convT"]; ytok_d = d["ytok"]
    V = "vector"
    with contextlib.ExitStack() as st:
        def mk(stack):
            return (lambda name, shape, dt=F32: stack.enter_context(nc.sbuf_tensor("sb%d_" % uid + name, shape, dt)),
                    lambda name, shape, dt=F32: stack.enter_context(nc.psum_tensor("ps%d_" % uid + name, shape, dt)))
        sb, ps = mk(st)
        ident = sb("ident", [128, 128])
        identb = sb("identb", [128, 128], BF16)
        dww = sb("dww", [128, 2, 31]); dwb = sb("dwb", [128, 2])
        WstT = sb("WstT", [128, G, 2, 128], BF16)
        Kloc = sb("Kloc", [128, G, 128], BF16)
        WoR = sb("WoR", [128, G, 128], BF16); WoI = sb("WoI", [128, G, 128], BF16)
        AR2 = sb("AR2", [128, 2, G]); AI2 = sb("AI2", [128, 2, G])
        Ub = sb("Ub", [128, G, NK], BF16)
        TR = sb("TR", [128, 2, 16, G]); TI = sb("TI", [128, 2, 16, G])
        AR128 = sb("AR128", [128, 2, G]); AI128 = sb("AI128", [128, 2, G])
        S.dma("sync", lambda e: e.dma_start(out=dww[:], in_=dww_d), writes=["dww"])
        S.dma("sync", lambda e: e.dma_start(out=dwb[:], in_=dwb_d), writes=["dwb"])
        S.op("gpsimd", lambda e: e.memset(ident[:], 1.0), writes=["ident"])
        S.op("gpsimd", lambda e: e.affine_select(out=ident[:], in_=ident[:], pattern=[[-1, 128]], compare_op=ALU.is_equal,
                                                 fill=0.0, base=0, channel_multiplier=1), reads=["ident"], writes=["ident"])
        S.op(V, lambda e: e.tensor_copy(out=identb[:], in_=ident[:]), reads=["ident"], writes=["identb"])

        with contextlib.ExitStack() as s1:
            sb1, ps1 = mk(s1)

            def ld(name, d, shape):
                t = sb1(name, shape)
                S.dma("sync", lambda e: e.dma_start(out=t[:], in_=d), writes=[name])
                return t
            are = ld("are", are_d, [128, G]); aim = ld("aim", aim_d, [128, G]); ldt = ld("ldt", ldt_d, [128, G])
            bre = ld("bre", bre_d, [128, G, 16]); bim = ld("bim", bim_d, [128, G, 16])
            cre = ld("cre", cre_d, [128, G, 16]); cim = ld("cim", cim_d, [128, G, 16])
            dbc = ld("dbc", dbc_d, [128, G, 128]); mf = ld("mf", mf_d, [128, 128]); mb = ld("mb", mb_d, [128, 128])
            Zs = [sb1("Zs%d" % i, [128, 8, 256]) for i in range(2)]
            Zp = [sb1("Zp%d" % i, [128, 16, 8, 16]) for i in range(2)]
            pzt = [ps1("pzt%d" % i, [128, 4, 128]) for i in range(2)]
            zi = 0
            for kb4 in range(NK // 128):
                zs = Zs[kb4 % 2]; zk = "Zs%d" % (kb4 % 2)
                S.dma("sync", lambda e, zs=zs, kb4=kb4: e.dma_start(
                    out=zs[:], in_=zu_d[kb4 * 1024:(kb4 + 1) * 1024, :].rearrange("(k s) c -> k s c", s=8)), writes=[zk])
                zp = Zp[kb4 % 2]; zpk = "Zp%d" % (kb4 % 2)
                S.op("scalar", lambda e, zs=zs, zp=zp: e.copy(out=zp[:].rearrange("p g s c -> p s g c"),
                                                              in_=zs[:].rearrange("p s (g c) -> p s g c", c=16)),
                     reads=[zk], writes=[zpk])
                for g4 in range(4):
                    pb = pzt[zi % 2]; pk = "pzt%d" % (zi % 2); zi += 1

                    def trz(e, pb=pb, zp=zp, g4=g4):
                        for j in range(4):
                            g = g4 * 4 + j
                            r = e.transpose(pb[:, j, :], zp[:, g].rearrange("p s c -> p (s c)"), ident[:])
                        return r
                    S.op("tensor", trz, reads=[zpk, "ident"], writes=[pk])
                    S.op("scalar", (lambda e, pb=pb, g4=g4, kb4=kb4: e.copy(out=Ub[:, g4 * 4:(g4 + 1) * 4, kb4 * 128:(kb4 + 1) * 128], in_=pb[:])),
                         reads=[pk], writes=["Ub%d_%d" % (g4, kb4)])
            cnt = [0]
            pools = {}

            def tmp(shape=(128, G), dt=F32, persist=True):
                shape = tuple(shape)
                if persist:
                    cnt[0] += 1
                    nm = "t%d" % cnt[0]
                    return sb1(nm, list(shape), dt), nm
                pl = pools.setdefault(shape, {"i": 0, "bufs": []})
                npool = 8
                if len(pl["bufs"]) < npool:
                    cnt[0] += 1
                    nm = "tp%d" % cnt[0]
                    pl["bufs"].append((sb1(nm, list(shape), dt), nm))
                r = pl["bufs"][pl["i"] % npool]
                pl["i"] += 1
                return r

            def tt(o, ok, a, ak, b, bk, op):
                S.op(V, lambda e: e.tensor_tensor(out=o, in0=a, in1=b, op=op), reads=[ak, bk], writes=[ok])

            def ts(o, ok, a, ak, s1_, s2_, op0, op1=None):
                if op1 is None:
                    S.op(V, lambda e: e.tensor_single_scalar(out=o, in_=a, scalar=s1_, op=op0), reads=[ak], writes=[ok])
                else:
                    S.op(V, lambda e: e.tensor_scalar(out=o, in0=a, scalar1=s1_, scalar2=s2_, op0=op0, op1=op1), reads=[ak], writes=[ok])

            def act(o, ok, a, ak, f):
                S.op("scalar", lambda e: e.activation(out=o, in_=a, func=f), reads=[ak], writes=[ok])

            def new_tt(a, ak, b, bk, op, shape=(128, G), persist=True):
                o, ok = tmp(shape, persist=persist)
                tt(o[:], ok, a, ak, b, bk, op)
                return o, ok

            def cmul_into(ore, orek, oim, oimk, ar_, ark, ai_, aik, br_, brk, bi_, bik, shape, sign=1.0):
                t1, k1 = new_tt(ar_, ark, br_, brk, ALU.mult, shape, False)
                t2, k2 = new_tt(ai_, aik, bi_, bik, ALU.mult, shape, False)
                tt(ore, orek, t1[:], k1, t2[:], k2, ALU.subtract)
                t3, k3 = new_tt(ar_, ark, bi_, bik, ALU.mult, shape, False)
                t4, k4 = new_tt(ai_, aik, br_, brk, ALU.mult, shape, False)
                tt(oim, oimk, t3[:], k3, t4[:], k4, ALU.add)

            def cmul(ar_, ark, ai_, aik, br_, brk, bi_, bik, shape):
                re, rk = tmp(shape); im, ik = tmp(shape)
                cmul_into(re[:], rk, im[:], ik, ar_, ark, ai_, aik, br_, brk, bi_, bik, shape)
                return re, rk, im, ik

            dt_, dtk = tmp(); act(dt_[:], dtk, ldt[:], "ldt", AF.Exp)
            adr, adrk = new_tt(are[:], "are", dt_[:], dtk, ALU.mult)
            adi, adik = new_tt(aim[:], "aim", dt_[:], dtk, ALU.mult)
            mag, magk = tmp(); act(mag[:], magk, adr[:], adrk, AF.Exp)

            def reduced(shift):
                r, rk = tmp(); ts(r[:], rk, adi[:], adik, 1.0 / TWO_PI, shift, ALU.mult, ALU.add)
                ni, nik = tmp((128, G), mybir.dt.int32)
                S.op(V, lambda e: e.tensor_copy(out=ni[:], in_=r[:]), reads=[rk], writes=[nik])
                nf, nfk = tmp()
                S.op(V, lambda e: e.tensor_copy(out=nf[:], in_=ni[:]), reads=[nik], writes=[nfk])
                fr, frk = new_tt(r[:], rk, nf[:], nfk, ALU.subtract)
                ng, ngk = tmp(); ts(ng[:], ngk, fr[:], frk, 0.0, None, ALU.is_lt)
                fr2, fr2k = new_tt(fr[:], frk, ng[:], ngk, ALU.add)
                th, thk = tmp(); ts(th[:], thk, fr2[:], fr2k, TWO_PI, -math.pi, ALU.mult, ALU.add)
                th2, th2k = tmp(); ts(th2[:], th2k, th[:], thk, 3.1415925, -3.1415925, ALU.min, ALU.max)
                o, ok = tmp(); act(o[:], ok, th2[:], th2k, AF.Sin)
                return o, ok
            sn, snk = reduced(0.5)
            cs, csk = reduced(0.75)
            abr, abrk = new_tt(mag[:], magk, cs[:], csk, ALU.mult)
            abi, abik = new_tt(mag[:], magk, sn[:], snk, ALU.mult)
            nr, nrk = tmp(); ts(nr[:], nrk, abr[:], abrk, -1.0, None, ALU.add)
            d1, d1k = new_tt(are[:], "are", are[:], "are", ALU.mult)
            d2, d2k = new_tt(aim[:], "aim", aim[:], "aim", ALU.mult)
            den, denk = new_tt(d1[:], d1k, d2[:], d2k, ALU.add)
            rden, rdenk = tmp(); S.op(V, lambda e: e.reciprocal(out=rden[:], in_=den[:]), reads=[denk], writes=[rdenk])
            u1, u1k = new_tt(nr[:], nrk, are[:], "are", ALU.mult)
            u2, u2k = new_tt(abi[:], abik, aim[:], "aim", ALU.mult)
            u3, u3k = new_tt(u1[:], u1k, u2[:], u2k, ALU.add)
            qre, qrek = new_tt(u3[:], u3k, rden[:], rdenk, ALU.mult)
            u4, u4k = new_tt(abi[:], abik, are[:], "are", ALU.mult)
            u5, u5k = new_tt(nr[:], nrk, aim[:], "aim", ALU.mult)
            u6, u6k = new_tt(u4[:], u4k, u5[:], u5k, ALU.subtract)
            qim, qimk = new_tt(u6[:], u6k, rden[:], rdenk, ALU.mult)
            m1, m1k = new_tt(abr[:], abrk, abr[:], abrk, ALU.mult)
            m2, m2k = new_tt(abi[:], abik, abi[:], abik, ALU.mult)
            m3, m3k = new_tt(m1[:], m1k, m2[:], m2k, ALU.add)
            rm, rmk = tmp(); S.op(V, lambda e: e.reciprocal(out=rm[:], in_=m3[:]), reads=[m3k], writes=[rmk])
            ibr, ibrk = new_tt(abr[:], abrk, rm[:], rmk, ALU.mult)
            ibi0, ibi0k = new_tt(abi[:], abik, rm[:], rmk, ALU.mult)
            ibi, ibik = tmp(); ts(ibi[:], ibik, ibi0[:], ibi0k, -1.0, None, ALU.mult)
            one, onek = tmp(); S.op(V, lambda e: e.memset(one[:], 1.0), writes=[onek])
            zero, zerok = tmp(); S.op(V, lambda e: e.memset(zero[:], 0.0), writes=[zerok])
            P = [(one, onek, zero, zerok), (abr, abrk, abi, abik)]
            for k in range(2, 9):
                pr, prk, pi, pik = P[-1]
                P.append(cmul(pr[:], prk, pi[:], pik, abr[:], abrk, abi[:], abik, (128, G)))
            N = [(one, onek, zero, zerok), (ibr, ibrk, ibi, ibik)]
            for k in range(2, 8):
                pr, prk, pi, pik = N[-1]
                N.append(cmul(pr[:], prk, pi[:], pik, ibr[:], ibrk, ibi[:], ibik, (128, G)))

            def bc16(t):
                return t[:].unsqueeze(2).to_broadcast([128, G, 16])
            sh3 = (128, G, 16)
            Bbr, Bbrk, Bbi, Bbik = cmul(bc16(qre), qrek, bc16(qim), qimk, bre[:], "bre", bim[:], "bim", sh3)
            Yr = sb1("Yr", [128, G, 8, 16]); Yi = sb1("Yi", [128, G, 8, 16])
            Xr = sb1("Xr", [128, G, 8, 16]); nXi = sb1("nXi", [128, G, 8, 16])
            YK = []; XK = []

            def put(dst, nm, s, src, srck, neg=False):
                for lo, pos in ((0, s), (64, 7 - s)):
                    key = "%s_%d_%d" % (nm, lo, pos)
                    if neg:
                        S.op(V, lambda e, lo=lo, pos=pos: e.tensor_single_scalar(out=dst[lo:lo + 64, :, pos, :], in_=src[lo:lo + 64],
                                                                                  scalar=-1.0, op=ALU.mult), reads=[srck], writes=[key])
                    else:
                        S.op(V, lambda e, lo=lo, pos=pos: e.tensor_copy(out=dst[lo:lo + 64, :, pos, :], in_=src[lo:lo + 64]),
                             reads=[srck], writes=[key])
                    (YK if nm[0] == "Y" else XK).append(key)
            for s in range(8):
                nr_, nrk_, ni_, nik_ = N[s]
                r, rk = tmp(sh3, persist=False); i, ik = tmp(sh3, persist=False)
                cmul_into(r[:], rk, i[:], ik, bc16(nr_), nrk_, bc16(ni_), nik_, Bbr[:], Bbrk, Bbi[:], Bbik, sh3)
                put(Yr, "Yr", s, r, rk); put(Yi, "Yi", s, i, ik)
                pr_, prk_, pi_, pik_ = P[s]
                r, rk = tmp(sh3, persist=False); i, ik = tmp(sh3, persist=False)
                cmul_into(r[:], rk, i[:], ik, bc16(pr_), prk_, bc16(pi_), pik_, cre[:], "cre", cim[:], "cim", sh3)
                put(Xr, "Xr", s, r, rk); put(nXi, "nXi", s, i, ik, neg=True)
            sh4 = (128, G, 128)

            def bc128(t):
                return t[:].unsqueeze(2).to_broadcast([128, G, 128])
            f3 = lambda t: t[:].rearrange("p g s c -> p g (s c)")
            p7r, p7rk, p7i, p7ik = P[7]
            Wr = sb1("Wr", [128, G, 128]); Wi = sb1("Wi", [128, G, 128])
            tA = sb1("tA", [128, G, 128]); tB = sb1("tB", [128, G, 128])
            tt(tA[:], "tA", bc128(p7r), p7rk, f3(Yr), YK, ALU.mult)
            tt(tB[:], "tB", bc128(p7i), p7ik, f3(Yi), YK, ALU.mult)
            tt(Wr[:], "Wr", tA[:], "tA", tB[:], "tB", ALU.subtract)
            tt(tA[:], "tA", bc128(p7r), p7rk, f3(Yi), YK, ALU.mult)
            tt(tB[:], "tB", bc128(p7i), p7ik, f3(Yr), YK, ALU.mult)
            tt(Wi[:], "Wi", tA[:], "tA", tB[:], "tB", ALU.add)
            p1r, p1rk, p1i, p1ik = P[1]
            tt(tA[:], "tA", bc128(p1r), p1rk, f3(Xr), XK, ALU.mult)
            tt(tB[:], "tB", bc128(p1i), p1ik, f3(nXi), XK, ALU.mult)
            tt(WoR[:], "WoR", tA[:], "tA", tB[:], "tB", ALU.add)
            tt(tA[:], "tA", bc128(p1r), p1rk, f3(nXi), XK, ALU.mult)
            tt(tB[:], "tB", bc128(p1i), p1ik, f3(Xr), XK, ALU.mult)
            tt(WoI[:], "WoI", tA[:], "tA", tB[:], "tB", ALU.subtract)
            ptr = [ps1("ptr%d" % i, [128, 128]) for i in range(2)]
            pkf = ps1("pkf", [128, 128]); pkb = ps1("pkb", [128, 128])
            k1 = sb1("k1", [128, 128]); k2 = sb1("k2", [128, 128]); k3 = sb1("k3", [128, 128])
            ti = 0
            for g in range(G):
                for ri, (src, nm) in enumerate(((Wr, "Wr"), (Wi, "Wi"))):
                    pb = ptr[ti % 2]; pk = "ptr%d" % (ti % 2); ti += 1
                    S.op("tensor", lambda e, pb=pb, src=src, g=g: e.transpose(pb[:], src[:, g, :], ident[:]),
                         reads=[nm, "ident"], writes=[pk])
                    S.op("scalar", lambda e, pb=pb, g=g, ri=ri: e.copy(out=WstT[:, g, ri, :], in_=pb[:]), reads=[pk], writes=["WstT"])

                def mmk(e, g=g):
                    fl = lambda t, lo: t[lo:lo + 64, g].rearrange("p s c -> p (s c)")
                    e.matmul(pkf[:], lhsT=fl(Yr, 0), rhs=fl(Xr, 0), start=True, stop=False)
                    e.matmul(pkf[:], lhsT=fl(Yi, 0), rhs=fl(nXi, 0), start=False, stop=True)
                    e.matmul(pkb[:], lhsT=fl(Yr, 64), rhs=fl(Xr, 64), start=True, stop=False)
                    return e.matmul(pkb[:], lhsT=fl(Yi, 64), rhs=fl(nXi, 64), start=False, stop=True)
                S.op("tensor", mmk, reads=YK + XK, writes=["pkf", "pkb"])
                S.op(V, lambda e: e.tensor_tensor(out=k1[:], in0=pkf[:], in1=mf[:], op=ALU.mult), reads=["pkf", "mf"], writes=["k1"])
                S.op(V, lambda e: e.tensor_tensor(out=k2[:], in0=pkb[:], in1=mb[:], op=ALU.mult), reads=["pkb", "mb"], writes=["k2"])
                S.op(V, lambda e, g=g: e.tensor_tensor(out=k3[:], in0=ident[:], in1=dbc[:, g, :], op=ALU.mult), reads=["ident", "dbc"], writes=["k3"])
                S.op(V, lambda e: e.tensor_tensor(out=k1[:], in0=k1[:], in1=k2[:], op=ALU.add), reads=["k1", "k2"], writes=["k1"])
                S.op(V, lambda e, g=g: e.tensor_tensor(out=Kloc[:, g, :], in0=k1[:], in1=k3[:], op=ALU.add), reads=["k1", "k3"], writes=["Kloc"])
            p8r, p8rk, p8i, p8ik = P[8]
            S.op(V, lambda e: e.tensor_copy(out=AR2[:, 0, :], in_=p8r[:]), reads=[p8rk], writes=["AR2a"])
            S.op(V, lambda e: e.tensor_copy(out=AR2[:, 1, :], in_=p8r[:]), reads=[p8rk], writes=["AR2b"])
            S.op(V, lambda e: e.tensor_single_scalar(out=AI2[:, 0, :], in_=p8i[:], scalar=-1.0, op=ALU.mult), reads=[p8ik], writes=["AI2a"])
            S.op(V, lambda e: e.tensor_copy(out=AI2[:, 1, :], in_=p8i[:]), reads=[p8ik], writes=["AI2b"])
            Q = [None, P[8]]
            for m_ in range(2, 17):
                qr, qrk, qi, qik = Q[-1]
                Q.append(cmul(qr[:], qrk, qi[:], qik, p8r[:], p8rk, p8i[:], p8ik, (128, G)))
            for j_ in range(16):
                qr, qrk, qi, qik = Q[j_ + 1]
                S.op(V, lambda e, j_=j_, qr=qr: e.tensor_copy(out=TR[:, 0, j_, :], in_=qr[:]), reads=[qrk], writes=["TRI"])
                S.op(V, lambda e, j_=j_, qr=qr: e.tensor_copy(out=TR[:, 1, j_, :], in_=qr[:]), reads=[qrk], writes=["TRI"])
                S.op(V, lambda e, j_=j_, qi=qi: e.tensor_single_scalar(out=TI[:, 0, j_, :], in_=qi[:], scalar=-1.0, op=ALU.mult), reads=[qik], writes=["TRI"])
                S.op(V, lambda e, j_=j_, qi=qi: e.tensor_copy(out=TI[:, 1, j_, :], in_=qi[:]), reads=[qik], writes=["TRI"])
            qr, qrk, qi, qik = Q[16]
            S.op(V, lambda e, qr=qr: e.tensor_copy(out=AR128[:, 0, :], in_=qr[:]), reads=[qrk], writes=["A128"])
            S.op(V, lambda e, qr=qr: e.tensor_copy(out=AR128[:, 1, :], in_=qr[:]), reads=[qrk], writes=["A128"])
            S.op(V, lambda e, qi=qi: e.tensor_single_scalar(out=AI128[:, 0, :], in_=qi[:], scalar=-1.0, op=ALU.mult), reads=[qik], writes=["A128"])
            S.op(V, lambda e, qi=qi: e.tensor_copy(out=AI128[:, 1, :], in_=qi[:]), reads=[qik], writes=["A128"])
            S.barrier()
            S.flush(sst)
        AK = ["AR2a", "AR2b", "AI2a", "AI2b"]

        St = sb("St", [128, 2, NK, G])
        with contextlib.ExitStack() as s2:
            sb2, ps2 = mk(s2)
            hc = sb2("hc", [128, 2, L + 30], BF16)
            S.op("gpsimd", lambda e: e.memset(hc[:, :, 0:15], 0.0), writes=["hcp0"])
            S.op("gpsimd", lambda e: e.memset(hc[:, :, L + 15:L + 30], 0.0), writes=["hcp1"])
            CW = 512
            vt = [sb2("vt%d" % i, [128, CW]) for i in range(4)]
            gt = [sb2("gt%d" % i, [128, CW]) for i in range(4)]
            ci = 0
            HCK = []
            for blk in range(2):
                for q in range((L // CW) // 2 + 1 if own_only else L // CW):
                    vb = vt[ci % 4]; gb = gt[ci % 4]; vk = "vt%d" % (ci % 4); gk = "gt%d" % (ci % 4)
                    sl = slice(q * CW, (q + 1) * CW)
                    S.dma("sync", lambda e, vb=vb, blk=blk, sl=sl: e.dma_start(out=vb[:], in_=vT_d[:, blk, sl]), writes=[vk])
                    S.dma("sync", lambda e, gb=gb, blk=blk, sl=sl: e.dma_start(out=gb[:], in_=gT_d[:, blk, sl]), writes=[gk])
                    S.op("scalar", lambda e, gb=gb: e.activation(out=gb[:], in_=gb[:], func=AF.Sigmoid), reads=[gk], writes=[gk])
                    hk = "hc%d_%d" % (blk, q)
                    S.op(V, lambda e, vb=vb, gb=gb, blk=blk, q=q: e.tensor_tensor(out=hc[:, blk, 15 + q * CW:15 + (q + 1) * CW],
                                                                                 in0=vb[:], in1=gb[:], op=ALU.mult),
                         reads=[vk, gk], writes=[hk])
                    HCK.append(hk)
                    ci += 1
            dg = sb2("dg", [128, 2, 31, 128], BF16)
            for blk in range(2):
                S.op("gpsimd", lambda e, blk=blk: e.tensor_tensor(out=dg[:, blk], in0=ident[:].unsqueeze(1).to_broadcast([128, 31, 128]),
                                                                  in1=dww[:, blk, :].unsqueeze(2).to_broadcast([128, 31, 128]), op=ALU.mult),
                     reads=["ident", "dww"], writes=["dg%d" % blk])
            pst = [ps2("pst%d" % i, [128, NK]) for i in range(2)]
            si = 0
            STK = []
            for g in range(G):
                for ri in range(2):
                    pb = pst[si % 2]; pk = "pst%d" % (si % 2)
                    S.op("tensor", lambda e, pb=pb, g=g, ri=ri: e.matmul(pb[:], lhsT=WstT[:, g, ri, :], rhs=Ub[:, g, :], start=True, stop=True),
                         reads=["WstT"], writes=[pk])
                    sk = "St%d_%d" % (ri, g)
                    S.op("scalar", lambda e, pb=pb, g=g, ri=ri: e.copy(out=St[0:64, ri, :, g], in_=pb[0:64, :]), reads=[pk], writes=[sk])
                    S.op(V, lambda e, pb=pb, g=g, ri=ri: e.tensor_copy(out=St[64:128, ri, :, g], in_=rev_ap(pb[64:128, :], 1)), reads=[pk], writes=[sk])
                    STK.append(sk)
                    si += 1
            sc = {nm: (sb2("sc1" + nm, [128, 2, G]), sb2("sc2" + nm, [128, 2, G])) for nm in ("f", "b")}

            def scan(eng, nm, lo, order, npart=64):
                t1, t2 = sc[nm]
                hk = "H" + nm
                first = True
                for kprev, kcur in order:
                    rd = (STK if first else []) + [hk] + AK
                    first = False
                    prev = St[lo:lo + npart, :, kprev, :]
                    prev_sw = rev_ap(St[lo:lo + npart, :, kprev, :], 1)
                    cur = St[lo:lo + npart, :, kcur, :]
                    S.op(eng, lambda e, prev=prev: e.tensor_tensor(out=t1[lo:lo + npart], in0=prev, in1=AR2[lo:lo + npart], op=ALU.mult),
                         reads=rd, writes=["sc1" + nm])
                    S.op(eng, lambda e, prev_sw=prev_sw: e.tensor_tensor(out=t2[lo:lo + npart], in0=prev_sw, in1=AI2[lo:lo + npart], op=ALU.mult),
                         reads=rd, writes=["sc2" + nm])
                    S.op(eng, lambda e, cur=cur: e.tensor_tensor(out=cur, in0=cur, in1=t1[lo:lo + npart], op=ALU.add),
                         reads=["sc1" + nm] + rd, writes=[hk])
                    S.op(eng, lambda e, cur=cur: e.tensor_tensor(out=cur, in0=cur, in1=t2[lo:lo + npart], op=ALU.add),
                         reads=["sc2" + nm], writes=[hk])
            NB = NK // 16
            Stv = St[:].rearrange("p r (b j) g -> p r b j g", j=16)
            t1b = sb2("t1b", [128, 2, NB, G]); t2b = sb2("t2b", [128, 2, NB, G]); Cb = sb2("Cb", [128, 2, NB, G])
            bcb = lambda t: t.unsqueeze(2).to_broadcast([128, 2, NB, G])
            HK = "Hf"
            for j_ in range(1, 16):
                prev = Stv[:, :, :, j_ - 1, :]; prev_sw = rev_ap(prev, 1); cur = Stv[:, :, :, j_, :]
                rd = (STK if j_ == 1 else []) + [HK] + AK
                S.op(V, lambda e, prev=prev: e.tensor_tensor(out=t1b[:], in0=prev, in1=bcb(AR2[:]), op=ALU.mult), reads=rd, writes=["t1b"])
                S.op(V, lambda e, prev_sw=prev_sw: e.tensor_tensor(out=t2b[:], in0=prev_sw, in1=bcb(AI2[:]), op=ALU.mult), reads=rd, writes=["t2b"])
                S.op(V, lambda e, cur=cur: e.tensor_tensor(out=cur, in0=cur, in1=t1b[:], op=ALU.add), reads=["t1b"] + rd, writes=[HK])
                S.op(V, lambda e, cur=cur: e.tensor_tensor(out=cur, in0=cur, in1=t2b[:], op=ALU.add), reads=["t2b"], writes=[HK])
            c1, c2 = sc["f"]
            S.op(V, lambda e: e.memset(Cb[:, :, 0, :], 0.0), writes=["Cb"])
            for b_ in range(NB - 1):
                cprev = Cb[:, :, b_, :]; cprev_sw = rev_ap(cprev, 1); cnext = Cb[:, :, b_ + 1, :]
                S.op(V, lambda e, cprev=cprev: e.tensor_tensor(out=c1[:], in0=cprev, in1=AR128[:], op=ALU.mult), reads=["Cb", "A128"], writes=["c1"])
                S.op(V, lambda e, cprev_sw=cprev_sw: e.tensor_tensor(out=c2[:], in0=cprev_sw, in1=AI128[:], op=ALU.mult), reads=["Cb", "A128"], writes=["c2"])
                S.op(V, lambda e, cnext=cnext, b_=b_: e.tensor_tensor(out=cnext, in0=c1[:], in1=Stv[:, :, b_, 15, :], op=ALU.add), reads=["c1", HK], writes=["Cb"])
                S.op(V, lambda e, cnext=cnext: e.tensor_tensor(out=cnext, in0=cnext, in1=c2[:], op=ALU.add), reads=["c2", "Cb"], writes=["Cb"])
            Cb_sw = rev_ap(Cb[:], 1)
            for j_ in range(16):
                cur = Stv[:, :, :, j_, :]
                S.op(V, lambda e, j_=j_: e.tensor_tensor(out=t1b[:], in0=Cb[:], in1=bcb(TR[:, :, j_, :]), op=ALU.mult), reads=["Cb", "TRI"], writes=["t1b"])
                S.op(V, lambda e, j_=j_: e.tensor_tensor(out=t2b[:], in0=Cb_sw, in1=bcb(TI[:, :, j_, :]), op=ALU.mult), reads=["Cb", "TRI"], writes=["t2b"])
                S.op(V, lambda e, cur=cur: e.tensor_tensor(out=cur, in0=cur, in1=t1b[:], op=ALU.add), reads=["t1b", HK], writes=[HK])
                S.op(V, lambda e, cur=cur: e.tensor_tensor(out=cur, in0=cur, in1=t2b[:], op=ALU.add), reads=["t2b", HK], writes=[HK])
            pc = [ps2("pc%d" % i, [128, 512]) for i in range(2)]
            co = [sb2("co%d" % i, [128, 512]) for i in range(2)]
            ti = 0
            for blk in range(2):
                for t in range((L // 512) // 2 if own_only else L // 512):
                    pb = pc[ti % 2]; pk = "pc%d" % (ti % 2); ob = co[ti % 2]; ok = "co%d" % (ti % 2)

                    def mmc(e, pb=pb, blk=blk, t=t):
                        for k in range(31):
                            r = e.matmul(pb[:], lhsT=dg[:, blk, k, :], rhs=hc[:, blk, t * 512 + k:t * 512 + k + 512],
                                         start=(k == 0), stop=(k == 30))
                        return r
                    S.op("tensor", mmc, reads=["dg%d" % blk, "hcp0", "hcp1"] + HCK, writes=[pk])
                    S.op("scalar", lambda e, pb=pb, ob=ob, blk=blk: e.activation(out=ob[:], in_=pb[:], func=AF.Identity, bias=dwb[:, blk:blk + 1]),
                         reads=[pk, "dwb"], writes=[ok])
                    S.dma("sync", lambda e, ob=ob, blk=blk, t=t: e.dma_start(out=convT_d[:, blk, t * 512:(t + 1) * 512], in_=ob[:]),
                          reads=[ok], writes=["convT_d"])
                    ti += 1
            S.barrier()
            S.flush(sst)
        with contextlib.ExitStack() as s3:
            sb3, ps3 = mk(s3)
            Hin = sb3("Hin", [128, 2, G, NK], BF16)
            S.op("gpsimd", lambda e: e.memset(Hin[:], 0.0), writes=["Hin"])
            for ri in range(2):
                S.op(V, lambda e, ri=ri: e.tensor_copy(out=Hin[0:64, ri, :, 1:NK], in_=St[0:64, ri, 0:NK - 1, :].rearrange("p k g -> p g k")),
                     reads=["Hf"], writes=["Hin"])
                S.op(V, lambda e, ri=ri: e.tensor_copy(out=Hin[64:128, ri, :, 0:NK - 1],
                                                       in_=rev_ap(St[64:128, ri, 0:NK - 1, :].rearrange("p k g -> p g k"), 2)),
                     reads=["Hf"], writes=["Hin"])
            py = [ps3("py%d" % i, [128, 128]) for i in range(2)]
            Yo = [sb3("Yo%d" % i, [128, 8, G, 16]) for i in range(2)]
            yi = 0
            for kb in range(2 if own_only else 4):
                yo = Yo[kb % 2]; yok = "Yo%d" % (kb % 2)
                for g in range(G):
                    pb = py[yi % 2]; pk = "py%d" % (yi % 2)

                    def mmy(e, pb=pb, g=g, kb=kb):
                        ks = slice(kb * 128, (kb + 1) * 128)
                        e.matmul(pb[:], lhsT=Ub[:, g, ks], rhs=Kloc[:, g, :], start=True, stop=False)
                        e.matmul(pb[:], lhsT=Hin[:, 0, g, ks], rhs=WoR[:, g, :], start=False, stop=False)
                        return e.matmul(pb[:], lhsT=Hin[:, 1, g, ks], rhs=WoI[:, g, :], start=False, stop=True)
                    S.op("tensor", mmy, reads=["Kloc", "Hin", "WoR", "WoI"], writes=[pk])
                    if yi % 2 == 0:
                        S.op("scalar", lambda e, pb=pb, g=g, yo=yo: e.copy(out=yo[:, :, g, :], in_=pb[:].rearrange("p (t c) -> p t c", c=16)), reads=[pk], writes=[yok])
                    else:
                        S.op(V, lambda e, pb=pb, g=g, yo=yo: e.tensor_copy(out=yo[:, :, g, :], in_=pb[:].rearrange("p (t c) -> p t c", c=16)), reads=[pk], writes=[yok])
                    yi += 1
                S.dma("sync", lambda e, kb=kb, yo=yo: e.dma_start(out=ytok_d[kb * 128:(kb + 1) * 128], in_=yo[:].rearrange("p t g c -> p t (g c)")), reads=[yok], writes=["Y_d"])
            S.barrier()
            S.flush(sst)


def emit_C(nc, S, sst, d, uid, n_exp, moe, last, xT_out=False):
    ffn = True
    convT_d = d["convT"]; ytok_d = d["ytok"]; gcT_d = d["gcT"]; gsT_d = d["gsT"]; x_d = d["x_tok"]
    lng_d = d["lng"]; lnb_d = d["lnb"]; bglu_d = d["bglu"]
    wcp_d = d["wcp"]; wglu_d = d["wglu"]; wsp_d = d["wsp"]; wout_d = d["wout"]; nfg_d = d["nfg"]
    wg_d = d["wg"]; wu_d = d["wu"]; wd_d = d["wd"]
    if moe:
        rw_d = d["rw"]; rb_d = d["rb"]
    if last:
        fg_d = d["fg"]
    out_d = d["out"]
    V = "vector"; A = "scalar"; T = "tensor"
    with contextlib.ExitStack() as st:
        def mk(stack):
            return (lambda name, shape, dt=F32: stack.enter_context(nc.sbuf_tensor("sb%d_" % uid + name, shape, dt)),
                    lambda name, shape, dt=F32: stack.enter_context(nc.psum_tensor("ps%d_" % uid + name, shape, dt)))
        sb, ps = mk(st)
        xm = sb("xm", [128, 16, D])
        ident = sb("ident", [128, 128])
        onesm = sb("onesm", [128, 128])
        S.op("gpsimd", lambda e: e.memset(ident[:], 1.0), writes=["ident"])
        S.op("gpsimd", lambda e: e.affine_select(out=ident[:], in_=ident[:], pattern=[[-1, 128]], compare_op=ALU.is_equal,
                                                 fill=0.0, base=0, channel_multiplier=1), reads=["ident"], writes=["ident"])
        S.op("gpsimd", lambda e: e.memset(onesm[:], 1.0 / 512.0), writes=["onesm"])

        def ldsm(sbf, name, d, shape):
            t = sbf(name, shape)
            S.dma("sync", lambda e: e.dma_start(out=t[:], in_=d), writes=[name])
            return t
        nfg = ldsm(sb, "nfg", nfg_d, [128, D])
        if last:
            fg = ldsm(sb, "fg", fg_d, [128, D])
        if moe:
            rw = ldsm(sb, "rw", rw_d, [128, 8, 8]); rb = ldsm(sb, "rb", rb_d, [128, 8])
            gates = sb("gates", [128, 16, 8])

        with contextlib.ExitStack() as s1:
            sb1, ps1 = mk(s1)
            lng = ldsm(sb1, "lng", lng_d, [128, 4]); lnb = ldsm(sb1, "lnb", lnb_d, [128, 4]); bglu = ldsm(sb1, "bglu", bglu_d, [128, 4])
            wcp = sb1("wcp", [128, 4, D], BF16); wglu = sb1("wglu", [128, 4, 512], BF16)
            wsp = sb1("wsp", [128, 4, D], BF16); wout = sb1("wout", [128, 8, D], BF16)
            for t_, d_, nm in ((wcp, wcp_d, "wcp"), (wglu, wglu_d, "wglu"), (wsp, wsp_d, "wsp"), (wout, wout_d, "wout")):
                S.dma("gpsimd", lambda e, t_=t_, d_=d_: e.dma_start(out=t_[:], in_=d_.rearrange("(kb p) n -> p kb n", p=128)), writes=[nm])
            cv = sb1("cv", [128, 4, TT]); sq = sb1("sq", [128, 4, TT])
            yv = sb1("yv", [128, 4, TT]); ytk = sb1("ytk", [128, 4, TT]); ygb = sb1("ygb", [128, 4, TT], BF16)
            mean = sb1("mean", [128, TT]); var = sb1("var", [128, TT]); rstd = var
            ca = sb1("ca", [128, 4, TT], BF16)
            gl = sb1("gl", [128, 4, TT]); y2 = sb1("y2", [128, 4, TT], BF16)
            gcb = [sb1("gcb%d" % i, [128, TT]) for i in range(2)]
            gsb = [sb1("gsb%d" % i, [128, TT]) for i in range(2)]
            m1 = [sb1("m1_%d" % i, [128, TT]) for i in range(2)]
            m2 = [sb1("m2_%d" % i, [128, TT]) for i in range(2)]
            mg = sb1("mg", [128, 8, TT], BF16)
            xin = [sb1("xin%d" % i, [128, D]) for i in range(2)]
            pmean = ps1("pmean", [128, TT]); pex2 = ps1("pex2", [128, TT])
            pA = [ps1("pA%d" % i, [128, TT]) for i in range(2)]
            pB = [ps1("pB%d" % i, [128, TT]) for i in range(2)]
            pW = [ps1("pW%d" % i, [128, TT]) for i in range(2)]
            fi = 0
            xi = 0
            caL = [ca, sb1("ca_b", [128, 4, TT], BF16)]; y2L = [y2, sb1("y2_b", [128, 4, TT], BF16)]

            def front(t):
                nonlocal fi
                ca = caL[t % 2]; y2 = y2L[t % 2]; sfx = str(t % 2)
                ts_ = slice(t * TT, (t + 1) * TT)
                S.dma("sync", lambda e, ts_=ts_: e.dma_start(out=cv[:], in_=convT_d[:, :, ts_]), writes=["cv"])
                S.dma("sync", lambda e, t=t: e.dma_start(out=ytk[:], in_=ytok_d[t * TT:(t + 1) * TT, :].rearrange("(s p) c -> p s c", p=128)),
                      writes=["ytk"])
                S.op(A, lambda e: e.activation(out=sq[:], in_=cv[:], func=AF.Square), reads=["cv"], writes=["sq"])

                def mm_stats(e):
                    for kb in range(4):
                        e.matmul(pmean[:], lhsT=onesm[:], rhs=cv[:, kb, :], start=(kb == 0), stop=(kb == 3))
                    for kb in range(4):
                        r = e.matmul(pex2[:], lhsT=onesm[:], rhs=sq[:, kb, :], start=(kb == 0), stop=(kb == 3))
                    return r
                S.op(T, mm_stats, reads=["onesm", "cv", "sq"], writes=["pmean", "pex2"])
                S.op(V, lambda e: e.tensor_copy(out=mean[:], in_=pmean[:]), reads=["pmean"], writes=["mean"])
                S.op(V, lambda e: e.tensor_tensor(out=var[:], in0=mean[:], in1=mean[:], op=ALU.mult), reads=["mean"], writes=["var"])
                S.op(V, lambda e: e.tensor_tensor(out=var[:], in0=pex2[:], in1=var[:], op=ALU.subtract), reads=["pex2", "var"], writes=["var"])
                S.op(V, lambda e: e.tensor_scalar(out=var[:], in0=var[:], scalar1=LN_EPS, scalar2=None, op0=ALU.add), reads=["var"], writes=["var"])
                S.op(A, lambda e: e.sqrt(out=var[:], in_=var[:]), reads=["var"], writes=["var"])
                S.op(V, lambda e: e.reciprocal(out=rstd[:], in_=var[:]), reads=["var"], writes=["var"])
                for kb in range(4):
                    S.op(V, lambda e, kb=kb: e.tensor_tensor(out=cv[:, kb, :], in0=cv[:, kb, :], in1=mean[:], op=ALU.subtract),
                         reads=["cv", "mean"], writes=["cv"])
                    S.op(V, lambda e, kb=kb: e.tensor_tensor(out=cv[:, kb, :], in0=cv[:, kb, :], in1=rstd[:], op=ALU.mult),
                         reads=["cv", "var"], writes=["cv"])
                    S.op(A, lambda e, kb=kb: e.activation(out=ca[:, kb, :], in_=cv[:, kb, :], func=AF.Silu,
                                                         scale=lng[:, kb:kb + 1], bias=lnb[:, kb:kb + 1]),
                         reads=["cv", "lng", "lnb"], writes=["ca" + sfx])
                S.op(A, lambda e: e.activation(out=ytk[:], in_=ytk[:], func=AF.Gelu_apprx_tanh), reads=["ytk"], writes=["ytk"])
                for cb in range(4):
                    pbt = (pA + pB)[cb]; pbk = ("pA0", "pA1", "pB0", "pB1")[cb]

                    def try_(e, pbt=pbt, cb=cb):
                        for s_ in range(4):
                            r = e.transpose(pbt[:, s_ * 128:(s_ + 1) * 128], ytk[:, s_, cb * 128:(cb + 1) * 128], ident[:])
                        return r
                    S.op(T, try_, reads=["ytk", "ident"], writes=[pbk])
                    S.op(V, lambda e, pbt=pbt, cb=cb: e.tensor_copy(out=yv[:, cb, :], in_=pbt[:]), reads=[pbk], writes=["yv"])
                S.op(V, lambda e: e.tensor_copy(out=ygb[:], in_=yv[:]), reads=["yv"], writes=["ygb"])
                for fo in range(4):
                    pb = pA[fi % 2]; pk = "pA%d" % (fi % 2); fi += 1

                    def mm_glu(e, pb=pb, fo=fo):
                        for kb in range(4):
                            r = e.matmul(pb[:], lhsT=wglu[:, kb, fo * 128:(fo + 1) * 128], rhs=ygb[:, kb, :], start=(kb == 0), stop=(kb == 3))
                        return r
                    S.op(T, mm_glu, reads=["wglu", "ygb"], writes=[pk])
                    S.op(A, lambda e, pb=pb, fo=fo: e.activation(out=gl[:, fo, :], in_=pb[:], func=AF.Sigmoid, bias=bglu[:, fo:fo + 1]),
                         reads=[pk, "bglu"], writes=["gl"])
                    S.op(V, lambda e, fo=fo: e.tensor_tensor(out=y2[:, fo, :], in0=yv[:, fo, :], in1=gl[:, fo, :], op=ALU.mult),
                         reads=["yv", "gl"], writes=["y2" + sfx])

            def back(t):
                nonlocal fi, xi
                ca = caL[t % 2]; y2 = y2L[t % 2]; sfx = str(t % 2)
                ts_ = slice(t * TT, (t + 1) * TT)
                for fo in range(8):
                    pa = pA[fi % 2]; pak = "pA%d" % (fi % 2)
                    pb = pB[fi % 2]; pbk = "pB%d" % (fi % 2)
                    gc = gcb[fi % 2]; gck = "gcb%d" % (fi % 2)
                    gs = gsb[fi % 2]; gsk = "gsb%d" % (fi % 2)
                    ma = m1[fi % 2]; mak = "m1_%d" % (fi % 2)
                    mb_ = m2[fi % 2]; mbk = "m2_%d" % (fi % 2)
                    fi += 1
                    S.dma("sync", lambda e, gc=gc, fo=fo, ts_=ts_: e.dma_start(out=gc[:], in_=gcT_d[:, fo, ts_]), writes=[gck])
                    S.dma("sync", lambda e, gs=gs, fo=fo, ts_=ts_: e.dma_start(out=gs[:], in_=gsT_d[:, fo, ts_]), writes=[gsk])
                    S.op(A, lambda e, gc=gc: e.activation(out=gc[:], in_=gc[:], func=AF.Sigmoid), reads=[gck], writes=[gck])
                    S.op(A, lambda e, gs=gs: e.activation(out=gs[:], in_=gs[:], func=AF.Sigmoid), reads=[gsk], writes=[gsk])

                    def mm_cp(e, pa=pa, fo=fo):
                        for kb in range(4):
                            r = e.matmul(pa[:], lhsT=wcp[:, kb, fo * 128:(fo + 1) * 128], rhs=ca[:, kb, :], start=(kb == 0), stop=(kb == 3))
                        return r
                    S.op(T, mm_cp, reads=["wcp", "ca" + sfx], writes=[pak])

                    def mm_sp(e, pb=pb, fo=fo):
                        for kb in range(4):
                            r = e.matmul(pb[:], lhsT=wsp[:, kb, fo * 128:(fo + 1) * 128], rhs=y2[:, kb, :], start=(kb == 0), stop=(kb == 3))
                        return r
                    S.op(T, mm_sp, reads=["wsp", "y2" + sfx], writes=[pbk])
                    S.op(V, lambda e, pa=pa, gc=gc, ma=ma: e.tensor_tensor(out=ma[:], in0=pa[:], in1=gc[:], op=ALU.mult), reads=[pak, gck], writes=[mak])
                    S.op(V, lambda e, pb=pb, gs=gs, mb_=mb_: e.tensor_tensor(out=mb_[:], in0=pb[:], in1=gs[:], op=ALU.mult), reads=[pbk, gsk], writes=[mbk])
                    S.op(V, lambda e, ma=ma, mb_=mb_, fo=fo: e.tensor_tensor(out=mg[:, fo, :], in0=ma[:], in1=mb_[:], op=ALU.add),
                         reads=[mak, mbk], writes=["mg%d" % fo])
                for sub in range(TT // 128):
                    tt_ = t * (TT // 128) + sub
                    xb = xin[xi % 2]; xk = "xin%d" % (xi % 2); xi += 1
                    S.dma("sync", lambda e, xb=xb, tt_=tt_: e.dma_start(out=xb[:], in_=x_d[:, tt_, :]), writes=[xk])

                    def mm_wo(e, sub=sub):
                        for h in range(2):
                            for kb in range(8):
                                r = e.matmul(pW[h][:], lhsT=mg[:, kb, sub * 128:(sub + 1) * 128], rhs=wout[:, kb, h * 512:(h + 1) * 512],
                                             start=(kb == 0), stop=(kb == 7))
                        return r
                    S.op(T, mm_wo, reads=["wout"] + ["mg%d" % k for k in range(8)], writes=["pW0", "pW1"])
                    for h in range(2):
                        S.op(V, lambda e, h=h, xb=xb, tt_=tt_: e.tensor_tensor(out=xm[:, tt_, h * 512:(h + 1) * 512], in0=pW[h][:],
                                                                               in1=xb[:, h * 512:(h + 1) * 512], op=ALU.add),
                             reads=["pW%d" % h, xk], writes=["xm%d" % tt_])

            front(0)
            for t in range(NT // TT):
                if t + 1 < NT // TT:
                    front(t + 1)
                back(t)
            S.barrier()
            S.flush(sst)

        with contextlib.ExitStack() as s2:
            sb2, ps2 = mk(s2)
            HT = NT // 2
            NSUB = HT // 128
            h2T = sb2("h2T", [128, 8, HT], BF16)
            h2f = sb2("h2f", [128, D])
            ssq = sb2("ssq", [128, 1]); rs = sb2("rs", [128, 1])
            if ffn:
                act = sb2("act", [128, NFB, HT], BF16)
                wdb = sb2("wdb", [128, NFB, D], BF16)
            WC = 256
            NCH = DFF // WC
            wgb = [sb2("wgb%d" % i, [128, 8, WC], BF16) for i in range(2)]
            wub = [sb2("wub%d" % i, [128, 8, WC], BF16) for i in range(2)]
            sg = [sb2("sg%d" % i, [128, TT], BF16) for i in range(2)]
            ptr = ps2("ptr", [128, 8 * 128])
            pg = [ps2("pg%d" % i, [128, TT]) for i in range(2)]
            pu = [ps2("pu%d" % i, [128, TT]) for i in range(2)]
            pd = ps2("pd", [128, D])
            if not moe:
                ob = sb2("ob", [128, D]); ob_ap = ob[:]; obk = "ob"
            if moe:
                h2T32 = sb2("h2T32", [128, 8, 128])
                ob_ap = h2T32[:].rearrange("p k t -> p (k t)"); obk = "h2T32"
                lg = sb2("lg", [128, 8]); l2 = sb2("l2", [128, 8]); eq1 = sb2("eq1", [128, 8]); eq2 = sb2("eq2", [128, 8])
                mx1 = sb2("mx1", [128, 1]); mx2 = sb2("mx2", [128, 1]); dd = sb2("dd", [128, 1]); w1 = sb2("w1", [128, 1]); w2 = sb2("w2", [128, 1])
                g1 = sb2("g1", [128, 8])
            wi = 0
            gi = 0
            for th in range(2):
                for sub in range(NSUB):
                    tt_ = th * NSUB + sub
                    xk = "xm%d" % tt_
                    S.op(A, lambda e, tt_=tt_: e.activation(out=ob_ap, in_=xm[:, tt_, :], func=AF.Square, accum_out=ssq[:]),
                         reads=[xk], writes=[obk, "ssq"])
                    S.op(V, lambda e: e.tensor_scalar(out=rs[:], in0=ssq[:], scalar1=1.0 / D, scalar2=RMS_EPS, op0=ALU.mult, op1=ALU.add),
                         reads=["ssq"], writes=["rs"])
                    S.op(A, lambda e: e.sqrt(out=rs[:], in_=rs[:]), reads=["rs"], writes=["rs"])
                    S.op(V, lambda e: e.reciprocal(out=rs[:], in_=rs[:]), reads=["rs"], writes=["rs"])
                    S.op(V, lambda e, tt_=tt_: e.scalar_tensor_tensor(out=h2f[:], in0=xm[:, tt_, :], scalar=rs[:, 0:1], in1=nfg[:],
                                                                      op0=ALU.mult, op1=ALU.mult),
                         reads=[xk, "rs", "nfg"], writes=["h2f"])

                    def tr8(e):
                        for kb in range(8):
                            r = e.transpose(ptr[:, kb * 128:(kb + 1) * 128], h2f[:, kb * 128:(kb + 1) * 128], ident[:])
                        return r
                    S.op(T, tr8, reads=["h2f", "ident"], writes=["ptr"])
                    S.op(V, lambda e, sub=sub: e.tensor_copy(out=h2T[:, :, sub * 128:(sub + 1) * 128], in_=ptr[:].rearrange("p (k t) -> p k t", t=128)),
                         reads=["ptr"], writes=["h2T_%d" % sub])
                    if moe:
                        S.op(A, lambda e: e.copy(out=h2T32[:], in_=ptr[:].rearrange("p (k t) -> p k t", t=128)),
                             reads=["ptr", "h2T_%d" % sub], writes=["h2T32"])
                        if not ffn:
                            S.dma("sync", lambda e, tt_=tt_: e.dma_start(out=h2T_o[:, :, tt_ * 128:(tt_ + 1) * 128], in_=h2T32[:]),
                                  reads=["h2T32"], writes=["h2T_o"], is_output=last)

                        def mm_r(e):
                            for kb in range(8):
                                r = e.matmul(pg[0][:, 0:8], lhsT=h2T32[:, kb, :], rhs=rw[:, kb, :], start=(kb == 0), stop=(kb == 7))
                            return r
                        S.op(T, mm_r, reads=["h2T32", "rw"], writes=["pg0"])
                        S.op(V, lambda e: e.tensor_tensor(out=lg[:], in0=pg[0][:, 0:8], in1=rb[:], op=ALU.add), reads=["pg0", "rb"], writes=["lg"])
                        S.op(V, lambda e: e.reduce_max(out=mx1[:], in_=lg[:], axis=AX.X), reads=["lg"], writes=["mx1"])
                        S.op(V, lambda e: e.tensor_scalar(out=eq1[:], in0=lg[:], scalar1=mx1[:, 0:1], scalar2=None, op0=ALU.is_equal),
                             reads=["lg", "mx1"], writes=["eq1"])
                        S.op(V, lambda e: e.scalar_tensor_tensor(out=l2[:], in0=eq1[:], scalar=-1e30, in1=lg[:], op0=ALU.mult, op1=ALU.add),
                             reads=["eq1", "lg"], writes=["l2"])
                        S.op(V, lambda e: e.reduce_max(out=mx2[:], in_=l2[:], axis=AX.X), reads=["l2"], writes=["mx2"])
                        S.op(V, lambda e: e.tensor_scalar(out=eq2[:], in0=l2[:], scalar1=mx2[:, 0:1], scalar2=None, op0=ALU.is_equal),
                             reads=["l2", "mx2"], writes=["eq2"])
                        S.op(V, lambda e: e.tensor_tensor(out=dd[:], in0=mx2[:], in1=mx1[:], op=ALU.subtract), reads=["mx1", "mx2"], writes=["dd"])
                        S.op(A, lambda e: e.activation(out=w2[:], in_=dd[:], func=AF.Sigmoid), reads=["dd"], writes=["w2"])
                        S.op(V, lambda e: e.tensor_scalar(out=w1[:], in0=w2[:], scalar1=-1.0, scalar2=1.0, op0=ALU.mult, op1=ALU.add),
                             reads=["w2"], writes=["w1"])
                        S.op(V, lambda e: e.tensor_scalar(out=g1[:], in0=eq1[:], scalar1=w1[:, 0:1], scalar2=None, op0=ALU.mult),
                             reads=["eq1", "w1"], writes=["g1"])
                        S.op(V, lambda e, tt_=tt_: e.scalar_tensor_tensor(out=gates[:, tt_, :], in0=eq2[:], scalar=w2[:, 0:1], in1=g1[:],
                                                                          op0=ALU.mult, op1=ALU.add),
                             reads=["eq2", "w2", "g1"], writes=["gates%d" % tt_])
                H2K = ["h2T_%d" % s_ for s_ in range(NSUB)]
                for ex in range(n_exp):
                    for q in range(2):
                        S.dma("gpsimd", lambda e, ex=ex, q=q: e.dma_start(
                            out=wdb[:, q * 11:(q + 1) * 11, :],
                            in_=wd_d[ex].rearrange("(fb p) n -> p fb n", p=128)[:, q * 11:(q + 1) * 11, :]), writes=["wdb%d" % q])
                    for c in range(NCH):
                        wgt = wgb[wi % 2]; wgk = "wgb%d" % (wi % 2)
                        wut = wub[wi % 2]; wuk = "wub%d" % (wi % 2)
                        wi += 1
                        cs = slice(c * WC, (c + 1) * WC)
                        S.dma("gpsimd", lambda e, wgt=wgt, ex=ex, cs=cs: e.dma_start(
                            out=wgt[:], in_=wg_d[ex].rearrange("(kb p) n -> p kb n", p=128)[:, :, cs]), writes=[wgk])
                        S.dma("gpsimd", lambda e, wut=wut, ex=ex, cs=cs: e.dma_start(
                            out=wut[:], in_=wu_d[ex].rearrange("(kb p) n -> p kb n", p=128)[:, :, cs]), writes=[wuk])
                        for fl in range(WC // 128):
                            fb = c * (WC // 128) + fl
                            for tt2 in range(HT // TT):
                                pgb = pg[gi % 2]; pgk = "pg%d" % (gi % 2)
                                pub = pu[gi % 2]; puk = "pu%d" % (gi % 2)
                                sgb = sg[gi % 2]; sgk = "sg%d" % (gi % 2)
                                gi += 1
                                tsl = slice(tt2 * TT, (tt2 + 1) * TT)

                                def mm_g(e, pgb=pgb, wgt=wgt, fl=fl, tsl=tsl):
                                    for kb in range(8):
                                        r = e.matmul(pgb[:], lhsT=wgt[:, kb, fl * 128:(fl + 1) * 128], rhs=h2T[:, kb, tsl], start=(kb == 0), stop=(kb == 7))
                                    return r
                                S.op(T, mm_g, reads=[wgk] + H2K, writes=[pgk])

                                def mm_u(e, pub=pub, wut=wut, fl=fl, tsl=tsl):
                                    for kb in range(8):
                                        r = e.matmul(pub[:], lhsT=wut[:, kb, fl * 128:(fl + 1) * 128], rhs=h2T[:, kb, tsl], start=(kb == 0), stop=(kb == 7))
                                    return r
                                S.op(T, mm_u, reads=[wuk] + H2K, writes=[puk])
                                S.op(A, lambda e, pgb=pgb, sgb=sgb: e.activation(out=sgb[:], in_=pgb[:], func=AF.Silu), reads=[pgk], writes=[sgk])
                                S.op(V, lambda e, pub=pub, sgb=sgb, fb=fb, tsl=tsl: e.tensor_tensor(out=act[:, fb, tsl], in0=pub[:], in1=sgb[:], op=ALU.mult),
                                     reads=[puk, sgk], writes=["act%d" % fb])
                    ACTK = ["act%d" % fb for fb in range(NFB)]
                    for sub in range(NSUB):
                        tt_ = th * NSUB + sub

                        def mm_d(e, sub=sub):
                            for h in range(2):
                                for fb in range(NFB):
                                    r = e.matmul(pd[:, h * 512:(h + 1) * 512], lhsT=act[:, fb, sub * 128:(sub + 1) * 128],
                                                 rhs=wdb[:, fb, h * 512:(h + 1) * 512], start=(fb == 0), stop=(fb == NFB - 1))
                            return r
                        S.op(T, mm_d, reads=ACTK + ["wdb0", "wdb1"], writes=["pd"])
                        if moe:
                            S.op(V, lambda e, tt_=tt_, ex=ex: e.scalar_tensor_tensor(out=xm[:, tt_, :], in0=pd[:], scalar=gates[:, tt_, ex:ex + 1],
                                                                                     in1=xm[:, tt_, :], op0=ALU.mult, op1=ALU.add),
                                 reads=["pd", "gates%d" % tt_, "xm%d" % tt_], writes=["xm%d" % tt_])
                        else:
                            S.op(V, lambda e, tt_=tt_: e.tensor_tensor(out=xm[:, tt_, :], in0=pd[:], in1=xm[:, tt_, :], op=ALU.add),
                                 reads=["pd", "xm%d" % tt_], writes=["xm%d" % tt_])
                if moe and not ffn and th == 1:
                    S.dma("sync", lambda e: e.dma_start(out=gates_o, in_=gates[:]), reads=["gates%d" % k for k in range(16)],
                          writes=["gates_o"], is_output=last)
                for sub in range(NSUB):
                    tt_ = th * NSUB + sub
                    xk = "xm%d" % tt_
                    if last:
                        S.op(A, lambda e, tt_=tt_: e.activation(out=h2f[:], in_=xm[:, tt_, :], func=AF.Square, accum_out=ssq[:]),
                             reads=[xk], writes=["h2f", "ssq"])
                        S.op(V, lambda e: e.tensor_scalar(out=rs[:], in0=ssq[:], scalar1=1.0 / D, scalar2=RMS_EPS, op0=ALU.mult, op1=ALU.add),
                             reads=["ssq"], writes=["rs"])
                        S.op(A, lambda e: e.sqrt(out=rs[:], in_=rs[:]), reads=["rs"], writes=["rs"])
                        S.op(V, lambda e: e.reciprocal(out=rs[:], in_=rs[:]), reads=["rs"], writes=["rs"])
                        S.op(V, lambda e, tt_=tt_: e.scalar_tensor_tensor(out=ob_ap, in0=xm[:, tt_, :], scalar=rs[:, 0:1], in1=fg[:],
                                                                          op0=ALU.mult, op1=ALU.mult),
                             reads=[xk, "rs", "fg"], writes=[obk])
                        S.dma("sync", lambda e, tt_=tt_: e.dma_start(out=out_d[:, tt_, :], in_=ob_ap), reads=[obk], writes=["out_d"], is_output=last)
                    else:
                        S.dma("sync", lambda e, tt_=tt_: e.dma_start(out=out_d[:, tt_, :], in_=xm[:, tt_, :]), reads=[xk], writes=["out_d"], is_output=last)
                        if xT_out:
                            def trx(e, tt_=tt_):
                                for kb in range(8):
                                    r = e.transpose(ptr[:, kb * 128:(kb + 1) * 128], xm[:, tt_, kb * 128:(kb + 1) * 128], ident[:])
                                return r
                            S.op(T, trx, reads=[xk, "ident"], writes=["ptr"])
                            S.op(V, lambda e: e.tensor_copy(out=h2f[:].rearrange("p (k t) -> p k t", t=128), in_=ptr[:].rearrange("p (k t) -> p k t", t=128)),
                                 reads=["ptr"], writes=["h2f"])
                            S.dma("sync", lambda e, tt_=tt_: e.dma_start(out=d["xT_out"][:, :, tt_ * 128:(tt_ + 1) * 128],
                                                                         in_=h2f[:].rearrange("p (k t) -> p k t", t=128)),
                                  reads=["h2f"], writes=["xT_scr"])
            S.barrier()
            S.flush(sst)


def _prep_Bp(p, chalf):
    gs = slice(chalf * G, (chalf + 1) * G)
    cs = slice(chalf * 256, (chalf + 1) * 256)
    dp = lambda a: np.ascontiguousarray(a[:, gs, :].transpose(0, 2, 1).reshape(128, G))
    ldt = np.ascontiguousarray(np.broadcast_to(p["ssm_log_dt"][:, gs][:, None, :], (2, 64, G)).reshape(128, G))
    bb = lambda a: np.ascontiguousarray(np.broadcast_to(a[gs].transpose(1, 0, 2)[None], (2, 64, G, 16)).reshape(128, G, 16))
    cc = lambda a: np.ascontiguousarray(a[:, gs].transpose(0, 3, 1, 2).reshape(128, G, 16))
    dsk = p["ssm_d"][cs].reshape(G, 16)
    dbc = np.ascontiguousarray(np.broadcast_to(dsk[None, :, None, :], (128, G, 8, 16)).reshape(128, G, 128))
    return {
        "dww": np.ascontiguousarray(p["conv_dw_w"][:, cs].T.reshape(2, 128, 31).transpose(1, 0, 2)),
        "dwb": np.ascontiguousarray(p["conv_dw_b"][cs].reshape(2, 128).T),
        "a_re": dp(p["ssm_a_re"]), "a_im": dp(p["ssm_a_im"]), "log_dt": ldt,
        "b_re": bb(p["ssm_b_re"]), "b_im": bb(p["ssm_b_im"]),
        "c_re": cc(p["ssm_c_re"]), "c_im": cc(p["ssm_c_im"]), "dbc": dbc,
    }


_BP_SHAPES = {"dww": [128, 2, 31], "dwb": [128, 2], "a_re": [128, G], "a_im": [128, G], "log_dt": [128, G],
              "b_re": [128, G, 16], "b_im": [128, G, 16], "c_re": [128, G, 16], "c_im": [128, G, 16], "dbc": [128, G, 128]}
_CP_SHAPES = {"lng": [128, 4], "lnb": [128, 4], "bglu": [128, 4], "wcp": [512, D], "wglu": [512, 512], "wsp": [512, D],
              "wout": [D, D], "nfg": [128, D]}


def build_mega(debug=False):
    nc = bass.Bass("TRN2", target_bir_lowering=False)
    din = lambda name, shape: nc.dram_tensor(name, shape, F32, kind="ExternalInput").ap()
    skind = "ExternalOutput" if debug else "Internal"
    scr = lambda name, shape: nc.dram_tensor(name, shape, F32, kind=skind).ap()
    LL = 4096
    xT0 = din("xT0", [128, 8, LL]); x_tok0 = din("x_tok0", [128, 32, D])
    mask_f = din("mask_f", [128, 128]); mask_b = din("mask_b", [128, 128])
    gcol = [din("gcol%d" % l, [128, 8]) for l in range(2)]
    w_in = [din("w_in%d" % l, [D, 3584]) for l in range(2)]
    bp = [[{k: din("B%d%d_%s" % (l, h, k), shp) for k, shp in _BP_SHAPES.items()} for h in range(2)] for l in range(2)]
    cp = [{k: din("C%d_%s" % (l, k), shp) for k, shp in _CP_SHAPES.items()} for l in range(2)]
    ffw = [{"wg": din("wg0", [1, D, DFF]), "wu": din("wu0", [1, D, DFF]), "wd": din("wd0", [1, DFF, D])},
           {"wg": din("wg1", [8, D, DFF]), "wu": din("wu1", [8, D, DFF]), "wd": din("wd1", [8, DFF, D])}]
    rw = din("rw", [128, 8, 8]); rb = din("rb", [128, 8]); fg = din("fg", [128, D])
    out = nc.dram_tensor("out", [128, 16, D], F32, kind="ExternalOutput").ap()
    vT = scr("s_vT", [128, 4, LL]); gT = scr("s_gT", [128, 4, LL]); zu = scr("s_zu", [LL, 512])
    gcT = scr("s_gcT", [128, 8, LL]); gsT = scr("s_gsT", [128, 8, LL])
    convT = scr("s_convT", [128, 4, LL]); ytok = scr("s_ytok", [LL, 512])
    x_tok1 = scr("s_xtok1", [128, 32, D]); xT1 = scr("s_xT1", [128, 8, LL])
    S = Sched(nc)
    uid = [0]

    def nu():
        uid[0] += 1
        return uid[0]
    with contextlib.ExitStack() as sst:
        for l in range(2):
            xT = xT0 if l == 0 else xT1
            xtok = x_tok0 if l == 0 else x_tok1
            emit_A(nc, S, sst, {"xT": xT, "gcol": gcol[l], "w": w_in[l], "vT": vT, "gT": gT, "zu": zu, "gcT": gcT, "gsT": gsT},
                   nu(), 8, 8 if l == 0 else 4)
            for h in range(2):
                dB = dict(bp[l][h])
                dB.update({"zu": zu[:, h * 256:(h + 1) * 256], "vT": vT[:, h * 2:(h + 1) * 2, :], "gT": gT[:, h * 2:(h + 1) * 2, :],
                           "convT": convT[:, h * 2:(h + 1) * 2, :], "mask_f": mask_f, "mask_b": mask_b,
                           "ytok": ytok.rearrange("(k t) c -> k t c", t=8)[:, :, h * 256:(h + 1) * 256]})
                emit_B(nc, S, sst, dB, nu(), own_only=(l == 1))
            for hf in range(2 if l == 0 else 1):
                tk = slice(hf * NT, (hf + 1) * NT)
                dC = dict(cp[l]); dC.update(ffw[l])
                dC.update({"convT": convT[:, :, tk], "ytok": ytok[tk, :], "gcT": gcT[:, :, tk], "gsT": gsT[:, :, tk],
                           "x_tok": xtok[:, hf * 16:(hf + 1) * 16, :], "rw": rw, "rb": rb, "fg": fg})
                if l == 0:
                    dC["out"] = x_tok1[:, hf * 16:(hf + 1) * 16, :]
                    dC["xT_out"] = xT1[:, :, tk]
                    emit_C(nc, S, sst, dC, nu(), 1, False, False, xT_out=True)
                else:
                    dC["out"] = out
                    emit_C(nc, S, sst, dC, nu(), 8, True, True)
        S.finish()
        S.flush(sst)
    return nc


def prep_mega(core, inp):
    b, hf = core // 2, core % 2
    rv = (hf == 1)
    xs = inp["x"][b]
    if rv:
        xs = xs[::-1]
    s_idx = np.arange(128) // 16
    m = {
        "xT0": np.ascontiguousarray(xs.T.reshape(8, 128, 4096).transpose(1, 0, 2)),
        "x_tok0": np.ascontiguousarray(xs.reshape(32, 128, D).transpose(1, 0, 2)),
        "mask_f": (s_idx[None, :] >= s_idx[:, None]).astype(np.float32),
        "mask_b": (s_idx[None, :] <= s_idx[:, None]).astype(np.float32),
    }
    col = lambda v, nb: np.ascontiguousarray(v.reshape(nb, 128).T)
    rep = lambda v: np.ascontiguousarray(np.broadcast_to(v[None, :], (128, v.shape[0])))
    for l in range(2):
        m["gcol%d" % l] = col(inp["norm_mix_g"][l], 8)
        m["w_in%d" % l] = inp["w_in"][l]
        p = {k: inp[k][l] for k in ["conv_dw_w", "conv_dw_b", "ssm_a_re", "ssm_a_im", "ssm_log_dt", "ssm_b_re", "ssm_b_im",
                                     "ssm_c_re", "ssm_c_im", "ssm_d"]}
        if rv:
            p["conv_dw_w"] = p["conv_dw_w"][::-1]
            for k in ["ssm_a_re", "ssm_a_im", "ssm_log_dt", "ssm_c_re", "ssm_c_im"]:
                p[k] = p[k][::-1]
        for h in range(2):
            for k, v in _prep_Bp(p, h).items():
                m["B%d%d_%s" % (l, h, k)] = v
        m["C%d_lng" % l] = col(inp["conv_ln_g"][l], 4); m["C%d_lnb" % l] = col(inp["conv_ln_b"][l], 4)
        m["C%d_bglu" % l] = col(inp["ssm_b_glu"][l], 4)
        m["C%d_wcp" % l] = inp["w_conv_proj"][l]; m["C%d_wglu" % l] = inp["ssm_w_glu"][l]
        m["C%d_wsp" % l] = inp["w_ssm_proj"][l]; m["C%d_wout" % l] = inp["w_out"][l]
        m["C%d_nfg" % l] = rep(inp["norm_ffn_g"][l])
    m["wg0"] = inp["ffn_w_gate"]; m["wu0"] = inp["ffn_w_up"]; m["wd0"] = inp["ffn_w_down"]
    m["wg1"] = inp["moe_w_gate"][0]; m["wu1"] = inp["moe_w_up"][0]; m["wd1"] = inp["moe_w_down"][0]
    m["rw"] = np.ascontiguousarray(inp["router_w"][0].reshape(8, 128, 8).transpose(1, 0, 2))
    m["rb"] = rep(inp["router_b"][0]); m["fg"] = rep(inp["final_norm_g"])
    return m


_NC = {}


def kernel(**inputs):
    inp = {k: np.ascontiguousarray(np.asarray(v, dtype=np.float32)) for k, v in inputs.items()}
    if "mega" not in _NC:
        _NC["mega"] = build_mega()
    cores = list(range(8))
    res = run_bass_kernel_spmd(_NC["mega"], [prep_mega(c, inp) for c in cores], core_ids=cores)
    out = np.zeros((4, 4096, D), np.float32)
    for c in cores:
        b, hf = c // 2, c % 2
        o = res.results[c]["out"].transpose(1, 0, 2).reshape(NT, D)
        if hf == 0:
            out[b, 0:NT] = o
        else:
            out[b, NT:] = o[::-1]
    return out
```

```python
import contextlib
import math
import numpy as np
import concourse.bass as bass
import concourse.mybir as mybir
from concourse.bass_utils import run_bass_kernel_spmd

F32 = mybir.dt.float32
BF16 = mybir.dt.bfloat16
AF = mybir.ActivationFunctionType
ALU = mybir.AluOpType
AX = mybir.AxisListType

ENGS = ["tensor", "vector", "scalar", "gpsimd", "sync"]


def _flat(keys):
    out = []
    for k in keys:
        if isinstance(k, (list, tuple)):
            out.extend(_flat(k))
        else:
            out.append(k)
    return out


class Sched:
    def __init__(self, nc):
        self.nc = nc
        self.q = {e: [] for e in ENGS}
        self.cnt = {e: 0 for e in ENGS}
        self.last_w = {}
        self.readers = {}
        self.waited = {e: {} for e in ENGS}
        self.dma_cnt = {}
        self.semkeys = list(ENGS)
        self.out_tokens = []

    def _deps(self, reads, writes):
        deps = {}
        reads = _flat(reads)
        writes = _flat(writes)

        def add(tok):
            if tok is None:
                return
            k, v = tok
            if deps.get(k, 0) < v:
                deps[k] = v

        for b in reads:
            add(self.last_w.get(b))
        for b in writes:
            add(self.last_w.get(b))
            for t in self.readers.get(b, ()):
                add(t)
        return deps

    def _emit_waits(self, eng, deps):
        w = self.waited[eng]
        todo = []
        for k, v in deps.items():
            if w.get(k, 0) >= v:
                continue
            w[k] = v
            todo.append((k, v))
        return todo

    def _commit(self, tok, reads, writes):
        reads = _flat(reads)
        writes = _flat(writes)
        for b in reads:
            self.readers.setdefault(b, []).append(tok)
        for b in writes:
            self.last_w[b] = tok
            self.readers[b] = []

    def op(self, eng, fn, reads=(), writes=()):
        deps = self._deps(reads, writes)
        todo = self._emit_waits(eng, deps)
        self.cnt[eng] += 1
        n = self.cnt[eng]
        tok = (eng, n)
        self.waited[eng][eng] = max(self.waited[eng].get(eng, 0), 0)
        self.q[eng].append(("op", todo, fn, eng))
        self._commit(tok, reads, writes)
        return tok

    def dma(self, eng, fn, reads=(), writes=(), key=None, is_output=False):
        deps = self._deps(reads, writes)
        todo = self._emit_waits(eng, deps)
        if key is None:
            key = _flat(writes)[0]
        if not hasattr(self, "dma_slot"):
            self.dma_slot = {}
            self.slot_val = []
            self.slot_free = []
        if key not in self.dma_slot:
            if self.slot_free:
                slot = self.slot_free.pop()
            else:
                slot = len(self.slot_val)
                self.slot_val.append(0)
                self.semkeys.append(("slot", slot))
            self.dma_slot[key] = slot
        slot = self.dma_slot[key]
        self.slot_val[slot] += 16
        sk = ("slot", slot)
        tok = (sk, self.slot_val[slot])
        self.q[eng].append(("dma", todo, fn, sk))
        self._commit(tok, reads, writes)
        if is_output:
            self.out_tokens.append(tok)
        return tok

    def barrier(self):
        deps = {e: self.cnt[e] for e in ENGS if self.cnt[e] > 0}
        if hasattr(self, "dma_slot"):
            for i, v in enumerate(self.slot_val):
                if v > 0:
                    deps[("slot", i)] = v
        for e in ENGS:
            todo = self._emit_waits(e, dict(deps))
            self.q[e].append(("wait", todo, None, None))
        if hasattr(self, "dma_slot"):
            for k, slot in self.dma_slot.items():
                if slot not in self.slot_free:
                    self.slot_free.append(slot)
            self.dma_slot = {}

    def finish(self):
        deps = {}
        for k, v in self.out_tokens:
            deps[k] = max(deps.get(k, 0), v)
        todo = self._emit_waits("sync", deps)
        self.q["sync"].append(("wait", todo, None, None))

    def flush(self, stack):
        nc = self.nc
        if not hasattr(self, "sems"):
            self.sems = {}
        sems = self.sems
        for k in self.semkeys:
            if k not in sems:
                sems[k] = stack.enter_context(nc.semaphore("s%d" % len(sems)))
        q = self.q
        self.q = {e: [] for e in ENGS}
        with nc.Block() as block:
            def run(engname, e):
                for kind, todo, fn, key in q[engname]:
                    for k, v in todo:
                        e.wait_ge(sems[k], v)
                    if kind == "op":
                        fn(e).then_inc(sems[key], 1)
                    elif kind == "dma":
                        fn(e).then_inc(sems[key], 16)

            @block.tensor
            def _(e):
                run("tensor", e)

            @block.vector
            def _(e):
                run("vector", e)

            @block.scalar
            def _(e):
                run("scalar", e)

            @block.gpsimd
            def _(e):
                run("gpsimd", e)

            @block.sync
            def _(e):
                run("sync", e)

    def emit(self):
        self._st = contextlib.ExitStack()
        self.flush(self._st)
        self._st.close()
from concourse.ap import AP
NT = 2048
D = 1024
KB = 8
TT = 512
L = 4096
NK = L // 8
G = 16
TWO_PI = 2.0 * math.pi


def rev_ap(ap, dim):
    pat = [list(x) for x in ap.ap]
    step, cnt = pat[dim]
    off = ap.offset + step * (cnt - 1)
    pat[dim] = [-step, cnt]
    return AP(ap.tensor, off, pat)


DFF = 2816
NFB = DFF // 128
TT = 512
LN_EPS = 1e-5
RMS_EPS = 1e-6


def emit_A(nc, S, sst, d, uid, ntiles, gate_tiles, d_in=3584, eps=1e-6):
    nfo = d_in // 128
    xT = d["xT"]; gcol = d["gcol"]; w = d["w"]
    wv = w.rearrange("(kb p) n -> p kb n", p=128)
    with contextlib.ExitStack() as st:
        sb = lambda name, shape, dt: st.enter_context(nc.sbuf_tensor("sb%d_" % uid + name, shape, dt))
        ps = lambda name, shape, dt: st.enter_context(nc.psum_tensor("ps%d_" % uid + name, shape, dt))
        wsb = sb("wsb", [128, KB, d_in], BF16)
        g_sb = sb("g_sb", [128, KB], F32)
        ones = sb("ones", [128, 128], F32)
        xt = [sb("xt%d" % i, [128, KB, TT], F32) for i in range(2)]
        sq = sb("sq", [128, KB, TT], F32)
        rstd = sb("rstd", [128, TT], F32)
        hT = [sb("hT%d" % i, [128, KB, TT], BF16) for i in range(2)]
        zo = [sb("zo%d" % i, [128, TT], F32) for i in range(4)]
        zg = [sb("zg%d" % i, [128, TT], BF16) for i in range(4)]
        pss = ps("pss", [128, TT], F32)
        pz = [ps("pz%d" % i, [128, TT], F32) for i in range(4)]

        S.op("vector", lambda e: e.memset(ones[:], 1.0), writes=["ones"])
        S.dma("sync", lambda e: e.dma_start(out=g_sb[:], in_=gcol), writes=["g_sb"])
        WCH = 512
        nwch = d_in // WCH
        for c in range(nwch):
            S.dma("gpsimd", lambda e, c=c: e.dma_start(out=wsb[:, :, c * WCH:(c + 1) * WCH],
                                                       in_=wv[:, :, c * WCH:(c + 1) * WCH]),
                  writes=["w%d" % c])
        nt = ntiles
        oi = 0

        def prep(t):
            xb = xt[t % 2]
            hb = hT[t % 2]
            S.dma("sync", lambda e, xb=xb, t=t: e.dma_start(out=xb[:], in_=xT[:, :, t * TT:(t + 1) * TT]),
                  writes=["xt%d" % (t % 2)])
            S.op("scalar", lambda e, xb=xb: e.activation(out=sq[:], in_=xb[:], func=AF.Square),
                 reads=["xt%d" % (t % 2)], writes=["sq"])

            def mm_ss(e):
                for kb in range(KB):
                    r = e.matmul(pss[:], lhsT=ones[:], rhs=sq[:, kb, :], start=(kb == 0), stop=(kb == KB - 1))
                return r
            S.op("tensor", mm_ss, reads=["ones", "sq"], writes=["pss"])
            S.op("vector", lambda e: e.tensor_scalar(out=rstd[:], in0=pss[:], scalar1=1.0 / D, scalar2=eps,
                                                     op0=ALU.mult, op1=ALU.add),
                 reads=["pss"], writes=["rstd"])
            S.op("scalar", lambda e: e.sqrt(out=rstd[:], in_=rstd[:]), reads=["rstd"], writes=["rstd"])
            S.op("vector", lambda e: e.reciprocal(out=rstd[:], in_=rstd[:]), reads=["rstd"], writes=["rstd"])
            for kb in range(KB):
                S.op("vector", lambda e, kb=kb, xb=xb, hb=hb: e.scalar_tensor_tensor(
                    out=hb[:, kb, :], in0=xb[:, kb, :], scalar=g_sb[:, kb:kb + 1], in1=rstd[:],
                    op0=ALU.mult, op1=ALU.mult),
                    reads=["xt%d" % (t % 2), "rstd", "g_sb"], writes=["hT%d_%d" % (t % 2, kb)])

        def tile(t):
            nonlocal oi
            hb = hT[t % 2]
            def fo_block(fo, dst, gate=False):
                nonlocal oi
                if gate:
                    pb = pz[oi % 4]; pk = "pz%d" % (oi % 4); obg = zg[oi % 4]; okg = "zg%d" % (oi % 4)

                    def mmg(e, fo=fo, pb=pb, hb=hb):
                        for kb in range(KB):
                            r = e.matmul(pb[:], lhsT=wsb[:, kb, fo * 128:(fo + 1) * 128], rhs=hb[:, kb, :],
                                         start=(kb == 0), stop=(kb == KB - 1))
                        return r
                    S.op("tensor", mmg, reads=["w%d" % (fo * 128 // WCH)] + ["hT%d_%d" % (t % 2, kb) for kb in range(KB)], writes=[pk])
                    S.op("scalar", lambda e, pb=pb, obg=obg: e.activation(out=obg[:], in_=pb[:], func=AF.Sigmoid), reads=[pk], writes=[okg])
                    S.dma("sync", lambda e, obg=obg, dst=dst: e.dma_start(out=dst, in_=obg[:]), reads=[okg], writes=["zscr"])
                    oi += 1
                    return
                pb = pz[oi % 4]
                ob = zo[oi % 4]
                pk = "pz%d" % (oi % 4)
                ok = "zo%d" % (oi % 4)

                def mm(e, fo=fo, pb=pb, hb=hb):
                    for kb in range(KB):
                        r = e.matmul(pb[:], lhsT=wsb[:, kb, fo * 128:(fo + 1) * 128], rhs=hb[:, kb, :],
                                     start=(kb == 0), stop=(kb == KB - 1))
                    return r
                S.op("tensor", mm, reads=["w%d" % (fo * 128 // WCH)] + ["hT%d_%d" % (t % 2, kb) for kb in range(KB)],
                     writes=[pk])
                if oi % 2 == 0:
                    S.op("scalar", lambda e, pb=pb, ob=ob: e.copy(out=ob[:], in_=pb[:]), reads=[pk], writes=[ok])
                else:
                    S.op("vector", lambda e, pb=pb, ob=ob: e.tensor_copy(out=ob[:], in_=pb[:]), reads=[pk], writes=[ok])
                S.dma("sync", lambda e, ob=ob, dst=dst: e.dma_start(out=dst, in_=ob[:]), reads=[ok], writes=["zscr"])
                oi += 1
            tsl = slice(t * TT, (t + 1) * TT)
            for fo in range(4):
                fo_block(fo, d["vT"][:, fo, tsl])
            for fo in range(4, 8):
                fo_block(fo, d["gT"][:, fo - 4, tsl])
            for sub in range(TT // 128):
                pb = pz[oi % 4]; ob = zo[oi % 4]; pk = "pz%d" % (oi % 4); ok = "zo%d" % (oi % 4)

                def mmu(e, pb=pb, hb=hb, sub=sub):
                    for kb in range(KB):
                        r = e.matmul(pb[:], lhsT=hb[:, kb, sub * 128:(sub + 1) * 128], rhs=wsb[:, kb, 1024:1536],
                                     start=(kb == 0), stop=(kb == KB - 1))
                    return r
                S.op("tensor", mmu, reads=["w2"] + ["hT%d_%d" % (t % 2, kb) for kb in range(KB)], writes=[pk])
                if oi % 2 == 0:
                    S.op("scalar", lambda e, pb=pb, ob=ob: e.copy(out=ob[:], in_=pb[:]), reads=[pk], writes=[ok])
                else:
                    S.op("vector", lambda e, pb=pb, ob=ob: e.tensor_copy(out=ob[:], in_=pb[:]), reads=[pk], writes=[ok])
                r0 = t * TT + sub * 128
                S.dma("sync", lambda e, ob=ob, r0=r0: e.dma_start(out=d["zu"][r0:r0 + 128, :], in_=ob[:]), reads=[ok], writes=["zscr"])
                oi += 1
            if t + 1 < nt:
                prep(t + 1)
            if t < gate_tiles:
                for fo in range(12, 20):
                    fo_block(fo, d["gcT"][:, fo - 12, tsl], gate=True)
                for fo in range(20, 28):
                    fo_block(fo, d["gsT"][:, fo - 20, tsl], gate=True)
        prep(0)
        for t in range(nt):
            tile(t)
        S.barrier()
        S.flush(sst)


def emit_B(nc, S, sst, d, uid, own_only=False):
    zu_d = d["zu"]; vT_d = d["vT"]; gT_d = d["gT"]; dww_d = d["dww"]; dwb_d = d["dwb"]
    are_d = d["a_re"]; aim_d = d["a_im"]; ldt_d = d["log_dt"]; bre_d = d["b_re"]; bim_d = d["b_im"]
    cre_d = d["c_re"]; cim_d = d["c_im"]; dbc_d = d["dbc"]; mf_d = d["mask_f"]; mb_d = d["mask_b"]
    convT_d = d["convT"]; ytok_d = d["ytok"]
    V = "vector"
    with contextlib.ExitStack() as st:
        def mk(stack):
            return (lambda name, shape, dt=F32: stack.enter_context(nc.sbuf_tensor("sb%d_" % uid + name, shape, dt)),
                    lambda name, shape, dt=F32: stack.enter_context(nc.psum_tensor("ps%d_" % uid + name, shape, dt)))
        sb, ps = mk(st)
        ident = sb("ident", [128, 128])
        identb = sb("identb", [128, 128], BF16)
        dww = sb("dww", [128, 2, 31]); dwb = sb("dwb", [128, 2])
        WstT = sb("WstT", [128, G, 2, 128], BF16)
        Kloc = sb("Kloc", [128, G, 128], BF16)
        WoR = sb("WoR", [128, G, 128], BF16); WoI = sb("WoI", [128, G, 128], BF16)
        AR2 = sb("AR2", [128, 2, G]); AI2 = sb("AI2", [128, 2, G])
        Ub = sb("Ub", [128, G, NK], BF16)
        TR = sb("TR", [128, 2, 16, G]); TI = sb("TI", [128, 2, 16, G])
        AR128 = sb("AR128", [128, 2, G]); AI128 = sb("AI128", [128, 2, G])
        S.dma("sync", lambda e: e.dma_start(out=dww[:], in_=dww_d), writes=["dww"])
        S.dma("sync", lambda e: e.dma_start(out=dwb[:], in_=dwb_d), writes=["dwb"])
        S.op("gpsimd", lambda e: e.memset(ident[:], 1.0), writes=["ident"])
        S.op("gpsimd", lambda e: e.affine_select(out=ident[:], in_=ident[:], pattern=[[-1, 128]], compare_op=ALU.is_equal,
                                                 fill=0.0, base=0, channel_multiplier=1), reads=["ident"], writes=["ident"])
        S.op(V, lambda e: e.tensor_copy(out=identb[:], in_=ident[:]), reads=["ident"], writes=["identb"])

        with contextlib.ExitStack() as s1:
            sb1, ps1 = mk(s1)

            def ld(name, d, shape):
                t = sb1(name, shape)
                S.dma("sync", lambda e: e.dma_start(out=t[:], in_=d), writes=[name])
                return t
            are = ld("are", are_d, [128, G]); aim = ld("aim", aim_d, [128, G]); ldt = ld("ldt", ldt_d, [128, G])
            bre = ld("bre", bre_d, [128, G, 16]); bim = ld("bim", bim_d, [128, G, 16])
            cre = ld("cre", cre_d, [128, G, 16]); cim = ld("cim", cim_d, [128, G, 16])
            dbc = ld("dbc", dbc_d, [128, G, 128]); mf = ld("mf", mf_d, [128, 128]); mb = ld("mb", mb_d, [128, 128])
            cnt = [0]
            pools = {}

            def tmp(shape=(128, G), dt=F32, persist=True):
                shape = tuple(shape)
                if persist:
                    cnt[0] += 1
                    nm = "t%d" % cnt[0]
                    return sb1(nm, list(shape), dt), nm
                pl = pools.setdefault(shape, {"i": 0, "bufs": []})
                npool = 8
                if len(pl["bufs"]) < npool:
                    cnt[0] += 1
                    nm = "tp%d" % cnt[0]
                    pl["bufs"].append((sb1(nm, list(shape), dt), nm))
                r = pl["bufs"][pl["i"] % npool]
                pl["i"] += 1
                return r

            def tt(o, ok, a, ak, b, bk, op):
                S.op(V, lambda e: e.tensor_tensor(out=o, in0=a, in1=b, op=op), reads=[ak, bk], writes=[ok])

            def ts(o, ok, a, ak, s1_, s2_, op0, op1=None):
                if op1 is None:
                    S.op(V, lambda e: e.tensor_single_scalar(out=o, in_=a, scalar=s1_, op=op0), reads=[ak], writes=[ok])
                else:
                    S.op(V, lambda e: e.tensor_scalar(out=o, in0=a, scalar1=s1_, scalar2=s2_, op0=op0, op1=op1), reads=[ak], writes=[ok])

            def act(o, ok, a, ak, f):
                S.op("scalar", lambda e: e.activation(out=o, in_=a, func=f), reads=[ak], writes=[ok])

            def new_tt(a, ak, b, bk, op, shape=(128, G), persist=True):
                o, ok = tmp(shape, persist=persist)
                tt(o[:], ok, a, ak, b, bk, op)
                return o, ok

            def cmul_into(ore, orek, oim, oimk, ar_, ark, ai_, aik, br_, brk, bi_, bik, shape, sign=1.0):
                t1, k1 = new_tt(ar_, ark, br_, brk, ALU.mult, shape, False)
                t2, k2 = new_tt(ai_, aik, bi_, bik, ALU.mult, shape, False)
                tt(ore, orek, t1[:], k1, t2[:], k2, ALU.subtract)
                t3, k3 = new_tt(ar_, ark, bi_, bik, ALU.mult, shape, False)
                t4, k4 = new_tt(ai_, aik, br_, brk, ALU.mult, shape, False)
                tt(oim, oimk, t3[:], k3, t4[:], k4, ALU.add)

            def cmul(ar_, ark, ai_, aik, br_, brk, bi_, bik, shape):
                re, rk = tmp(shape); im, ik = tmp(shape)
                cmul_into(re[:], rk, im[:], ik, ar_, ark, ai_, aik, br_, brk, bi_, bik, shape)
                return re, rk, im, ik

            dt_, dtk = tmp(); act(dt_[:], dtk, ldt[:], "ldt", AF.Exp)
            adr, adrk = new_tt(are[:], "are", dt_[:], dtk, ALU.mult)
            adi, adik = new_tt(aim[:], "aim", dt_[:], dtk, ALU.mult)
            mag, magk = tmp(); act(mag[:], magk, adr[:], adrk, AF.Exp)

            def reduced(shift):
                r, rk = tmp(); ts(r[:], rk, adi[:], adik, 1.0 / TWO_PI, shift, ALU.mult, ALU.add)
                ni, nik = tmp((128, G), mybir.dt.int32)
                S.op(V, lambda e: e.tensor_copy(out=ni[:], in_=r[:]), reads=[rk], writes=[nik])
                nf, nfk = tmp()
                S.op(V, lambda e: e.tensor_copy(out=nf[:], in_=ni[:]), reads=[nik], writes=[nfk])
                fr, frk = new_tt(r[:], rk, nf[:], nfk, ALU.subtract)
                ng, ngk = tmp(); ts(ng[:], ngk, fr[:], frk, 0.0, None, ALU.is_lt)
                fr2, fr2k = new_tt(fr[:], frk, ng[:], ngk, ALU.add)
                th, thk = tmp(); ts(th[:], thk, fr2[:], fr2k, TWO_PI, -math.pi, ALU.mult, ALU.add)
                th2, th2k = tmp(); ts(th2[:], th2k, th[:], thk, 3.1415925, -3.1415925, ALU.min, ALU.max)
                o, ok = tmp(); act(o[:], ok, th2[:], th2k, AF.Sin)
                return o, ok
            sn, snk = reduced(0.5)
            cs, csk = reduced(0.75)
            abr, abrk = new_tt(mag[:], magk, cs[:], csk, ALU.mult)
            abi, abik = new_tt(mag[:], magk, sn[:], snk, ALU.mult)
            Zs = [sb1("Zs%d" % i, [128, 8, 256]) for i in range(2)]
            Zp = [sb1("Zp%d" % i, [128, 16, 8, 16]) for i in range(2)]
            pzt = [ps1("pzt%d" % i, [128, 4, 128]) for i in range(2)]
            zi = 0
            for kb4 in range(NK // 128):
                zs = Zs[kb4 % 2]; zk = "Zs%d" % (kb4 % 2)
                S.dma("sync", lambda e, zs=zs, kb4=kb4: e.dma_start(
                    out=zs[:], in_=zu_d[kb4 * 1024:(kb4 + 1) * 1024, :].rearrange("(k s) c -> k s c", s=8)), writes=[zk])
                zp = Zp[kb4 % 2]; zpk = "Zp%d" % (kb4 % 2)
                S.op("scalar", lambda e, zs=zs, zp=zp: e.copy(out=zp[:].rearrange("p g s c -> p s g c"),
                                                              in_=zs[:].rearrange("p s (g c) -> p s g c", c=16)),
                     reads=[zk], writes=[zpk])
                for g4 in range(4):
                    pb = pzt[zi % 2]; pk = "pzt%d" % (zi % 2); zi += 1

                    def trz(e, pb=pb, zp=zp, g4=g4):
                        for j in range(4):
                            g = g4 * 4 + j
                            r = e.transpose(pb[:, j, :], zp[:, g].rearrange("p s c -> p (s c)"), ident[:])
                        return r
                    S.op("tensor", trz, reads=[zpk, "ident"], writes=[pk])
                    S.op("scalar", (lambda e, pb=pb, g4=g4, kb4=kb4: e.copy(out=Ub[:, g4 * 4:(g4 + 1) * 4, kb4 * 128:(kb4 + 1) * 128], in_=pb[:])),
                         reads=[pk], writes=["Ub%d_%d" % (g4, kb4)])
            nr, nrk = tmp(); ts(nr[:], nrk, abr[:], abrk, -1.0, None, ALU.add)
            d1, d1k = new_tt(are[:], "are", are[:], "are", ALU.mult)
            d2, d2k = new_tt(aim[:], "aim", aim[:], "aim", ALU.mult)
            den, denk = new_tt(d1[:], d1k, d2[:], d2k, ALU.add)
            rden, rdenk = tmp(); S.op(V, lambda e: e.reciprocal(out=rden[:], in_=den[:]), reads=[denk], writes=[rdenk])
            u1, u1k = new_tt(nr[:], nrk, are[:], "are", ALU.mult)
            u2, u2k = new_tt(abi[:], abik, aim[:], "aim", ALU.mult)
            u3, u3k = new_tt(u1[:], u1k, u2[:], u2k, ALU.add)
            qre, qrek = new_tt(u3[:], u3k, rden[:], rdenk, ALU.mult)
            u4, u4k = new_tt(abi[:], abik, are[:], "are", ALU.mult)
            u5, u5k = new_tt(nr[:], nrk, aim[:], "aim", ALU.mult)
            u6, u6k = new_tt(u4[:], u4k, u5[:], u5k, ALU.subtract)
            qim, qimk = new_tt(u6[:], u6k, rden[:], rdenk, ALU.mult)
            m1, m1k = new_tt(abr[:], abrk, abr[:], abrk, ALU.mult)
            m2, m2k = new_tt(abi[:], abik, abi[:], abik, ALU.mult)
            m3, m3k = new_tt(m1[:], m1k, m2[:], m2k, ALU.add)
            rm, rmk = tmp(); S.op(V, lambda e: e.reciprocal(out=rm[:], in_=m3[:]), reads=[m3k], writes=[rmk])
            ibr, ibrk = new_tt(abr[:], abrk, rm[:], rmk, ALU.mult)
            ibi0, ibi0k = new_tt(abi[:], abik, rm[:], rmk, ALU.mult)
            ibi, ibik = tmp(); ts(ibi[:], ibik, ibi0[:], ibi0k, -1.0, None, ALU.mult)
            one, onek = tmp(); S.op(V, lambda e: e.memset(one[:], 1.0), writes=[onek])
            zero, zerok = tmp(); S.op(V, lambda e: e.memset(zero[:], 0.0), writes=[zerok])
            P = [(one, onek, zero, zerok), (abr, abrk, abi, abik)]
            for k in range(2, 9):
                pr, prk, pi, pik = P[-1]
                P.append(cmul(pr[:], prk, pi[:], pik, abr[:], abrk, abi[:], abik, (128, G)))
            N = [(one, onek, zero, zerok), (ibr, ibrk, ibi, ibik)]
            for k in range(2, 8):
                pr, prk, pi, pik = N[-1]
                N.append(cmul(pr[:], prk, pi[:], pik, ibr[:], ibrk, ibi[:], ibik, (128, G)))

            def bc16(t):
                return t[:].unsqueeze(2).to_broadcast([128, G, 16])
            sh3 = (128, G, 16)
            Bbr, Bbrk, Bbi, Bbik = cmul(bc16(qre), qrek, bc16(qim), qimk, bre[:], "bre", bim[:], "bim", sh3)
            Yr = sb1("Yr", [128, G, 8, 16]); Yi = sb1("Yi", [128, G, 8, 16])
            Xr = sb1("Xr", [128, G, 8, 16]); nXi = sb1("nXi", [128, G, 8, 16])
            YK = []; XK = []

            def put(dst, nm, s, src, srck, neg=False):
                for lo, pos in ((0, s), (64, 7 - s)):
                    key = "%s_%d_%d" % (nm, lo, pos)
                    if neg:
                        S.op(V, lambda e, lo=lo, pos=pos: e.tensor_single_scalar(out=dst[lo:lo + 64, :, pos, :], in_=src[lo:lo + 64],
                                                                                  scalar=-1.0, op=ALU.mult), reads=[srck], writes=[key])
                    else:
                        S.op(V, lambda e, lo=lo, pos=pos: e.tensor_copy(out=dst[lo:lo + 64, :, pos, :], in_=src[lo:lo + 64]),
                             reads=[srck], writes=[key])
                    (YK if nm[0] == "Y" else XK).append(key)
            for s in range(8):
                nr_, nrk_, ni_, nik_ = N[s]
                r, rk = tmp(sh3, persist=False); i, ik = tmp(sh3, persist=False)
                cmul_into(r[:], rk, i[:], ik, bc16(nr_), nrk_, bc16(ni_), nik_, Bbr[:], Bbrk, Bbi[:], Bbik, sh3)
                put(Yr, "Yr", s, r, rk); put(Yi, "Yi", s, i, ik)
                pr_, prk_, pi_, pik_ = P[s]
                r, rk = tmp(sh3, persist=False); i, ik = tmp(sh3, persist=False)
                cmul_into(r[:], rk, i[:], ik, bc16(pr_), prk_, bc16(pi_), pik_, cre[:], "cre", cim[:], "cim", sh3)
                put(Xr, "Xr", s, r, rk); put(nXi, "nXi", s, i, ik, neg=True)
            sh4 = (128, G, 128)

            def bc128(t):
                return t[:].unsqueeze(2).to_broadcast([128, G, 128])
            f3 = lambda t: t[:].rearrange("p g s c -> p g (s c)")
            p7r, p7rk, p7i, p7ik = P[7]
            Wr = sb1("Wr", [128, G, 128]); Wi = sb1("Wi", [128, G, 128])
            tA = sb1("tA", [128, G, 128]); tB = sb1("tB", [128, G, 128])
            tt(tA[:], "tA", bc128(p7r), p7rk, f3(Yr), YK, ALU.mult)
            tt(tB[:], "tB", bc128(p7i), p7ik, f3(Yi), YK, ALU.mult)
            tt(Wr[:], "Wr", tA[:], "tA", tB[:], "tB", ALU.subtract)
            tt(tA[:], "tA", bc128(p7r), p7rk, f3(Yi), YK, ALU.mult)
            tt(tB[:], "tB", bc128(p7i), p7ik, f3(Yr), YK, ALU.mult)
            tt(Wi[:], "Wi", tA[:], "tA", tB[:], "tB", ALU.add)
            p1r, p1rk, p1i, p1ik = P[1]
            tt(tA[:], "tA", bc128(p1r), p1rk, f3(Xr), XK, ALU.mult)
            tt(tB[:], "tB", bc128(p1i), p1ik, f3(nXi), XK, ALU.mult)
            tt(WoR[:], "WoR", tA[:], "tA", tB[:], "tB", ALU.add)
            tt(tA[:], "tA", bc128(p1r), p1rk, f3(nXi), XK, ALU.mult)
            tt(tB[:], "tB", bc128(p1i), p1ik, f3(Xr), XK, ALU.mult)
            tt(WoI[:], "WoI", tA[:], "tA", tB[:], "tB", ALU.subtract)
            ptr = [ps1("ptr%d" % i, [128, 128]) for i in range(2)]
            pkf = ps1("pkf", [128, 128]); pkb = ps1("pkb", [128, 128])
            k1 = sb1("k1", [128, 128]); k2 = sb1("k2", [128, 128]); k3 = sb1("k3", [128, 128])
            ti = 0
            for g in range(G):
                for ri, (src, nm) in enumerate(((Wr, "Wr"), (Wi, "Wi"))):
                    pb = ptr[ti % 2]; pk = "ptr%d" % (ti % 2); ti += 1
                    S.op("tensor", lambda e, pb=pb, src=src, g=g: e.transpose(pb[:], src[:, g, :], ident[:]),
                         reads=[nm, "ident"], writes=[pk])
                    S.op("scalar", lambda e, pb=pb, g=g, ri=ri: e.copy(out=WstT[:, g, ri, :], in_=pb[:]), reads=[pk], writes=["WstT"])

                def mmk(e, g=g):
                    fl = lambda t, lo: t[lo:lo + 64, g].rearrange("p s c -> p (s c)")
                    e.matmul(pkf[:], lhsT=fl(Yr, 0), rhs=fl(Xr, 0), start=True, stop=False)
                    e.matmul(pkf[:], lhsT=fl(Yi, 0), rhs=fl(nXi, 0), start=False, stop=True)
                    e.matmul(pkb[:], lhsT=fl(Yr, 64), rhs=fl(Xr, 64), start=True, stop=False)
                    return e.matmul(pkb[:], lhsT=fl(Yi, 64), rhs=fl(nXi, 64), start=False, stop=True)
                S.op("tensor", mmk, reads=YK + XK, writes=["pkf", "pkb"])
                S.op(V, lambda e: e.tensor_tensor(out=k1[:], in0=pkf[:], in1=mf[:], op=ALU.mult), reads=["pkf", "mf"], writes=["k1"])
                S.op(V, lambda e: e.tensor_tensor(out=k2[:], in0=pkb[:], in1=mb[:], op=ALU.mult), reads=["pkb", "mb"], writes=["k2"])
                S.op(V, lambda e, g=g: e.tensor_tensor(out=k3[:], in0=ident[:], in1=dbc[:, g, :], op=ALU.mult), reads=["ident", "dbc"], writes=["k3"])
                S.op(V, lambda e: e.tensor_tensor(out=k1[:], in0=k1[:], in1=k2[:], op=ALU.add), reads=["k1", "k2"], writes=["k1"])
                S.op(V, lambda e, g=g: e.tensor_tensor(out=Kloc[:, g, :], in0=k1[:], in1=k3[:], op=ALU.add), reads=["k1", "k3"], writes=["Kloc"])
            p8r, p8rk, p8i, p8ik = P[8]
            S.op(V, lambda e: e.tensor_copy(out=AR2[:, 0, :], in_=p8r[:]), reads=[p8rk], writes=["AR2a"])
            S.op(V, lambda e: e.tensor_copy(out=AR2[:, 1, :], in_=p8r[:]), reads=[p8rk], writes=["AR2b"])
            S.op(V, lambda e: e.tensor_single_scalar(out=AI2[:, 0, :], in_=p8i[:], scalar=-1.0, op=ALU.mult), reads=[p8ik], writes=["AI2a"])
            S.op(V, lambda e: e.tensor_copy(out=AI2[:, 1, :], in_=p8i[:]), reads=[p8ik], writes=["AI2b"])
            Q = [None, P[8]]
            for m_ in range(2, 17):
                qr, qrk, qi, qik = Q[-1]
                Q.append(cmul(qr[:], qrk, qi[:], qik, p8r[:], p8rk, p8i[:], p8ik, (128, G)))
            for j_ in range(16):
                qr, qrk, qi, qik = Q[j_ + 1]
                S.op(V, lambda e, j_=j_, qr=qr: e.tensor_copy(out=TR[:, 0, j_, :], in_=qr[:]), reads=[qrk], writes=["TRI"])
                S.op(V, lambda e, j_=j_, qr=qr: e.tensor_copy(out=TR[:, 1, j_, :], in_=qr[:]), reads=[qrk], writes=["TRI"])
                S.op(V, lambda e, j_=j_, qi=qi: e.tensor_single_scalar(out=TI[:, 0, j_, :], in_=qi[:], scalar=-1.0, op=ALU.mult), reads=[qik], writes=["TRI"])
                S.op(V, lambda e, j_=j_, qi=qi: e.tensor_copy(out=TI[:, 1, j_, :], in_=qi[:]), reads=[qik], writes=["TRI"])
            qr, qrk, qi, qik = Q[16]
            S.op(V, lambda e, qr=qr: e.tensor_copy(out=AR128[:, 0, :], in_=qr[:]), reads=[qrk], writes=["A128"])
            S.op(V, lambda e, qr=qr: e.tensor_copy(out=AR128[:, 1, :], in_=qr[:]), reads=[qrk], writes=["A128"])
            S.op(V, lambda e, qi=qi: e.tensor_single_scalar(out=AI128[:, 0, :], in_=qi[:], scalar=-1.0, op=ALU.mult), reads=[qik], writes=["A128"])
            S.op(V, lambda e, qi=qi: e.tensor_copy(out=AI128[:, 1, :], in_=qi[:]), reads=[qik], writes=["A128"])
            S.barrier()
            S.flush(sst)
        AK = ["AR2a", "AR2b", "AI2a", "AI2b"]

        St = sb("St", [128, 2, NK, G])
        with contextlib.ExitStack() as s2:
            sb2, ps2 = mk(s2)
            hc = sb2("hc", [128, 2, L + 30], BF16)
            S.op("gpsimd", lambda e: e.memset(hc[:, :, 0:15], 0.0), writes=["hcp0"])
            S.op("gpsimd", lambda e: e.memset(hc[:, :, L + 15:L + 30], 0.0), writes=["hcp1"])
            CW = 512
            vt = [sb2("vt%d" % i, [128, CW]) for i in range(4)]
            gt = [sb2("gt%d" % i, [128, CW]) for i in range(4)]
            ci = 0
            HCK = []
            for blk in range(2):
                for q in range((L // CW) // 2 + 1 if own_only else L // CW):
                    vb = vt[ci % 4]; gb = gt[ci % 4]; vk = "vt%d" % (ci % 4); gk = "gt%d" % (ci % 4)
                    sl = slice(q * CW, (q + 1) * CW)
                    S.dma("sync", lambda e, vb=vb, blk=blk, sl=sl: e.dma_start(out=vb[:], in_=vT_d[:, blk, sl]), writes=[vk])
                    S.dma("sync", lambda e, gb=gb, blk=blk, sl=sl: e.dma_start(out=gb[:], in_=gT_d[:, blk, sl]), writes=[gk])
                    S.op("scalar", lambda e, gb=gb: e.activation(out=gb[:], in_=gb[:], func=AF.Sigmoid), reads=[gk], writes=[gk])
                    hk = "hc%d_%d" % (blk, q)
                    S.op(V, lambda e, vb=vb, gb=gb, blk=blk, q=q: e.tensor_tensor(out=hc[:, blk, 15 + q * CW:15 + (q + 1) * CW],
                                                                                 in0=vb[:], in1=gb[:], op=ALU.mult),
                         reads=[vk, gk], writes=[hk])
                    HCK.append(hk)
                    ci += 1
            dg = sb2("dg", [128, 2, 31, 128], BF16)
            for blk in range(2):
                S.op("gpsimd", lambda e, blk=blk: e.tensor_tensor(out=dg[:, blk], in0=ident[:].unsqueeze(1).to_broadcast([128, 31, 128]),
                                                                  in1=dww[:, blk, :].unsqueeze(2).to_broadcast([128, 31, 128]), op=ALU.mult),
                     reads=["ident", "dww"], writes=["dg%d" % blk])
            pst = [ps2("pst%d" % i, [128, NK]) for i in range(2)]
            si = 0
            STK = []
            for g in range(G):
                for ri in range(2):
                    pb = pst[si % 2]; pk = "pst%d" % (si % 2)
                    S.op("tensor", lambda e, pb=pb, g=g, ri=ri: e.matmul(pb[:], lhsT=WstT[:, g, ri, :], rhs=Ub[:, g, :], start=True, stop=True),
                         reads=["WstT"], writes=[pk])
                    sk = "St%d_%d" % (ri, g)
                    S.op("scalar", lambda e, pb=pb, g=g, ri=ri: e.copy(out=St[0:64, ri, :, g], in_=pb[0:64, :]), reads=[pk], writes=[sk])
                    S.op(V, lambda e, pb=pb, g=g, ri=ri: e.tensor_copy(out=St[64:128, ri, :, g], in_=rev_ap(pb[64:128, :], 1)), reads=[pk], writes=[sk])
                    STK.append(sk)
                    si += 1
            sc = {nm: (sb2("sc1" + nm, [128, 2, G]), sb2("sc2" + nm, [128, 2, G])) for nm in ("f", "b")}

            def scan(eng, nm, lo, order, npart=64):
                t1, t2 = sc[nm]
                hk = "H" + nm
                first = True
                for kprev, kcur in order:
                    rd = (STK if first else []) + [hk] + AK
                    first = False
                    prev = St[lo:lo + npart, :, kprev, :]
                    prev_sw = rev_ap(St[lo:lo + npart, :, kprev, :], 1)
                    cur = St[lo:lo + npart, :, kcur, :]
                    S.op(eng, lambda e, prev=prev: e.tensor_tensor(out=t1[lo:lo + npart], in0=prev, in1=AR2[lo:lo + npart], op=ALU.mult),
                         reads=rd, writes=["sc1" + nm])
                    S.op(eng, lambda e, prev_sw=prev_sw: e.tensor_tensor(out=t2[lo:lo + npart], in0=prev_sw, in1=AI2[lo:lo + npart], op=ALU.mult),
                         reads=rd, writes=["sc2" + nm])
                    S.op(eng, lambda e, cur=cur: e.tensor_tensor(out=cur, in0=cur, in1=t1[lo:lo + npart], op=ALU.add),
                         reads=["sc1" + nm] + rd, writes=[hk])
                    S.op(eng, lambda e, cur=cur: e.tensor_tensor(out=cur, in0=cur, in1=t2[lo:lo + npart], op=ALU.add),
                         reads=["sc2" + nm], writes=[hk])
            NB = NK // 16
            Stv = St[:].rearrange("p r (b j) g -> p r b j g", j=16)
            t1b = sb2("t1b", [128, 2, NB, G]); t2b = sb2("t2b", [128, 2, NB, G]); Cb = sb2("Cb", [128, 2, NB, G])
            bcb = lambda t: t.unsqueeze(2).to_broadcast([128, 2, NB, G])
            HK = "Hf"
            for j_ in range(1, 16):
                prev = Stv[:, :, :, j_ - 1, :]; prev_sw = rev_ap(prev, 1); cur = Stv[:, :, :, j_, :]
                rd = (STK if j_ == 1 else []) + [HK] + AK
                S.op(V, lambda e, prev=prev: e.tensor_tensor(out=t1b[:], in0=prev, in1=bcb(AR2[:]), op=ALU.mult), reads=rd, writes=["t1b"])
                S.op(V, lambda e, prev_sw=prev_sw: e.tensor_tensor(out=t2b[:], in0=prev_sw, in1=bcb(AI2[:]), op=ALU.mult), reads=rd, writes=["t2b"])
                S.op(V, lambda e, cur=cur: e.tensor_tensor(out=cur, in0=cur, in1=t1b[:], op=ALU.add), reads=["t1b"] + rd, writes=[HK])
                S.op(V, lambda e, cur=cur: e.tensor_tensor(out=cur, in0=cur, in1=t2b[:], op=ALU.add), reads=["t2b"], writes=[HK])
            c1, c2 = sc["f"]
            S.op(V, lambda e: e.memset(Cb[:, :, 0, :], 0.0), writes=["Cb"])
            for b_ in range(NB - 1):
                cprev = Cb[:, :, b_, :]; cprev_sw = rev_ap(cprev, 1); cnext = Cb[:, :, b_ + 1, :]
                S.op(V, lambda e, cprev=cprev: e.tensor_tensor(out=c1[:], in0=cprev, in1=AR128[:], op=ALU.mult), reads=["Cb", "A128"], writes=["c1"])
                S.op(V, lambda e, cprev_sw=cprev_sw: e.tensor_tensor(out=c2[:], in0=cprev_sw, in1=AI128[:], op=ALU.mult), reads=["Cb", "A128"], writes=["c2"])
                S.op(V, lambda e, cnext=cnext, b_=b_: e.tensor_tensor(out=cnext, in0=c1[:], in1=Stv[:, :, b_, 15, :], op=ALU.add), reads=["c1", HK], writes=["Cb"])
                S.op(V, lambda e, cnext=cnext: e.tensor_tensor(out=cnext, in0=cnext, in1=c2[:], op=ALU.add), reads=["c2", "Cb"], writes=["Cb"])
            Cb_sw = rev_ap(Cb[:], 1)
            for j_ in range(16):
                cur = Stv[:, :, :, j_, :]
                S.op(V, lambda e, j_=j_: e.tensor_tensor(out=t1b[:], in0=Cb[:], in1=bcb(TR[:, :, j_, :]), op=ALU.mult), reads=["Cb", "TRI"], writes=["t1b"])
                S.op(V, lambda e, j_=j_: e.tensor_tensor(out=t2b[:], in0=Cb_sw, in1=bcb(TI[:, :, j_, :]), op=ALU.mult), reads=["Cb", "TRI"], writes=["t2b"])
                S.op(V, lambda e, cur=cur: e.tensor_tensor(out=cur, in0=cur, in1=t1b[:], op=ALU.add), reads=["t1b", HK], writes=[HK])
                S.op(V, lambda e, cur=cur: e.tensor_tensor(out=cur, in0=cur, in1=t2b[:], op=ALU.add), reads=["t2b", HK], writes=[HK])
            pc = [ps2("pc%d" % i, [128, 512]) for i in range(2)]
            co = [sb2("co%d" % i, [128, 512]) for i in range(2)]
            ti = 0
            for blk in range(2):
                for t in range((L // 512) // 2 if own_only else L // 512):
                    pb = pc[ti % 2]; pk = "pc%d" % (ti % 2); ob = co[ti % 2]; ok = "co%d" % (ti % 2)

                    def mmc(e, pb=pb, blk=blk, t=t):
                        for k in range(31):
                            r = e.matmul(pb[:], lhsT=dg[:, blk, k, :], rhs=hc[:, blk, t * 512 + k:t * 512 + k + 512],
                                         start=(k == 0), stop=(k == 30))
                        return r
                    S.op("tensor", mmc, reads=["dg%d" % blk, "hcp0", "hcp1"] + HCK, writes=[pk])
                    S.op("scalar", lambda e, pb=pb, ob=ob, blk=blk: e.activation(out=ob[:], in_=pb[:], func=AF.Identity, bias=dwb[:, blk:blk + 1]),
                         reads=[pk, "dwb"], writes=[ok])
                    S.dma("sync", lambda e, ob=ob, blk=blk, t=t: e.dma_start(out=convT_d[:, blk, t * 512:(t + 1) * 512], in_=ob[:]),
                          reads=[ok], writes=["convT_d"])
                    ti += 1
            S.barrier()
            S.flush(sst)
        with contextlib.ExitStack() as s3:
            sb3, ps3 = mk(s3)
            Hin = sb3("Hin", [128, 2, G, NK], BF16)
            S.op("gpsimd", lambda e: e.memset(Hin[:], 0.0), writes=["Hin"])
            for ri in range(2):
                S.op(V, lambda e, ri=ri: e.tensor_copy(out=Hin[0:64, ri, :, 1:NK], in_=St[0:64, ri, 0:NK - 1, :].rearrange("p k g -> p g k")),
                     reads=["Hf"], writes=["Hin"])
                S.op(V, lambda e, ri=ri: e.tensor_copy(out=Hin[64:128, ri, :, 0:NK - 1],
                                                       in_=rev_ap(St[64:128, ri, 0:NK - 1, :].rearrange("p k g -> p g k"), 2)),
                     reads=["Hf"], writes=["Hin"])
            py = [ps3("py%d" % i, [128, 128]) for i in range(2)]
            Yo = [sb3("Yo%d" % i, [128, 8, G, 16]) for i in range(2)]
            yi = 0
            for kb in range(2 if own_only else 4):
                yo = Yo[kb % 2]; yok = "Yo%d" % (kb % 2)
                for g in range(G):
                    pb = py[yi % 2]; pk = "py%d" % (yi % 2)

                    def mmy(e, pb=pb, g=g, kb=kb):
                        ks = slice(kb * 128, (kb + 1) * 128)
                        e.matmul(pb[:], lhsT=Ub[:, g, ks], rhs=Kloc[:, g, :], start=True, stop=False)
                        e.matmul(pb[:], lhsT=Hin[:, 0, g, ks], rhs=WoR[:, g, :], start=False, stop=False)
                        return e.matmul(pb[:], lhsT=Hin[:, 1, g, ks], rhs=WoI[:, g, :], start=False, stop=True)
                    S.op("tensor", mmy, reads=["Kloc", "Hin", "WoR", "WoI"], writes=[pk])
                    if yi % 2 == 0:
                        S.op("scalar", lambda e, pb=pb, g=g, yo=yo: e.copy(out=yo[:, :, g, :], in_=pb[:].rearrange("p (t c) -> p t c", c=16)), reads=[pk], writes=[yok])
                    else:
                        S.op(V, lambda e, pb=pb, g=g, yo=yo: e.tensor_copy(out=yo[:, :, g, :], in_=pb[:].rearrange("p (t c) -> p t c", c=16)), reads=[pk], writes=[yok])
                    yi += 1
                S.dma("sync", lambda e, kb=kb, yo=yo: e.dma_start(out=ytok_d[kb * 128:(kb + 1) * 128], in_=yo[:].rearrange("p t g c -> p t (g c)")), reads=[yok], writes=["Y_d"])
            S.barrier()
            S.flush(sst)


def emit_C(nc, S, sst, d, uid, n_exp, moe, last, xT_out=False):
    ffn = True
    convT_d = d["convT"]; ytok_d = d["ytok"]; gcT_d = d["gcT"]; gsT_d = d["gsT"]; x_d = d["x_tok"]
    lng_d = d["lng"]; lnb_d = d["lnb"]; bglu_d = d["bglu"]
    wcp_d = d["wcp"]; wglu_d = d["wglu"]; wsp_d = d["wsp"]; wout_d = d["wout"]; nfg_d = d["nfg"]
    wg_d = d["wg"]; wu_d = d["wu"]; wd_d = d["wd"]
    if moe:
        rw_d = d["rw"]; rb_d = d["rb"]
    if last:
        fg_d = d["fg"]
    out_d = d["out"]
    V = "vector"; A = "scalar"; T = "tensor"
    with contextlib.ExitStack() as st:
        def mk(stack):
            return (lambda name, shape, dt=F32: stack.enter_context(nc.sbuf_tensor("sb%d_" % uid + name, shape, dt)),
                    lambda name, shape, dt=F32: stack.enter_context(nc.psum_tensor("ps%d_" % uid + name, shape, dt)))
        sb, ps = mk(st)
        xm = sb("xm", [128, 16, D])
        ident = sb("ident", [128, 128])
        onesm = sb("onesm", [128, 128])
        S.op("gpsimd", lambda e: e.memset(ident[:], 1.0), writes=["ident"])
        S.op("gpsimd", lambda e: e.affine_select(out=ident[:], in_=ident[:], pattern=[[-1, 128]], compare_op=ALU.is_equal,
                                                 fill=0.0, base=0, channel_multiplier=1), reads=["ident"], writes=["ident"])
        S.op("gpsimd", lambda e: e.memset(onesm[:], 1.0 / 512.0), writes=["onesm"])

        def ldsm(sbf, name, d, shape):
            t = sbf(name, shape)
            S.dma("sync", lambda e: e.dma_start(out=t[:], in_=d), writes=[name])
            return t
        nfg = ldsm(sb, "nfg", nfg_d, [128, D])
        if last:
            fg = ldsm(sb, "fg", fg_d, [128, D])
        if moe:
            rw = ldsm(sb, "rw", rw_d, [128, 8, 8]); rb = ldsm(sb, "rb", rb_d, [128, 8])
            gates = sb("gates", [128, 16, 8])

        with contextlib.ExitStack() as s1:
            sb1, ps1 = mk(s1)
            lng = ldsm(sb1, "lng", lng_d, [128, 4]); lnb = ldsm(sb1, "lnb", lnb_d, [128, 4]); bglu = ldsm(sb1, "bglu", bglu_d, [128, 4])
            wcp = sb1("wcp", [128, 4, D], BF16); wglu = sb1("wglu", [128, 4, 512], BF16)
            wsp = sb1("wsp", [128, 4, D], BF16); wout = sb1("wout", [128, 8, D], BF16)
            for t_, d_, nm in ((wcp, wcp_d, "wcp"), (wglu, wglu_d, "wglu"), (wsp, wsp_d, "wsp"), (wout, wout_d, "wout")):
                S.dma("gpsimd", lambda e, t_=t_, d_=d_: e.dma_start(out=t_[:], in_=d_.rearrange("(kb p) n -> p kb n", p=128)), writes=[nm])
            cv = sb1("cv", [128, 4, TT]); sq = sb1("sq", [128, 4, TT])
            yv = sb1("yv", [128, 4, TT]); ytk = sb1("ytk", [128, 4, TT]); ygb = sb1("ygb", [128, 4, TT], BF16)
            mean = sb1("mean", [128, TT]); var = sb1("var", [128, TT]); rstd = var
            ca = sb1("ca", [128, 4, TT], BF16)
            gl = sb1("gl", [128, 4, TT]); y2 = sb1("y2", [128, 4, TT], BF16)
            gcb = [sb1("gcb%d" % i, [128, TT], BF16) for i in range(4)]
            gsb = [sb1("gsb%d" % i, [128, TT], BF16) for i in range(4)]
            m1 = [sb1("m1_%d" % i, [128, TT]) for i in range(2)]
            m2 = [sb1("m2_%d" % i, [128, TT]) for i in range(2)]
            mg = sb1("mg", [128, 8, TT], BF16)
            xin = [sb1("xin%d" % i, [128, D]) for i in range(2)]
            pmean = ps1("pmean", [128, TT]); pex2 = ps1("pex2", [128, TT])
            pA = [ps1("pA%d" % i, [128, TT]) for i in range(2)]
            pB = [ps1("pB%d" % i, [128, TT]) for i in range(2)]
            pW = [ps1("pW%d" % i, [128, TT]) for i in range(2)]
            fi = 0
            xi = 0
            caL = [ca, sb1("ca_b", [128, 4, TT], BF16)]; y2L = [y2, sb1("y2_b", [128, 4, TT], BF16)]

            def front(t):
                nonlocal fi
                ca = caL[t % 2]; y2 = y2L[t % 2]; sfx = str(t % 2)
                ts_ = slice(t * TT, (t + 1) * TT)
                S.dma("sync", lambda e, ts_=ts_: e.dma_start(out=cv[:], in_=convT_d[:, :, ts_]), writes=["cv"])
                S.dma("sync", lambda e, t=t: e.dma_start(out=ytk[:], in_=ytok_d[t * TT:(t + 1) * TT, :].rearrange("(s p) c -> p s c", p=128)),
                      writes=["ytk"])
                S.op(A, lambda e: e.activation(out=sq[:], in_=cv[:], func=AF.Square), reads=["cv"], writes=["sq"])

                def mm_stats(e):
                    for kb in range(4):
                        e.matmul(pmean[:], lhsT=onesm[:], rhs=cv[:, kb, :], start=(kb == 0), stop=(kb == 3))
                    for kb in range(4):
                        r = e.matmul(pex2[:], lhsT=onesm[:], rhs=sq[:, kb, :], start=(kb == 0), stop=(kb == 3))
                    return r
                S.op(T, mm_stats, reads=["onesm", "cv", "sq"], writes=["pmean", "pex2"])
                S.op(V, lambda e: e.tensor_copy(out=mean[:], in_=pmean[:]), reads=["pmean"], writes=["mean"])
                S.op(V, lambda e: e.tensor_tensor(out=var[:], in0=mean[:], in1=mean[:], op=ALU.mult), reads=["mean"], writes=["var"])
                S.op(V, lambda e: e.tensor_tensor(out=var[:], in0=pex2[:], in1=var[:], op=ALU.subtract), reads=["pex2", "var"], writes=["var"])
                S.op(V, lambda e: e.tensor_scalar(out=var[:], in0=var[:], scalar1=LN_EPS, scalar2=None, op0=ALU.add), reads=["var"], writes=["var"])
                S.op(A, lambda e: e.sqrt(out=var[:], in_=var[:]), reads=["var"], writes=["var"])
                S.op(V, lambda e: e.reciprocal(out=rstd[:], in_=var[:]), reads=["var"], writes=["var"])
                for kb in range(4):
                    S.op(V, lambda e, kb=kb: e.tensor_tensor(out=cv[:, kb, :], in0=cv[:, kb, :], in1=mean[:], op=ALU.subtract),
                         reads=["cv", "mean"], writes=["cv"])
                    S.op(V, lambda e, kb=kb: e.tensor_tensor(out=cv[:, kb, :], in0=cv[:, kb, :], in1=rstd[:], op=ALU.mult),
                         reads=["cv", "var"], writes=["cv"])
                    S.op(A, lambda e, kb=kb: e.activation(out=ca[:, kb, :], in_=cv[:, kb, :], func=AF.Silu,
                                                         scale=lng[:, kb:kb + 1], bias=lnb[:, kb:kb + 1]),
                         reads=["cv", "lng", "lnb"], writes=["ca" + sfx])
                S.op(A, lambda e: e.activation(out=ytk[:], in_=ytk[:], func=AF.Gelu_apprx_tanh), reads=["ytk"], writes=["ytk"])
                for cb in range(4):
                    pbt = (pA + pB)[cb]; pbk = ("pA0", "pA1", "pB0", "pB1")[cb]

                    def try_(e, pbt=pbt, cb=cb):
                        for s_ in range(4):
                            r = e.transpose(pbt[:, s_ * 128:(s_ + 1) * 128], ytk[:, s_, cb * 128:(cb + 1) * 128], ident[:])
                        return r
                    S.op(T, try_, reads=["ytk", "ident"], writes=[pbk])
                    S.op(V, lambda e, pbt=pbt, cb=cb: e.tensor_copy(out=yv[:, cb, :], in_=pbt[:]), reads=[pbk], writes=["yv"])
                S.op(V, lambda e: e.tensor_copy(out=ygb[:], in_=yv[:]), reads=["yv"], writes=["ygb"])
                for fo in range(4):
                    pb = pA[fi % 2]; pk = "pA%d" % (fi % 2); fi += 1

                    def mm_glu(e, pb=pb, fo=fo):
                        for kb in range(4):
                            r = e.matmul(pb[:], lhsT=wglu[:, kb, fo * 128:(fo + 1) * 128], rhs=ygb[:, kb, :], start=(kb == 0), stop=(kb == 3))
                        return r
                    S.op(T, mm_glu, reads=["wglu", "ygb"], writes=[pk])
                    S.op(A, lambda e, pb=pb, fo=fo: e.activation(out=gl[:, fo, :], in_=pb[:], func=AF.Sigmoid, bias=bglu[:, fo:fo + 1]),
                         reads=[pk, "bglu"], writes=["gl"])
                    S.op(V, lambda e, fo=fo: e.tensor_tensor(out=y2[:, fo, :], in0=yv[:, fo, :], in1=gl[:, fo, :], op=ALU.mult),
                         reads=["yv", "gl"], writes=["y2" + sfx])

            def back(t):
                nonlocal fi, xi
                ca = caL[t % 2]; y2 = y2L[t % 2]; sfx = str(t % 2)
                ts_ = slice(t * TT, (t + 1) * TT)
                for fo in range(8):
                    pa = pA[fi % 2]; pak = "pA%d" % (fi % 2)
                    pb = pB[fi % 2]; pbk = "pB%d" % (fi % 2)
                    gc = gcb[fi % 4]; gck = "gcb%d" % (fi % 4)
                    gs = gsb[fi % 4]; gsk = "gsb%d" % (fi % 4)
                    ma = m1[fi % 2]; mak = "m1_%d" % (fi % 2)
                    mb_ = m2[fi % 2]; mbk = "m2_%d" % (fi % 2)
                    fi += 1
                    S.dma("sync", lambda e, gc=gc, fo=fo, ts_=ts_: e.dma_start(out=gc[:], in_=gcT_d[:, fo, ts_]), writes=[gck])
                    S.dma("sync", lambda e, gs=gs, fo=fo, ts_=ts_: e.dma_start(out=gs[:], in_=gsT_d[:, fo, ts_]), writes=[gsk])

                    def mm_cp(e, pa=pa, fo=fo):
                        for kb in range(4):
                            r = e.matmul(pa[:], lhsT=wcp[:, kb, fo * 128:(fo + 1) * 128], rhs=ca[:, kb, :], start=(kb == 0), stop=(kb == 3))
                        return r
                    S.op(T, mm_cp, reads=["wcp", "ca" + sfx], writes=[pak])

                    def mm_sp(e, pb=pb, fo=fo):
                        for kb in range(4):
                            r = e.matmul(pb[:], lhsT=wsp[:, kb, fo * 128:(fo + 1) * 128], rhs=y2[:, kb, :], start=(kb == 0), stop=(kb == 3))
                        return r
                    S.op(T, mm_sp, reads=["wsp", "y2" + sfx], writes=[pbk])
                    S.op(V, lambda e, pa=pa, gc=gc, ma=ma: e.tensor_tensor(out=ma[:], in0=pa[:], in1=gc[:], op=ALU.mult), reads=[pak, gck], writes=[mak])
                    S.op(V, lambda e, pb=pb, gs=gs, mb_=mb_: e.tensor_tensor(out=mb_[:], in0=pb[:], in1=gs[:], op=ALU.mult), reads=[pbk, gsk], writes=[mbk])
                    S.op(V, lambda e, ma=ma, mb_=mb_, fo=fo: e.tensor_tensor(out=mg[:, fo, :], in0=ma[:], in1=mb_[:], op=ALU.add),
                         reads=[mak, mbk], writes=["mg%d" % fo])
                for sub in range(TT // 128):
                    tt_ = t * (TT // 128) + sub
                    xb = xin[xi % 2]; xk = "xin%d" % (xi % 2); xi += 1
                    S.dma("sync", lambda e, xb=xb, tt_=tt_: e.dma_start(out=xb[:], in_=x_d[:, tt_, :]), writes=[xk])

                    def mm_wo(e, sub=sub):
                        for h in range(2):
                            for kb in range(8):
                                r = e.matmul(pW[h][:], lhsT=mg[:, kb, sub * 128:(sub + 1) * 128], rhs=wout[:, kb, h * 512:(h + 1) * 512],
                                             start=(kb == 0), stop=(kb == 7))
                        return r
                    S.op(T, mm_wo, reads=["wout"] + ["mg%d" % k for k in range(8)], writes=["pW0", "pW1"])
                    for h in range(2):
                        S.op(V, lambda e, h=h, xb=xb, tt_=tt_: e.tensor_tensor(out=xm[:, tt_, h * 512:(h + 1) * 512], in0=pW[h][:],
                                                                               in1=xb[:, h * 512:(h + 1) * 512], op=ALU.add),
                             reads=["pW%d" % h, xk], writes=["xm%d" % tt_])

            front(0)
            for t in range(NT // TT):
                if t + 1 < NT // TT:
                    front(t + 1)
                back(t)
            S.barrier()
            S.flush(sst)

        with contextlib.ExitStack() as s2:
            sb2, ps2 = mk(s2)
            HT = NT // 2
            NSUB = HT // 128
            h2T = sb2("h2T", [128, 8, HT], BF16)
            h2f = sb2("h2f", [128, D])
            ssq = sb2("ssq", [128, 1]); rs = sb2("rs", [128, 1])
            if ffn:
                act = sb2("act", [128, NFB, HT], BF16)
                wdb = sb2("wdb", [128, NFB, D], BF16)
            WC = 256
            NCH = DFF // WC
            wgb = [sb2("wgb%d" % i, [128, 8, WC], BF16) for i in range(2)]
            wub = [sb2("wub%d" % i, [128, 8, WC], BF16) for i in range(2)]
            sg = [sb2("sg%d" % i, [128, TT], BF16) for i in range(2)]
            ptr = ps2("ptr", [128, 8 * 128])
            pg = [ps2("pg%d" % i, [128, TT]) for i in range(2)]
            pu = [ps2("pu%d" % i, [128, TT]) for i in range(2)]
            pd = ps2("pd", [128, D])
            if not moe:
                ob = sb2("ob", [128, D]); ob_ap = ob[:]; obk = "ob"
            if moe:
                h2T32 = sb2("h2T32", [128, 8, 128])
                ob_ap = h2T32[:].rearrange("p k t -> p (k t)"); obk = "h2T32"
                lg = sb2("lg", [128, 8]); l2 = sb2("l2", [128, 8]); eq1 = sb2("eq1", [128, 8]); eq2 = sb2("eq2", [128, 8])
                mx1 = sb2("mx1", [128, 1]); mx2 = sb2("mx2", [128, 1]); dd = sb2("dd", [128, 1]); w1 = sb2("w1", [128, 1]); w2 = sb2("w2", [128, 1])
                g1 = sb2("g1", [128, 8])
            wi = 0
            gi = 0
            for th in range(2):
                for sub in range(NSUB):
                    tt_ = th * NSUB + sub
                    xk = "xm%d" % tt_
                    S.op(A, lambda e, tt_=tt_: e.activation(out=ob_ap, in_=xm[:, tt_, :], func=AF.Square, accum_out=ssq[:]),
                         reads=[xk], writes=[obk, "ssq"])
                    S.op(V, lambda e: e.tensor_scalar(out=rs[:], in0=ssq[:], scalar1=1.0 / D, scalar2=RMS_EPS, op0=ALU.mult, op1=ALU.add),
                         reads=["ssq"], writes=["rs"])
                    S.op(A, lambda e: e.sqrt(out=rs[:], in_=rs[:]), reads=["rs"], writes=["rs"])
                    S.op(V, lambda e: e.reciprocal(out=rs[:], in_=rs[:]), reads=["rs"], writes=["rs"])
                    S.op(V, lambda e, tt_=tt_: e.scalar_tensor_tensor(out=h2f[:], in0=xm[:, tt_, :], scalar=rs[:, 0:1], in1=nfg[:],
                                                                      op0=ALU.mult, op1=ALU.mult),
                         reads=[xk, "rs", "nfg"], writes=["h2f"])

                    def tr8(e):
                        for kb in range(8):
                            r = e.transpose(ptr[:, kb * 128:(kb + 1) * 128], h2f[:, kb * 128:(kb + 1) * 128], ident[:])
                        return r
                    S.op(T, tr8, reads=["h2f", "ident"], writes=["ptr"])
                    S.op(V, lambda e, sub=sub: e.tensor_copy(out=h2T[:, :, sub * 128:(sub + 1) * 128], in_=ptr[:].rearrange("p (k t) -> p k t", t=128)),
                         reads=["ptr"], writes=["h2T_%d" % sub])
                    if moe:
                        S.op(A, lambda e: e.copy(out=h2T32[:], in_=ptr[:].rearrange("p (k t) -> p k t", t=128)),
                             reads=["ptr", "h2T_%d" % sub], writes=["h2T32"])
                        if not ffn:
                            S.dma("sync", lambda e, tt_=tt_: e.dma_start(out=h2T_o[:, :, tt_ * 128:(tt_ + 1) * 128], in_=h2T32[:]),
                                  reads=["h2T32"], writes=["h2T_o"], is_output=last)

                        def mm_r(e):
                            for kb in range(8):
                                r = e.matmul(pg[0][:, 0:8], lhsT=h2T32[:, kb, :], rhs=rw[:, kb, :], start=(kb == 0), stop=(kb == 7))
                            return r
                        S.op(T, mm_r, reads=["h2T32", "rw"], writes=["pg0"])
                        S.op(V, lambda e: e.tensor_tensor(out=lg[:], in0=pg[0][:, 0:8], in1=rb[:], op=ALU.add), reads=["pg0", "rb"], writes=["lg"])
                        S.op(V, lambda e: e.reduce_max(out=mx1[:], in_=lg[:], axis=AX.X), reads=["lg"], writes=["mx1"])
                        S.op(V, lambda e: e.tensor_scalar(out=eq1[:], in0=lg[:], scalar1=mx1[:, 0:1], scalar2=None, op0=ALU.is_equal),
                             reads=["lg", "mx1"], writes=["eq1"])
                        S.op(V, lambda e: e.scalar_tensor_tensor(out=l2[:], in0=eq1[:], scalar=-1e30, in1=lg[:], op0=ALU.mult, op1=ALU.add),
                             reads=["eq1", "lg"], writes=["l2"])
                        S.op(V, lambda e: e.reduce_max(out=mx2[:], in_=l2[:], axis=AX.X), reads=["l2"], writes=["mx2"])
                        S.op(V, lambda e: e.tensor_scalar(out=eq2[:], in0=l2[:], scalar1=mx2[:, 0:1], scalar2=None, op0=ALU.is_equal),
                             reads=["l2", "mx2"], writes=["eq2"])
                        S.op(V, lambda e: e.tensor_tensor(out=dd[:], in0=mx2[:], in1=mx1[:], op=ALU.subtract), reads=["mx1", "mx2"], writes=["dd"])
                        S.op(A, lambda e: e.activation(out=w2[:], in_=dd[:], func=AF.Sigmoid), reads=["dd"], writes=["w2"])
                        S.op(V, lambda e: e.tensor_scalar(out=w1[:], in0=w2[:], scalar1=-1.0, scalar2=1.0, op0=ALU.mult, op1=ALU.add),
                             reads=["w2"], writes=["w1"])
                        S.op(V, lambda e: e.tensor_scalar(out=g1[:], in0=eq1[:], scalar1=w1[:, 0:1], scalar2=None, op0=ALU.mult),
                             reads=["eq1", "w1"], writes=["g1"])
                        S.op(V, lambda e, tt_=tt_: e.scalar_tensor_tensor(out=gates[:, tt_, :], in0=eq2[:], scalar=w2[:, 0:1], in1=g1[:],
                                                                          op0=ALU.mult, op1=ALU.add),
                             reads=["eq2", "w2", "g1"], writes=["gates%d" % tt_])
                H2K = ["h2T_%d" % s_ for s_ in range(NSUB)]
                for ex in range(n_exp):
                    for q in range(2):
                        S.dma("gpsimd", lambda e, ex=ex, q=q: e.dma_start(
                            out=wdb[:, q * 11:(q + 1) * 11, :],
                            in_=wd_d[ex].rearrange("(fb p) n -> p fb n", p=128)[:, q * 11:(q + 1) * 11, :]), writes=["wdb%d" % q])
                    for c in range(NCH):
                        wgt = wgb[wi % 2]; wgk = "wgb%d" % (wi % 2)
                        wut = wub[wi % 2]; wuk = "wub%d" % (wi % 2)
                        wi += 1
                        cs = slice(c * WC, (c + 1) * WC)
                        S.dma("gpsimd", lambda e, wgt=wgt, ex=ex, cs=cs: e.dma_start(
                            out=wgt[:], in_=wg_d[ex].rearrange("(kb p) n -> p kb n", p=128)[:, :, cs]), writes=[wgk])
                        S.dma("gpsimd", lambda e, wut=wut, ex=ex, cs=cs: e.dma_start(
                            out=wut[:], in_=wu_d[ex].rearrange("(kb p) n -> p kb n", p=128)[:, :, cs]), writes=[wuk])
                        for fl in range(WC // 128):
                            fb = c * (WC // 128) + fl
                            for tt2 in range(HT // TT):
                                pgb = pg[gi % 2]; pgk = "pg%d" % (gi % 2)
                                pub = pu[gi % 2]; puk = "pu%d" % (gi % 2)
                                sgb = sg[gi % 2]; sgk = "sg%d" % (gi % 2)
                                gi += 1
                                tsl = slice(tt2 * TT, (tt2 + 1) * TT)

                                def mm_g(e, pgb=pgb, wgt=wgt, fl=fl, tsl=tsl):
                                    for kb in range(8):
                                        r = e.matmul(pgb[:], lhsT=wgt[:, kb, fl * 128:(fl + 1) * 128], rhs=h2T[:, kb, tsl], start=(kb == 0), stop=(kb == 7))
                                    return r
                                S.op(T, mm_g, reads=[wgk] + H2K, writes=[pgk])

                                def mm_u(e, pub=pub, wut=wut, fl=fl, tsl=tsl):
                                    for kb in range(8):
                                        r = e.matmul(pub[:], lhsT=wut[:, kb, fl * 128:(fl + 1) * 128], rhs=h2T[:, kb, tsl], start=(kb == 0), stop=(kb == 7))
                                    return r
                                S.op(T, mm_u, reads=[wuk] + H2K, writes=[puk])
                                S.op(A, lambda e, pgb=pgb, sgb=sgb: e.activation(out=sgb[:], in_=pgb[:], func=AF.Silu), reads=[pgk], writes=[sgk])
                                S.op(V, lambda e, pub=pub, sgb=sgb, fb=fb, tsl=tsl: e.tensor_tensor(out=act[:, fb, tsl], in0=pub[:], in1=sgb[:], op=ALU.mult),
                                     reads=[puk, sgk], writes=["act%d" % fb])
                    ACTK = ["act%d" % fb for fb in range(NFB)]
                    for sub in range(NSUB):
                        tt_ = th * NSUB + sub

                        def mm_d(e, sub=sub):
                            for h in range(2):
                                for fb in range(NFB):
                                    r = e.matmul(pd[:, h * 512:(h + 1) * 512], lhsT=act[:, fb, sub * 128:(sub + 1) * 128],
                                                 rhs=wdb[:, fb, h * 512:(h + 1) * 512], start=(fb == 0), stop=(fb == NFB - 1))
                            return r
                        S.op(T, mm_d, reads=ACTK + ["wdb0", "wdb1"], writes=["pd"])
                        if moe:
                            S.op(V, lambda e, tt_=tt_, ex=ex: e.scalar_tensor_tensor(out=xm[:, tt_, :], in0=pd[:], scalar=gates[:, tt_, ex:ex + 1],
                                                                                     in1=xm[:, tt_, :], op0=ALU.mult, op1=ALU.add),
                                 reads=["pd", "gates%d" % tt_, "xm%d" % tt_], writes=["xm%d" % tt_])
                        else:
                            S.op(V, lambda e, tt_=tt_: e.tensor_tensor(out=xm[:, tt_, :], in0=pd[:], in1=xm[:, tt_, :], op=ALU.add),
                                 reads=["pd", "xm%d" % tt_], writes=["xm%d" % tt_])
                if moe and not ffn and th == 1:
                    S.dma("sync", lambda e: e.dma_start(out=gates_o, in_=gates[:]), reads=["gates%d" % k for k in range(16)],
                          writes=["gates_o"], is_output=last)
                for sub in range(NSUB):
                    tt_ = th * NSUB + sub
                    xk = "xm%d" % tt_
                    if last:
                        S.op(A, lambda e, tt_=tt_: e.activation(out=h2f[:], in_=xm[:, tt_, :], func=AF.Square, accum_out=ssq[:]),
                             reads=[xk], writes=["h2f", "ssq"])
                        S.op(V, lambda e: e.tensor_scalar(out=rs[:], in0=ssq[:], scalar1=1.0 / D, scalar2=RMS_EPS, op0=ALU.mult, op1=ALU.add),
                             reads=["ssq"], writes=["rs"])
                        S.op(A, lambda e: e.sqrt(out=rs[:], in_=rs[:]), reads=["rs"], writes=["rs"])
                        S.op(V, lambda e: e.reciprocal(out=rs[:], in_=rs[:]), reads=["rs"], writes=["rs"])
                        S.op(V, lambda e, tt_=tt_: e.scalar_tensor_tensor(out=ob_ap, in0=xm[:, tt_, :], scalar=rs[:, 0:1], in1=fg[:],
                                                                          op0=ALU.mult, op1=ALU.mult),
                             reads=[xk, "rs", "fg"], writes=[obk])
                        S.dma("sync", lambda e, tt_=tt_: e.dma_start(out=out_d[:, tt_, :], in_=ob_ap), reads=[obk], writes=["out_d"], is_output=last)
                    else:
                        S.dma("sync", lambda e, tt_=tt_: e.dma_start(out=out_d[:, tt_, :], in_=xm[:, tt_, :]), reads=[xk], writes=["out_d"], is_output=last)
                        if xT_out:
                            def trx(e, tt_=tt_):
                                for kb in range(8):
                                    r = e.transpose(ptr[:, kb * 128:(kb + 1) * 128], xm[:, tt_, kb * 128:(kb + 1) * 128], ident[:])
                                return r
                            S.op(T, trx, reads=[xk, "ident"], writes=["ptr"])
                            S.op(V, lambda e: e.tensor_copy(out=h2f[:].rearrange("p (k t) -> p k t", t=128), in_=ptr[:].rearrange("p (k t) -> p k t", t=128)),
                                 reads=["ptr"], writes=["h2f"])
                            S.dma("sync", lambda e, tt_=tt_: e.dma_start(out=d["xT_out"][:, :, tt_ * 128:(tt_ + 1) * 128],
                                                                         in_=h2f[:].rearrange("p (k t) -> p k t", t=128)),
                                  reads=["h2f"], writes=["xT_scr"])
            S.barrier()
            S.flush(sst)


def _prep_Bp(p, chalf):
    gs = slice(chalf * G, (chalf + 1) * G)
    cs = slice(chalf * 256, (chalf + 1) * 256)
    dp = lambda a: np.ascontiguousarray(a[:, gs, :].transpose(0, 2, 1).reshape(128, G))
    ldt = np.ascontiguousarray(np.broadcast_to(p["ssm_log_dt"][:, gs][:, None, :], (2, 64, G)).reshape(128, G))
    bb = lambda a: np.ascontiguousarray(np.broadcast_to(a[gs].transpose(1, 0, 2)[None], (2, 64, G, 16)).reshape(128, G, 16))
    cc = lambda a: np.ascontiguousarray(a[:, gs].transpose(0, 3, 1, 2).reshape(128, G, 16))
    dsk = p["ssm_d"][cs].reshape(G, 16)
    dbc = np.ascontiguousarray(np.broadcast_to(dsk[None, :, None, :], (128, G, 8, 16)).reshape(128, G, 128))
    return {
        "dww": np.ascontiguousarray(p["conv_dw_w"][:, cs].T.reshape(2, 128, 31).transpose(1, 0, 2)),
        "dwb": np.ascontiguousarray(p["conv_dw_b"][cs].reshape(2, 128).T),
        "a_re": dp(p["ssm_a_re"]), "a_im": dp(p["ssm_a_im"]), "log_dt": ldt,
        "b_re": bb(p["ssm_b_re"]), "b_im": bb(p["ssm_b_im"]),
        "c_re": cc(p["ssm_c_re"]), "c_im": cc(p["ssm_c_im"]), "dbc": dbc,
    }


_BP_SHAPES = {"dww": [128, 2, 31], "dwb": [128, 2], "a_re": [128, G], "a_im": [128, G], "log_dt": [128, G],
              "b_re": [128, G, 16], "b_im": [128, G, 16], "c_re": [128, G, 16], "c_im": [128, G, 16], "dbc": [128, G, 128]}
_CP_SHAPES = {"lng": [128, 4], "lnb": [128, 4], "bglu": [128, 4], "wcp": [512, D], "wglu": [512, 512], "wsp": [512, D],
              "wout": [D, D], "nfg": [128, D]}


def build_mega(debug=False):
    nc = bass.Bass("TRN2", target_bir_lowering=False)
    din = lambda name, shape: nc.dram_tensor(name, shape, F32, kind="ExternalInput").ap()
    skind = "ExternalOutput" if debug else "Internal"
    scr = lambda name, shape, dt=F32: nc.dram_tensor(name, shape, dt, kind=skind).ap()
    LL = 4096
    xT0 = din("xT0", [128, 8, LL]); x_tok0 = din("x_tok0", [128, 32, D])
    mask_f = din("mask_f", [128, 128]); mask_b = din("mask_b", [128, 128])
    gcol = [din("gcol%d" % l, [128, 8]) for l in range(2)]
    w_in = [din("w_in%d" % l, [D, 3584]) for l in range(2)]
    bp = [[{k: din("B%d%d_%s" % (l, h, k), shp) for k, shp in _BP_SHAPES.items()} for h in range(2)] for l in range(2)]
    cp = [{k: din("C%d_%s" % (l, k), shp) for k, shp in _CP_SHAPES.items()} for l in range(2)]
    ffw = [{"wg": din("wg0", [1, D, DFF]), "wu": din("wu0", [1, D, DFF]), "wd": din("wd0", [1, DFF, D])},
           {"wg": din("wg1", [8, D, DFF]), "wu": din("wu1", [8, D, DFF]), "wd": din("wd1", [8, DFF, D])}]
    rw = din("rw", [128, 8, 8]); rb = din("rb", [128, 8]); fg = din("fg", [128, D])
    out = nc.dram_tensor("out", [128, 16, D], F32, kind="ExternalOutput").ap()
    vT = scr("s_vT", [128, 4, LL]); gT = scr("s_gT", [128, 4, LL]); zu = scr("s_zu", [LL, 512])
    gcT = scr("s_gcT", [128, 8, LL], BF16); gsT = scr("s_gsT", [128, 8, LL], BF16)
    convT = scr("s_convT", [128, 4, LL]); ytok = scr("s_ytok", [LL, 512])
    x_tok1 = scr("s_xtok1", [128, 32, D]); xT1 = scr("s_xT1", [128, 8, LL])
    S = Sched(nc)
    uid = [0]

    def nu():
        uid[0] += 1
        return uid[0]
    with contextlib.ExitStack() as sst:
        for l in range(2):
            xT = xT0 if l == 0 else xT1
            xtok = x_tok0 if l == 0 else x_tok1
            emit_A(nc, S, sst, {"xT": xT, "gcol": gcol[l], "w": w_in[l], "vT": vT, "gT": gT, "zu": zu, "gcT": gcT, "gsT": gsT},
                   nu(), 8, 8 if l == 0 else 4)
            for h in range(2):
                dB = dict(bp[l][h])
                dB.update({"zu": zu[:, h * 256:(h + 1) * 256], "vT": vT[:, h * 2:(h + 1) * 2, :], "gT": gT[:, h * 2:(h + 1) * 2, :],
                           "convT": convT[:, h * 2:(h + 1) * 2, :], "mask_f": mask_f, "mask_b": mask_b,
                           "ytok": ytok.rearrange("(k t) c -> k t c", t=8)[:, :, h * 256:(h + 1) * 256]})
                emit_B(nc, S, sst, dB, nu(), own_only=(l == 1))
            for hf in range(2 if l == 0 else 1):
                tk = slice(hf * NT, (hf + 1) * NT)
                dC = dict(cp[l]); dC.update(ffw[l])
                dC.update({"convT": convT[:, :, tk], "ytok": ytok[tk, :], "gcT": gcT[:, :, tk], "gsT": gsT[:, :, tk],
                           "x_tok": xtok[:, hf * 16:(hf + 1) * 16, :], "rw": rw, "rb": rb, "fg": fg})
                if l == 0:
                    dC["out"] = x_tok1[:, hf * 16:(hf + 1) * 16, :]
                    dC["xT_out"] = xT1[:, :, tk]
                    emit_C(nc, S, sst, dC, nu(), 1, False, False, xT_out=True)
                else:
                    dC["out"] = out
                    emit_C(nc, S, sst, dC, nu(), 8, True, True)
        S.finish()
        S.flush(sst)
    return nc


def prep_mega(core, inp):
    b, hf = core // 2, core % 2
    rv = (hf == 1)
    xs = inp["x"][b]
    if rv:
        xs = xs[::-1]
    s_idx = np.arange(128) // 16
    m = {
        "xT0": np.ascontiguousarray(xs.T.reshape(8, 128, 4096).transpose(1, 0, 2)),
        "x_tok0": np.ascontiguousarray(xs.reshape(32, 128, D).transpose(1, 0, 2)),
        "mask_f": (s_idx[None, :] >= s_idx[:, None]).astype(np.float32),
        "mask_b": (s_idx[None, :] <= s_idx[:, None]).astype(np.float32),
    }
    col = lambda v, nb: np.ascontiguousarray(v.reshape(nb, 128).T)
    rep = lambda v: np.ascontiguousarray(np.broadcast_to(v[None, :], (128, v.shape[0])))
    for l in range(2):
        m["gcol%d" % l] = col(inp["norm_mix_g"][l], 8)
        m["w_in%d" % l] = inp["w_in"][l]
        p = {k: inp[k][l] for k in ["conv_dw_w", "conv_dw_b", "ssm_a_re", "ssm_a_im", "ssm_log_dt", "ssm_b_re", "ssm_b_im",
                                     "ssm_c_re", "ssm_c_im", "ssm_d"]}
        if rv:
            p["conv_dw_w"] = p["conv_dw_w"][::-1]
            for k in ["ssm_a_re", "ssm_a_im", "ssm_log_dt", "ssm_c_re", "ssm_c_im"]:
                p[k] = p[k][::-1]
        for h in range(2):
            for k, v in _prep_Bp(p, h).items():
                m["B%d%d_%s" % (l, h, k)] = v
        m["C%d_lng" % l] = col(inp["conv_ln_g"][l], 4); m["C%d_lnb" % l] = col(inp["conv_ln_b"][l], 4)
        m["C%d_bglu" % l] = col(inp["ssm_b_glu"][l], 4)
        m["C%d_wcp" % l] = inp["w_conv_proj"][l]; m["C%d_wglu" % l] = inp["ssm_w_glu"][l]
        m["C%d_wsp" % l] = inp["w_ssm_proj"][l]; m["C%d_wout" % l] = inp["w_out"][l]
        m["C%d_nfg" % l] = rep(inp["norm_ffn_g"][l])
    m["wg0"] = inp["ffn_w_gate"]; m["wu0"] = inp["ffn_w_up"]; m["wd0"] = inp["ffn_w_down"]
    m["wg1"] = inp["moe_w_gate"][0]; m["wu1"] = inp["moe_w_up"][0]; m["wd1"] = inp["moe_w_down"][0]
    m["rw"] = np.ascontiguousarray(inp["router_w"][0].reshape(8, 128, 8).transpose(1, 0, 2))
    m["rb"] = rep(inp["router_b"][0]); m["fg"] = rep(inp["final_norm_g"])
    return m


_NC = {}


def kernel(**inputs):
    inp = {k: np.ascontiguousarray(np.asarray(v, dtype=np.float32)) for k, v in inputs.items()}
    if "mega" not in _NC:
        _NC["mega"] = build_mega()
    cores = list(range(8))
    res = run_bass_kernel_spmd(_NC["mega"], [prep_mega(c, inp) for c in cores], core_ids=cores)
    out = np.zeros((4, 4096, D), np.float32)
    for c in cores:
        b, hf = c // 2, c % 2
        o = res.results[c]["out"].transpose(1, 0, 2).reshape(NT, D)
        if hf == 0:
            out[b, 0:NT] = o
        else:
            out[b, NT:] = o[::-1]
    return out
```

```python
import contextlib
import math
import numpy as np
import concourse.bass as bass
import concourse.mybir as mybir
from concourse.bass_utils import run_bass_kernel_spmd

F32 = mybir.dt.float32
BF16 = mybir.dt.bfloat16
AF = mybir.ActivationFunctionType
ALU = mybir.AluOpType
AX = mybir.AxisListType

ENGS = ["tensor", "vector", "scalar", "gpsimd", "sync"]


def _flat(keys):
    out = []
    for k in keys:
        if isinstance(k, (list, tuple)):
            out.extend(_flat(k))
        else:
            out.append(k)
    return out


class Sched:
    def __init__(self, nc):
        self.nc = nc
        self.q = {e: [] for e in ENGS}
        self.cnt = {e: 0 for e in ENGS}
        self.last_w = {}
        self.readers = {}
        self.waited = {e: {} for e in ENGS}
        self.dma_cnt = {}
        self.semkeys = list(ENGS)
        self.out_tokens = []

    def _deps(self, reads, writes):
        deps = {}
        reads = _flat(reads)
        writes = _flat(writes)

        def add(tok):
            if tok is None:
                return
            k, v = tok
            if deps.get(k, 0) < v:
                deps[k] = v

        for b in reads:
            add(self.last_w.get(b))
        for b in writes:
            add(self.last_w.get(b))
            for t in self.readers.get(b, ()):
                add(t)
        return deps

    def _emit_waits(self, eng, deps):
        w = self.waited[eng]
        todo = []
        for k, v in deps.items():
            if w.get(k, 0) >= v:
                continue
            w[k] = v
            todo.append((k, v))
        return todo

    def _commit(self, tok, reads, writes):
        reads = _flat(reads)
        writes = _flat(writes)
        for b in reads:
            self.readers.setdefault(b, []).append(tok)
        for b in writes:
            self.last_w[b] = tok
            self.readers[b] = []

    def op(self, eng, fn, reads=(), writes=()):
        deps = self._deps(reads, writes)
        todo = self._emit_waits(eng, deps)
        self.cnt[eng] += 1
        n = self.cnt[eng]
        tok = (eng, n)
        self.waited[eng][eng] = max(self.waited[eng].get(eng, 0), 0)
        self.q[eng].append(("op", todo, fn, eng))
        self._commit(tok, reads, writes)
        return tok

    def dma(self, eng, fn, reads=(), writes=(), key=None, is_output=False):
        deps = self._deps(reads, writes)
        todo = self._emit_waits(eng, deps)
        if key is None:
            key = _flat(writes)[0]
        if not hasattr(self, "dma_slot"):
            self.dma_slot = {}
            self.slot_val = []
            self.slot_free = []
        if key not in self.dma_slot:
            if self.slot_free:
                slot = self.slot_free.pop()
            else:
                slot = len(self.slot_val)
                self.slot_val.append(0)
                self.semkeys.append(("slot", slot))
            self.dma_slot[key] = slot
        slot = self.dma_slot[key]
        self.slot_val[slot] += 16
        sk = ("slot", slot)
        tok = (sk, self.slot_val[slot])
        self.q[eng].append(("dma", todo, fn, sk))
        self._commit(tok, reads, writes)
        if is_output:
            self.out_tokens.append(tok)
        return tok

    def barrier(self):
        deps = {e: self.cnt[e] for e in ENGS if self.cnt[e] > 0}
        if hasattr(self, "dma_slot"):
            for i, v in enumerate(self.slot_val):
                if v > 0:
                    deps[("slot", i)] = v
        for e in ENGS:
            todo = self._emit_waits(e, dict(deps))
            self.q[e].append(("wait", todo, None, None))
        if hasattr(self, "dma_slot"):
            for k, slot in self.dma_slot.items():
                if slot not in self.slot_free:
                    self.slot_free.append(slot)
            self.dma_slot = {}

    def finish(self):
        deps = {}
        for k, v in self.out_tokens:
            deps[k] = max(deps.get(k, 0), v)
        todo = self._emit_waits("sync", deps)
        self.q["sync"].append(("wait", todo, None, None))

    def flush(self, stack):
        nc = self.nc
        if not hasattr(self, "sems"):
            self.sems = {}
        sems = self.sems
        for k in self.semkeys:
            if k not in sems:
                sems[k] = stack.enter_context(nc.semaphore("s%d" % len(sems)))
        q = self.q
        self.q = {e: [] for e in ENGS}
        with nc.Block() as block:
            def run(engname, e):
                for kind, todo, fn, key in q[engname]:
                    for k, v in todo:
                        e.wait_ge(sems[k], v)
                    if kind == "op":
                        fn(e).then_inc(sems[key], 1)
                    elif kind == "dma":
                        fn(e).then_inc(sems[key], 16)

            @block.tensor
            def _(e):
                run("tensor", e)

            @block.vector
            def _(e):
                run("vector", e)

            @block.scalar
            def _(e):
                run("scalar", e)

            @block.gpsimd
            def _(e):
                run("gpsimd", e)

            @block.sync
            def _(e):
                run("sync", e)

    def emit(self):
        self._st = contextlib.ExitStack()
        self.flush(self._st)
        self._st.close()
from concourse.ap import AP
NT = 2048
D = 1024
KB = 8
TT = 512
L = 4096
NK = L // 8
G = 16
TWO_PI = 2.0 * math.pi


def rev_ap(ap, dim):
    pat = [list(x) for x in ap.ap]
    step, cnt = pat[dim]
    off = ap.offset + step * (cnt - 1)
    pat[dim] = [-step, cnt]
    return AP(ap.tensor, off, pat)


DFF = 2816
NFB = DFF // 128
TT = 512
LN_EPS = 1e-5
RMS_EPS = 1e-6


def emit_A(nc, S, sst, d, uid, ntiles, gate_tiles, d_in=3584, eps=1e-6):
    nfo = d_in // 128
    xT = d["xT"]; gcol = d["gcol"]; w = d["w"]
    wv = w.rearrange("(kb p) n -> p kb n", p=128)
    with contextlib.ExitStack() as st:
        sb = lambda name, shape, dt: st.enter_context(nc.sbuf_tensor("sb%d_" % uid + name, shape, dt))
        ps = lambda name, shape, dt: st.enter_context(nc.psum_tensor("ps%d_" % uid + name, shape, dt))
        wsb = sb("wsb", [128, KB, d_in], BF16)
        g_sb = sb("g_sb", [128, KB], F32)
        ones = sb("ones", [128, 128], F32)
        xt = [sb("xt%d" % i, [128, KB, TT], F32) for i in range(2)]
        sq = sb("sq", [128, KB, TT], F32)
        rstd = sb("rstd", [128, TT], F32)
        hT = [sb("hT%d" % i, [128, KB, TT], BF16) for i in range(2)]
        zo = [sb("zo%d" % i, [128, TT], F32) for i in range(4)]
        zg = [sb("zg%d" % i, [128, TT], BF16) for i in range(4)]
        pss = ps("pss", [128, TT], F32)
        pz = [ps("pz%d" % i, [128, TT], F32) for i in range(4)]

        S.op("vector", lambda e: e.memset(ones[:], 1.0), writes=["ones"])
        S.dma("sync", lambda e: e.dma_start(out=g_sb[:], in_=gcol), writes=["g_sb"])
        WCH = 512
        nwch = d_in // WCH
        for c in range(nwch):
            S.dma("gpsimd", lambda e, c=c: e.dma_start(out=wsb[:, :, c * WCH:(c + 1) * WCH],
                                                       in_=wv[:, :, c * WCH:(c + 1) * WCH]),
                  writes=["w%d" % c])
        nt = ntiles
        oi = 0

        def prep(t):
            xb = xt[t % 2]
            hb = hT[t % 2]
            S.dma("sync", lambda e, xb=xb, t=t: e.dma_start(out=xb[:], in_=xT[:, :, t * TT:(t + 1) * TT]),
                  writes=["xt%d" % (t % 2)])
            S.op("scalar", lambda e, xb=xb: e.activation(out=sq[:], in_=xb[:], func=AF.Square),
                 reads=["xt%d" % (t % 2)], writes=["sq"])

            def mm_ss(e):
                for kb in range(KB):
                    r = e.matmul(pss[:], lhsT=ones[:], rhs=sq[:, kb, :], start=(kb == 0), stop=(kb == KB - 1))
                return r
            S.op("tensor", mm_ss, reads=["ones", "sq"], writes=["pss"])
            S.op("vector", lambda e: e.tensor_scalar(out=rstd[:], in0=pss[:], scalar1=1.0 / D, scalar2=eps,
                                                     op0=ALU.mult, op1=ALU.add),
                 reads=["pss"], writes=["rstd"])
            S.op("scalar", lambda e: e.sqrt(out=rstd[:], in_=rstd[:]), reads=["rstd"], writes=["rstd"])
            S.op("vector", lambda e: e.reciprocal(out=rstd[:], in_=rstd[:]), reads=["rstd"], writes=["rstd"])
            for kb in range(KB):
                S.op("vector", lambda e, kb=kb, xb=xb, hb=hb: e.scalar_tensor_tensor(
                    out=hb[:, kb, :], in0=xb[:, kb, :], scalar=g_sb[:, kb:kb + 1], in1=rstd[:],
                    op0=ALU.mult, op1=ALU.mult),
                    reads=["xt%d" % (t % 2), "rstd", "g_sb"], writes=["hT%d_%d" % (t % 2, kb)])

        def tile(t):
            nonlocal oi
            hb = hT[t % 2]
            def fo_block(fo, dst, gate=False):
                nonlocal oi
                if gate:
                    pb = pz[oi % 4]; pk = "pz%d" % (oi % 4); obg = zg[oi % 4]; okg = "zg%d" % (oi % 4)

                    def mmg(e, fo=fo, pb=pb, hb=hb):
                        for kb in range(KB):
                            r = e.matmul(pb[:], lhsT=wsb[:, kb, fo * 128:(fo + 1) * 128], rhs=hb[:, kb, :],
                                         start=(kb == 0), stop=(kb == KB - 1))
                        return r
                    S.op("tensor", mmg, reads=["w%d" % (fo * 128 // WCH)] + ["hT%d_%d" % (t % 2, kb) for kb in range(KB)], writes=[pk])
                    S.op("scalar", lambda e, pb=pb, obg=obg: e.activation(out=obg[:], in_=pb[:], func=AF.Sigmoid), reads=[pk], writes=[okg])
                    S.dma("sync", lambda e, obg=obg, dst=dst: e.dma_start(out=dst, in_=obg[:]), reads=[okg], writes=["zscr"])
                    oi += 1
                    return
                pb = pz[oi % 4]
                ob = zo[oi % 4]
                pk = "pz%d" % (oi % 4)
                ok = "zo%d" % (oi % 4)

                def mm(e, fo=fo, pb=pb, hb=hb):
                    for kb in range(KB):
                        r = e.matmul(pb[:], lhsT=wsb[:, kb, fo * 128:(fo + 1) * 128], rhs=hb[:, kb, :],
                                     start=(kb == 0), stop=(kb == KB - 1))
                    return r
                S.op("tensor", mm, reads=["w%d" % (fo * 128 // WCH)] + ["hT%d_%d" % (t % 2, kb) for kb in range(KB)],
                     writes=[pk])
                if oi % 2 == 0:
                    S.op("scalar", lambda e, pb=pb, ob=ob: e.copy(out=ob[:], in_=pb[:]), reads=[pk], writes=[ok])
                else:
                    S.op("vector", lambda e, pb=pb, ob=ob: e.tensor_copy(out=ob[:], in_=pb[:]), reads=[pk], writes=[ok])
                S.dma("sync", lambda e, ob=ob, dst=dst: e.dma_start(out=dst, in_=ob[:]), reads=[ok], writes=["zscr"])
                oi += 1
            tsl = slice(t * TT, (t + 1) * TT)
            for fo in range(4):
                fo_block(fo, d["vT"][:, fo, tsl])
            for fo in range(4, 8):
                fo_block(fo, d["gT"][:, fo - 4, tsl])
            for sub in range(TT // 128):
                pb = pz[oi % 4]; ob = zo[oi % 4]; pk = "pz%d" % (oi % 4); ok = "zo%d" % (oi % 4)

                def mmu(e, pb=pb, hb=hb, sub=sub):
                    for kb in range(KB):
                        r = e.matmul(pb[:], lhsT=hb[:, kb, sub * 128:(sub + 1) * 128], rhs=wsb[:, kb, 1024:1536],
                                     start=(kb == 0), stop=(kb == KB - 1))
                    return r
                S.op("tensor", mmu, reads=["w2"] + ["hT%d_%d" % (t % 2, kb) for kb in range(KB)], writes=[pk])
                if oi % 2 == 0:
                    S.op("scalar", lambda e, pb=pb, ob=ob: e.copy(out=ob[:], in_=pb[:]), reads=[pk], writes=[ok])
                else:
                    S.op("vector", lambda e, pb=pb, ob=ob: e.tensor_copy(out=ob[:], in_=pb[:]), reads=[pk], writes=[ok])
                r0 = t * TT + sub * 128
                S.dma("sync", lambda e, ob=ob, r0=r0: e.dma_start(out=d["zu"][r0:r0 + 128, :], in_=ob[:]), reads=[ok], writes=["zscr"])
                oi += 1
            if t + 1 < nt:
                prep(t + 1)
            if t < gate_tiles:
                for fo in range(12, 20):
                    fo_block(fo, d["gcT"][:, fo - 12, tsl], gate=True)
                for fo in range(20, 28):
                    fo_block(fo, d["gsT"][:, fo - 20, tsl], gate=True)
        prep(0)
        for t in range(nt):
            tile(t)
        S.barrier()
        S.flush(sst)


def emit_B(nc, S, sst, d, uid, own_only=False):
    zu_d = d["zu"]; vT_d = d["vT"]; gT_d = d["gT"]; dww_d = d["dww"]; dwb_d = d["dwb"]
    are_d = d["a_re"]; aim_d = d["a_im"]; ldt_d = d["log_dt"]; bre_d = d["b_re"]; bim_d = d["b_im"]
    cre_d = d["c_re"]; cim_d = d["c_im"]; dbc_d = d["dbc"]; mf_d = d["mask_f"]; mb_d = d["mask_b"]
    convT_d = d["convT"]; ytok_d = d["ytok"]
    V = "vector"
    with contextlib.ExitStack() as st:
        def mk(stack):
            return (lambda name, shape, dt=F32: stack.enter_context(nc.sbuf_tensor("sb%d_" % uid + name, shape, dt)),
                    lambda name, shape, dt=F32: stack.enter_context(nc.psum_tensor("ps%d_" % uid + name, shape, dt)))
        sb, ps = mk(st)
        ident = sb("ident", [128, 128])
        identb = sb("identb", [128, 128], BF16)
        dww = sb("dww", [128, 2, 31]); dwb = sb("dwb", [128, 2])
        WstT = sb("WstT", [128, G, 2, 128], BF16)
        Kloc = sb("Kloc", [128, G, 128], BF16)
        WoR = sb("WoR", [128, G, 128], BF16); WoI = sb("WoI", [128, G, 128], BF16)
        AR2 = sb("AR2", [128, 2, G]); AI2 = sb("AI2", [128, 2, G])
        Ub = sb("Ub", [128, G, NK], BF16)
        TR = sb("TR", [128, 2, 16, G]); TI = sb("TI", [128, 2, 16, G])
        AR128 = sb("AR128", [128, 2, G]); AI128 = sb("AI128", [128, 2, G])
        S.dma("sync", lambda e: e.dma_start(out=dww[:], in_=dww_d), writes=["dww"])
        S.dma("sync", lambda e: e.dma_start(out=dwb[:], in_=dwb_d), writes=["dwb"])
        S.op("gpsimd", lambda e: e.memset(ident[:], 1.0), writes=["ident"])
        S.op("gpsimd", lambda e: e.affine_select(out=ident[:], in_=ident[:], pattern=[[-1, 128]], compare_op=ALU.is_equal,
                                                 fill=0.0, base=0, channel_multiplier=1), reads=["ident"], writes=["ident"])
        S.op(V, lambda e: e.tensor_copy(out=identb[:], in_=ident[:]), reads=["ident"], writes=["identb"])

        with contextlib.ExitStack() as s1:
            sb1, ps1 = mk(s1)

            def ld(name, d, shape):
                t = sb1(name, shape)
                S.dma("sync", lambda e: e.dma_start(out=t[:], in_=d), writes=[name])
                return t
            are = ld("are", are_d, [128, G]); aim = ld("aim", aim_d, [128, G]); ldt = ld("ldt", ldt_d, [128, G])
            bre = ld("bre", bre_d, [128, G, 16]); bim = ld("bim", bim_d, [128, G, 16])
            cre = ld("cre", cre_d, [128, G, 16]); cim = ld("cim", cim_d, [128, G, 16])
            dbc = ld("dbc", dbc_d, [128, G, 128]); mf = ld("mf", mf_d, [128, 128]); mb = ld("mb", mb_d, [128, 128])
            cnt = [0]
            pools = {}

            def tmp(shape=(128, G), dt=F32, persist=True):
                shape = tuple(shape)
                if persist:
                    cnt[0] += 1
                    nm = "t%d" % cnt[0]
                    return sb1(nm, list(shape), dt), nm
                pl = pools.setdefault(shape, {"i": 0, "bufs": []})
                npool = 8
                if len(pl["bufs"]) < npool:
                    cnt[0] += 1
                    nm = "tp%d" % cnt[0]
                    pl["bufs"].append((sb1(nm, list(shape), dt), nm))
                r = pl["bufs"][pl["i"] % npool]
                pl["i"] += 1
                return r

            def tt(o, ok, a, ak, b, bk, op):
                S.op(V, lambda e: e.tensor_tensor(out=o, in0=a, in1=b, op=op), reads=[ak, bk], writes=[ok])

            def ts(o, ok, a, ak, s1_, s2_, op0, op1=None):
                if op1 is None:
                    S.op(V, lambda e: e.tensor_single_scalar(out=o, in_=a, scalar=s1_, op=op0), reads=[ak], writes=[ok])
                else:
                    S.op(V, lambda e: e.tensor_scalar(out=o, in0=a, scalar1=s1_, scalar2=s2_, op0=op0, op1=op1), reads=[ak], writes=[ok])

            def act(o, ok, a, ak, f):
                S.op("scalar", lambda e: e.activation(out=o, in_=a, func=f), reads=[ak], writes=[ok])

            def new_tt(a, ak, b, bk, op, shape=(128, G), persist=True):
                o, ok = tmp(shape, persist=persist)
                tt(o[:], ok, a, ak, b, bk, op)
                return o, ok

            def cmul_into(ore, orek, oim, oimk, ar_, ark, ai_, aik, br_, brk, bi_, bik, shape, sign=1.0):
                t1, k1 = new_tt(ar_, ark, br_, brk, ALU.mult, shape, False)
                t2, k2 = new_tt(ai_, aik, bi_, bik, ALU.mult, shape, False)
                tt(ore, orek, t1[:], k1, t2[:], k2, ALU.subtract)
                t3, k3 = new_tt(ar_, ark, bi_, bik, ALU.mult, shape, False)
                t4, k4 = new_tt(ai_, aik, br_, brk, ALU.mult, shape, False)
                tt(oim, oimk, t3[:], k3, t4[:], k4, ALU.add)

            def cmul(ar_, ark, ai_, aik, br_, brk, bi_, bik, shape):
                re, rk = tmp(shape); im, ik = tmp(shape)
                cmul_into(re[:], rk, im[:], ik, ar_, ark, ai_, aik, br_, brk, bi_, bik, shape)
                return re, rk, im, ik

            dt_, dtk = tmp(); act(dt_[:], dtk, ldt[:], "ldt", AF.Exp)
            adr, adrk = new_tt(are[:], "are", dt_[:], dtk, ALU.mult)
            adi, adik = new_tt(aim[:], "aim", dt_[:], dtk, ALU.mult)
            mag, magk = tmp(); act(mag[:], magk, adr[:], adrk, AF.Exp)

            def reduced(shift):
                r, rk = tmp(); ts(r[:], rk, adi[:], adik, 1.0 / TWO_PI, shift, ALU.mult, ALU.add)
                ni, nik = tmp((128, G), mybir.dt.int32)
                S.op(V, lambda e: e.tensor_copy(out=ni[:], in_=r[:]), reads=[rk], writes=[nik])
                nf, nfk = tmp()
                S.op(V, lambda e: e.tensor_copy(out=nf[:], in_=ni[:]), reads=[nik], writes=[nfk])
                fr, frk = new_tt(r[:], rk, nf[:], nfk, ALU.subtract)
                ng, ngk = tmp(); ts(ng[:], ngk, fr[:], frk, 0.0, None, ALU.is_lt)
                fr2, fr2k = new_tt(fr[:], frk, ng[:], ngk, ALU.add)
                th, thk = tmp(); ts(th[:], thk, fr2[:], fr2k, TWO_PI, -math.pi, ALU.mult, ALU.add)
                th2, th2k = tmp(); ts(th2[:], th2k, th[:], thk, 3.1415925, -3.1415925, ALU.min, ALU.max)
                o, ok = tmp(); act(o[:], ok, th2[:], th2k, AF.Sin)
                return o, ok
            sn, snk = reduced(0.5)
            cs, csk = reduced(0.75)
            abr, abrk = new_tt(mag[:], magk, cs[:], csk, ALU.mult)
            abi, abik = new_tt(mag[:], magk, sn[:], snk, ALU.mult)
            Zs = [sb1("Zs%d" % i, [128, 8, 256]) for i in range(2)]
            Zp = [sb1("Zp%d" % i, [128, 16, 8, 16]) for i in range(2)]
            pzt = [ps1("pzt%d" % i, [128, 4, 128]) for i in range(2)]
            zi = 0
            for kb4 in range(NK // 128):
                zs = Zs[kb4 % 2]; zk = "Zs%d" % (kb4 % 2)
                S.dma("sync", lambda e, zs=zs, kb4=kb4: e.dma_start(
                    out=zs[:], in_=zu_d[kb4 * 1024:(kb4 + 1) * 1024, :].rearrange("(k s) c -> k s c", s=8)), writes=[zk])
                zp = Zp[kb4 % 2]; zpk = "Zp%d" % (kb4 % 2)
                S.op("scalar", lambda e, zs=zs, zp=zp: e.copy(out=zp[:].rearrange("p g s c -> p s g c"),
                                                              in_=zs[:].rearrange("p s (g c) -> p s g c", c=16)),
                     reads=[zk], writes=[zpk])
                for g4 in range(4):
                    pb = pzt[zi % 2]; pk = "pzt%d" % (zi % 2); zi += 1

                    def trz(e, pb=pb, zp=zp, g4=g4):
                        for j in range(4):
                            g = g4 * 4 + j
                            r = e.transpose(pb[:, j, :], zp[:, g].rearrange("p s c -> p (s c)"), ident[:])
                        return r
                    S.op("tensor", trz, reads=[zpk, "ident"], writes=[pk])
                    S.op("scalar", (lambda e, pb=pb, g4=g4, kb4=kb4: e.copy(out=Ub[:, g4 * 4:(g4 + 1) * 4, kb4 * 128:(kb4 + 1) * 128], in_=pb[:])),
                         reads=[pk], writes=["Ub%d_%d" % (g4, kb4)])
            nr, nrk = tmp(); ts(nr[:], nrk, abr[:], abrk, -1.0, None, ALU.add)
            d1, d1k = new_tt(are[:], "are", are[:], "are", ALU.mult)
            d2, d2k = new_tt(aim[:], "aim", aim[:], "aim", ALU.mult)
            den, denk = new_tt(d1[:], d1k, d2[:], d2k, ALU.add)
            rden, rdenk = tmp(); S.op(V, lambda e: e.reciprocal(out=rden[:], in_=den[:]), reads=[denk], writes=[rdenk])
            u1, u1k = new_tt(nr[:], nrk, are[:], "are", ALU.mult)
            u2, u2k = new_tt(abi[:], abik, aim[:], "aim", ALU.mult)
            u3, u3k = new_tt(u1[:], u1k, u2[:], u2k, ALU.add)
            qre, qrek = new_tt(u3[:], u3k, rden[:], rdenk, ALU.mult)
            u4, u4k = new_tt(abi[:], abik, are[:], "are", ALU.mult)
            u5, u5k = new_tt(nr[:], nrk, aim[:], "aim", ALU.mult)
            u6, u6k = new_tt(u4[:], u4k, u5[:], u5k, ALU.subtract)
            qim, qimk = new_tt(u6[:], u6k, rden[:], rdenk, ALU.mult)
            m1, m1k = new_tt(abr[:], abrk, abr[:], abrk, ALU.mult)
            m2, m2k = new_tt(abi[:], abik, abi[:], abik, ALU.mult)
            m3, m3k = new_tt(m1[:], m1k, m2[:], m2k, ALU.add)
            rm, rmk = tmp(); S.op(V, lambda e: e.reciprocal(out=rm[:], in_=m3[:]), reads=[m3k], writes=[rmk])
            ibr, ibrk = new_tt(abr[:], abrk, rm[:], rmk, ALU.mult)
            ibi0, ibi0k = new_tt(abi[:], abik, rm[:], rmk, ALU.mult)
            ibi, ibik = tmp(); ts(ibi[:], ibik, ibi0[:], ibi0k, -1.0, None, ALU.mult)
            one, onek = tmp(); S.op(V, lambda e: e.memset(one[:], 1.0), writes=[onek])
            zero, zerok = tmp(); S.op(V, lambda e: e.memset(zero[:], 0.0), writes=[zerok])
            P = [(one, onek, zero, zerok), (abr, abrk, abi, abik)]
            for k in range(2, 9):
                pr, prk, pi, pik = P[-1]
                P.append(cmul(pr[:], prk, pi[:], pik, abr[:], abrk, abi[:], abik, (128, G)))
            N = [(one, onek, zero, zerok), (ibr, ibrk, ibi, ibik)]
            for k in range(2, 8):
                pr, prk, pi, pik = N[-1]
                N.append(cmul(pr[:], prk, pi[:], pik, ibr[:], ibrk, ibi[:], ibik, (128, G)))

            def bc16(t):
                return t[:].unsqueeze(2).to_broadcast([128, G, 16])
            sh3 = (128, G, 16)
            Bbr, Bbrk, Bbi, Bbik = cmul(bc16(qre), qrek, bc16(qim), qimk, bre[:], "bre", bim[:], "bim", sh3)
            Yr = sb1("Yr", [128, G, 8, 16]); Yi = sb1("Yi", [128, G, 8, 16])
            Xr = sb1("Xr", [128, G, 8, 16]); nXi = sb1("nXi", [128, G, 8, 16])
            YK = []; XK = []

            def put(dst, nm, s, src, srck, neg=False):
                for lo, pos in ((0, s), (64, 7 - s)):
                    key = "%s_%d_%d" % (nm, lo, pos)
                    if neg:
                        S.op(V, lambda e, lo=lo, pos=pos: e.tensor_single_scalar(out=dst[lo:lo + 64, :, pos, :], in_=src[lo:lo + 64],
                                                                                  scalar=-1.0, op=ALU.mult), reads=[srck], writes=[key])
                    else:
                        S.op(V, lambda e, lo=lo, pos=pos: e.tensor_copy(out=dst[lo:lo + 64, :, pos, :], in_=src[lo:lo + 64]),
                             reads=[srck], writes=[key])
                    (YK if nm[0] == "Y" else XK).append(key)
            for s in range(8):
                nr_, nrk_, ni_, nik_ = N[s]
                r, rk = tmp(sh3, persist=False); i, ik = tmp(sh3, persist=False)
                cmul_into(r[:], rk, i[:], ik, bc16(nr_), nrk_, bc16(ni_), nik_, Bbr[:], Bbrk, Bbi[:], Bbik, sh3)
                put(Yr, "Yr", s, r, rk); put(Yi, "Yi", s, i, ik)
                pr_, prk_, pi_, pik_ = P[s]
                r, rk = tmp(sh3, persist=False); i, ik = tmp(sh3, persist=False)
                cmul_into(r[:], rk, i[:], ik, bc16(pr_), prk_, bc16(pi_), pik_, cre[:], "cre", cim[:], "cim", sh3)
                put(Xr, "Xr", s, r, rk); put(nXi, "nXi", s, i, ik, neg=True)
            sh4 = (128, G, 128)

            def bc128(t):
                return t[:].unsqueeze(2).to_broadcast([128, G, 128])
            f3 = lambda t: t[:].rearrange("p g s c -> p g (s c)")
            p7r, p7rk, p7i, p7ik = P[7]
            Wr = sb1("Wr", [128, G, 128]); Wi = sb1("Wi", [128, G, 128])
            tA = sb1("tA", [128, G, 128]); tB = sb1("tB", [128, G, 128])
            tt(tA[:], "tA", bc128(p7r), p7rk, f3(Yr), YK, ALU.mult)
            tt(tB[:], "tB", bc128(p7i), p7ik, f3(Yi), YK, ALU.mult)
            tt(Wr[:], "Wr", tA[:], "tA", tB[:], "tB", ALU.subtract)
            tt(tA[:], "tA", bc128(p7r), p7rk, f3(Yi), YK, ALU.mult)
            tt(tB[:], "tB", bc128(p7i), p7ik, f3(Yr), YK, ALU.mult)
            tt(Wi[:], "Wi", tA[:], "tA", tB[:], "tB", ALU.add)
            p1r, p1rk, p1i, p1ik = P[1]
            tt(tA[:], "tA", bc128(p1r), p1rk, f3(Xr), XK, ALU.mult)
            tt(tB[:], "tB", bc128(p1i), p1ik, f3(nXi), XK, ALU.mult)
            tt(WoR[:], "WoR", tA[:], "tA", tB[:], "tB", ALU.add)
            tt(tA[:], "tA", bc128(p1r), p1rk, f3(nXi), XK, ALU.mult)
            tt(tB[:], "tB", bc128(p1i), p1ik, f3(Xr), XK, ALU.mult)
            tt(WoI[:], "WoI", tA[:], "tA", tB[:], "tB", ALU.subtract)
            ptr = [ps1("ptr%d" % i, [128, 128]) for i in range(2)]
            pkf = ps1("pkf", [128, 128]); pkb = ps1("pkb", [128, 128])
            k1 = sb1("k1", [128, 128]); k2 = sb1("k2", [128, 128]); k3 = sb1("k3", [128, 128])
            ti = 0
            for g in range(G):
                for ri, (src, nm) in enumerate(((Wr, "Wr"), (Wi, "Wi"))):
                    pb = ptr[ti % 2]; pk = "ptr%d" % (ti % 2); ti += 1
                    S.op("tensor", lambda e, pb=pb, src=src, g=g: e.transpose(pb[:], src[:, g, :], ident[:]),
                         reads=[nm, "ident"], writes=[pk])
                    S.op("scalar", lambda e, pb=pb, g=g, ri=ri: e.copy(out=WstT[:, g, ri, :], in_=pb[:]), reads=[pk], writes=["WstT"])

                def mmk(e, g=g):
                    fl = lambda t, lo: t[lo:lo + 64, g].rearrange("p s c -> p (s c)")
                    e.matmul(pkf[:], lhsT=fl(Yr, 0), rhs=fl(Xr, 0), start=True, stop=False)
                    e.matmul(pkf[:], lhsT=fl(Yi, 0), rhs=fl(nXi, 0), start=False, stop=True)
                    e.matmul(pkb[:], lhsT=fl(Yr, 64), rhs=fl(Xr, 64), start=True, stop=False)
                    return e.matmul(pkb[:], lhsT=fl(Yi, 64), rhs=fl(nXi, 64), start=False, stop=True)
                S.op("tensor", mmk, reads=YK + XK, writes=["pkf", "pkb"])
                S.op(V, lambda e: e.tensor_tensor(out=k1[:], in0=pkf[:], in1=mf[:], op=ALU.mult), reads=["pkf", "mf"], writes=["k1"])
                S.op(V, lambda e: e.tensor_tensor(out=k2[:], in0=pkb[:], in1=mb[:], op=ALU.mult), reads=["pkb", "mb"], writes=["k2"])
                S.op(V, lambda e, g=g: e.tensor_tensor(out=k3[:], in0=ident[:], in1=dbc[:, g, :], op=ALU.mult), reads=["ident", "dbc"], writes=["k3"])
                S.op(V, lambda e: e.tensor_tensor(out=k1[:], in0=k1[:], in1=k2[:], op=ALU.add), reads=["k1", "k2"], writes=["k1"])
                S.op(V, lambda e, g=g: e.tensor_tensor(out=Kloc[:, g, :], in0=k1[:], in1=k3[:], op=ALU.add), reads=["k1", "k3"], writes=["Kloc"])
            p8r, p8rk, p8i, p8ik = P[8]
            S.op(V, lambda e: e.tensor_copy(out=AR2[:, 0, :], in_=p8r[:]), reads=[p8rk], writes=["AR2a"])
            S.op(V, lambda e: e.tensor_copy(out=AR2[:, 1, :], in_=p8r[:]), reads=[p8rk], writes=["AR2b"])
            S.op(V, lambda e: e.tensor_single_scalar(out=AI2[:, 0, :], in_=p8i[:], scalar=-1.0, op=ALU.mult), reads=[p8ik], writes=["AI2a"])
            S.op(V, lambda e: e.tensor_copy(out=AI2[:, 1, :], in_=p8i[:]), reads=[p8ik], writes=["AI2b"])
            Q = [None, P[8]]
            for m_ in range(2, 17):
                qr, qrk, qi, qik = Q[-1]
                Q.append(cmul(qr[:], qrk, qi[:], qik, p8r[:], p8rk, p8i[:], p8ik, (128, G)))
            for j_ in range(16):
                qr, qrk, qi, qik = Q[j_ + 1]
                S.op(V, lambda e, j_=j_, qr=qr: e.tensor_copy(out=TR[:, 0, j_, :], in_=qr[:]), reads=[qrk], writes=["TRI"])
                S.op(V, lambda e, j_=j_, qr=qr: e.tensor_copy(out=TR[:, 1, j_, :], in_=qr[:]), reads=[qrk], writes=["TRI"])
                S.op(V, lambda e, j_=j_, qi=qi: e.tensor_single_scalar(out=TI[:, 0, j_, :], in_=qi[:], scalar=-1.0, op=ALU.mult), reads=[qik], writes=["TRI"])
                S.op(V, lambda e, j_=j_, qi=qi: e.tensor_copy(out=TI[:, 1, j_, :], in_=qi[:]), reads=[qik], writes=["TRI"])
            qr, qrk, qi, qik = Q[16]
            S.op(V, lambda e, qr=qr: e.tensor_copy(out=AR128[:, 0, :], in_=qr[:]), reads=[qrk], writes=["A128"])
            S.op(V, lambda e, qr=qr: e.tensor_copy(out=AR128[:, 1, :], in_=qr[:]), reads=[qrk], writes=["A128"])
            S.op(V, lambda e, qi=qi: e.tensor_single_scalar(out=AI128[:, 0, :], in_=qi[:], scalar=-1.0, op=ALU.mult), reads=[qik], writes=["A128"])
            S.op(V, lambda e, qi=qi: e.tensor_copy(out=AI128[:, 1, :], in_=qi[:]), reads=[qik], writes=["A128"])
            S.barrier()
            S.flush(sst)
        AK = ["AR2a", "AR2b", "AI2a", "AI2b"]

        St = sb("St", [128, 2, NK, G])
        with contextlib.ExitStack() as s2:
            sb2, ps2 = mk(s2)
            hc = sb2("hc", [128, 2, L + 30], BF16)
            S.op("gpsimd", lambda e: e.memset(hc[:, :, 0:15], 0.0), writes=["hcp0"])
            S.op("gpsimd", lambda e: e.memset(hc[:, :, L + 15:L + 30], 0.0), writes=["hcp1"])
            CW = 512
            vt = [sb2("vt%d" % i, [128, CW]) for i in range(4)]
            gt = [sb2("gt%d" % i, [128, CW]) for i in range(4)]
            ci = 0
            HCK = []
            for blk in range(2):
                for q in range((L // CW) // 2 + 1 if own_only else L // CW):
                    vb = vt[ci % 4]; gb = gt[ci % 4]; vk = "vt%d" % (ci % 4); gk = "gt%d" % (ci % 4)
                    sl = slice(q * CW, (q + 1) * CW)
                    S.dma("sync", lambda e, vb=vb, blk=blk, sl=sl: e.dma_start(out=vb[:], in_=vT_d[:, blk, sl]), writes=[vk])
                    S.dma("sync", lambda e, gb=gb, blk=blk, sl=sl: e.dma_start(out=gb[:], in_=gT_d[:, blk, sl]), writes=[gk])
                    S.op("scalar", lambda e, gb=gb: e.activation(out=gb[:], in_=gb[:], func=AF.Sigmoid), reads=[gk], writes=[gk])
                    hk = "hc%d_%d" % (blk, q)
                    S.op(V, lambda e, vb=vb, gb=gb, blk=blk, q=q: e.tensor_tensor(out=hc[:, blk, 15 + q * CW:15 + (q + 1) * CW],
                                                                                 in0=vb[:], in1=gb[:], op=ALU.mult),
                         reads=[vk, gk], writes=[hk])
                    HCK.append(hk)
                    ci += 1
            dg = sb2("dg", [128, 2, 31, 128], BF16)
            for blk in range(2):
                S.op("gpsimd", lambda e, blk=blk: e.tensor_tensor(out=dg[:, blk], in0=ident[:].unsqueeze(1).to_broadcast([128, 31, 128]),
                                                                  in1=dww[:, blk, :].unsqueeze(2).to_broadcast([128, 31, 128]), op=ALU.mult),
                     reads=["ident", "dww"], writes=["dg%d" % blk])
            pst = [ps2("pst%d" % i, [128, NK]) for i in range(2)]
            si = 0
            STK = []
            for g in range(G):
                for ri in range(2):
                    pb = pst[si % 2]; pk = "pst%d" % (si % 2)
                    S.op("tensor", lambda e, pb=pb, g=g, ri=ri: e.matmul(pb[:], lhsT=WstT[:, g, ri, :], rhs=Ub[:, g, :], start=True, stop=True),
                         reads=["WstT"], writes=[pk])
                    sk = "St%d_%d" % (ri, g)
                    S.op("scalar", lambda e, pb=pb, g=g, ri=ri: e.copy(out=St[0:64, ri, :, g], in_=pb[0:64, :]), reads=[pk], writes=[sk])
                    S.op(V, lambda e, pb=pb, g=g, ri=ri: e.tensor_copy(out=St[64:128, ri, :, g], in_=rev_ap(pb[64:128, :], 1)), reads=[pk], writes=[sk])
                    STK.append(sk)
                    si += 1
            sc = {nm: (sb2("sc1" + nm, [128, 2, G]), sb2("sc2" + nm, [128, 2, G])) for nm in ("f", "b")}

            def scan(eng, nm, lo, order, npart=64):
                t1, t2 = sc[nm]
                hk = "H" + nm
                first = True
                for kprev, kcur in order:
                    rd = (STK if first else []) + [hk] + AK
                    first = False
                    prev = St[lo:lo + npart, :, kprev, :]
                    prev_sw = rev_ap(St[lo:lo + npart, :, kprev, :], 1)
                    cur = St[lo:lo + npart, :, kcur, :]
                    S.op(eng, lambda e, prev=prev: e.tensor_tensor(out=t1[lo:lo + npart], in0=prev, in1=AR2[lo:lo + npart], op=ALU.mult),
                         reads=rd, writes=["sc1" + nm])
                    S.op(eng, lambda e, prev_sw=prev_sw: e.tensor_tensor(out=t2[lo:lo + npart], in0=prev_sw, in1=AI2[lo:lo + npart], op=ALU.mult),
                         reads=rd, writes=["sc2" + nm])
                    S.op(eng, lambda e, cur=cur: e.tensor_tensor(out=cur, in0=cur, in1=t1[lo:lo + npart], op=ALU.add),
                         reads=["sc1" + nm] + rd, writes=[hk])
                    S.op(eng, lambda e, cur=cur: e.tensor_tensor(out=cur, in0=cur, in1=t2[lo:lo + npart], op=ALU.add),
                         reads=["sc2" + nm], writes=[hk])
            NB = NK // 16
            Stv = St[:].rearrange("p r (b j) g -> p r b j g", j=16)
            t1b = sb2("t1b", [128, 2, NB, G]); t2b = sb2("t2b", [128, 2, NB, G]); Cb = sb2("Cb", [128, 2, NB, G])
            bcb = lambda t: t.unsqueeze(2).to_broadcast([128, 2, NB, G])
            HK = "Hf"
            for j_ in range(1, 16):
                prev = Stv[:, :, :, j_ - 1, :]; prev_sw = rev_ap(prev, 1); cur = Stv[:, :, :, j_, :]
                rd = (STK if j_ == 1 else []) + [HK] + AK
                S.op(V, lambda e, prev=prev: e.tensor_tensor(out=t1b[:], in0=prev, in1=bcb(AR2[:]), op=ALU.mult), reads=rd, writes=["t1b"])
                S.op(V, lambda e, prev_sw=prev_sw: e.tensor_tensor(out=t2b[:], in0=prev_sw, in1=bcb(AI2[:]), op=ALU.mult), reads=rd, writes=["t2b"])
                S.op(V, lambda e, cur=cur: e.tensor_tensor(out=cur, in0=cur, in1=t1b[:], op=ALU.add), reads=["t1b"] + rd, writes=[HK])
                S.op(V, lambda e, cur=cur: e.tensor_tensor(out=cur, in0=cur, in1=t2b[:], op=ALU.add), reads=["t2b"], writes=[HK])
            c1, c2 = sc["f"]
            S.op(V, lambda e: e.memset(Cb[:, :, 0, :], 0.0), writes=["Cb"])
            for b_ in range(NB - 1):
                cprev = Cb[:, :, b_, :]; cprev_sw = rev_ap(cprev, 1); cnext = Cb[:, :, b_ + 1, :]
                S.op(V, lambda e, cprev=cprev: e.tensor_tensor(out=c1[:], in0=cprev, in1=AR128[:], op=ALU.mult), reads=["Cb", "A128"], writes=["c1"])
                S.op(V, lambda e, cprev_sw=cprev_sw: e.tensor_tensor(out=c2[:], in0=cprev_sw, in1=AI128[:], op=ALU.mult), reads=["Cb", "A128"], writes=["c2"])
                S.op(V, lambda e, cnext=cnext, b_=b_: e.tensor_tensor(out=cnext, in0=c1[:], in1=Stv[:, :, b_, 15, :], op=ALU.add), reads=["c1", HK], writes=["Cb"])
                S.op(V, lambda e, cnext=cnext: e.tensor_tensor(out=cnext, in0=cnext, in1=c2[:], op=ALU.add), reads=["c2", "Cb"], writes=["Cb"])
            Cb_sw = rev_ap(Cb[:], 1)
            for j_ in range(16):
                cur = Stv[:, :, :, j_, :]
                S.op(V, lambda e, j_=j_: e.tensor_tensor(out=t1b[:], in0=Cb[:], in1=bcb(TR[:, :, j_, :]), op=ALU.mult), reads=["Cb", "TRI"], writes=["t1b"])
                S.op(V, lambda e, j_=j_: e.tensor_tensor(out=t2b[:], in0=Cb_sw, in1=bcb(TI[:, :, j_, :]), op=ALU.mult), reads=["Cb", "TRI"], writes=["t2b"])
                S.op(V, lambda e, cur=cur: e.tensor_tensor(out=cur, in0=cur, in1=t1b[:], op=ALU.add), reads=["t1b", HK], writes=[HK])
                S.op(V, lambda e, cur=cur: e.tensor_tensor(out=cur, in0=cur, in1=t2b[:], op=ALU.add), reads=["t2b", HK], writes=[HK])
            pc = [ps2("pc%d" % i, [128, 512]) for i in range(2)]
            co = [sb2("co%d" % i, [128, 512]) for i in range(2)]
            ti = 0
            for blk in range(2):
                for t in range((L // 512) // 2 if own_only else L // 512):
                    pb = pc[ti % 2]; pk = "pc%d" % (ti % 2); ob = co[ti % 2]; ok = "co%d" % (ti % 2)

                    def mmc(e, pb=pb, blk=blk, t=t):
                        for k in range(31):
                            r = e.matmul(pb[:], lhsT=dg[:, blk, k, :], rhs=hc[:, blk, t * 512 + k:t * 512 + k + 512],
                                         start=(k == 0), stop=(k == 30))
                        return r
                    S.op("tensor", mmc, reads=["dg%d" % blk, "hcp0", "hcp1"] + HCK, writes=[pk])
                    S.op("scalar", lambda e, pb=pb, ob=ob, blk=blk: e.activation(out=ob[:], in_=pb[:], func=AF.Identity, bias=dwb[:, blk:blk + 1]),
                         reads=[pk, "dwb"], writes=[ok])
                    S.dma("sync", lambda e, ob=ob, blk=blk, t=t: e.dma_start(out=convT_d[:, blk, t * 512:(t + 1) * 512], in_=ob[:]),
                          reads=[ok], writes=["convT_d"])
                    ti += 1
            S.barrier()
            S.flush(sst)
        with contextlib.ExitStack() as s3:
            sb3, ps3 = mk(s3)
            Hin = sb3("Hin", [128, 2, G, NK], BF16)
            S.op("gpsimd", lambda e: e.memset(Hin[:], 0.0), writes=["Hin"])
            for ri in range(2):
                S.op(V, lambda e, ri=ri: e.tensor_copy(out=Hin[0:64, ri, :, 1:NK], in_=St[0:64, ri, 0:NK - 1, :].rearrange("p k g -> p g k")),
                     reads=["Hf"], writes=["Hin"])
                S.op(V, lambda e, ri=ri: e.tensor_copy(out=Hin[64:128, ri, :, 0:NK - 1],
                                                       in_=rev_ap(St[64:128, ri, 0:NK - 1, :].rearrange("p k g -> p g k"), 2)),
                     reads=["Hf"], writes=["Hin"])
            py = [ps3("py%d" % i, [128, 128]) for i in range(2)]
            Yo = [sb3("Yo%d" % i, [128, 8, G, 16]) for i in range(2)]
            yi = 0
            for kb in range(2 if own_only else 4):
                yo = Yo[kb % 2]; yok = "Yo%d" % (kb % 2)
                for g in range(G):
                    pb = py[yi % 2]; pk = "py%d" % (yi % 2)

                    def mmy(e, pb=pb, g=g, kb=kb):
                        ks = slice(kb * 128, (kb + 1) * 128)
                        e.matmul(pb[:], lhsT=Ub[:, g, ks], rhs=Kloc[:, g, :], start=True, stop=False)
                        e.matmul(pb[:], lhsT=Hin[:, 0, g, ks], rhs=WoR[:, g, :], start=False, stop=False)
                        return e.matmul(pb[:], lhsT=Hin[:, 1, g, ks], rhs=WoI[:, g, :], start=False, stop=True)
                    S.op("tensor", mmy, reads=["Kloc", "Hin", "WoR", "WoI"], writes=[pk])
                    if yi % 2 == 0:
                        S.op("scalar", lambda e, pb=pb, g=g, yo=yo: e.copy(out=yo[:, :, g, :], in_=pb[:].rearrange("p (t c) -> p t c", c=16)), reads=[pk], writes=[yok])
                    else:
                        S.op(V, lambda e, pb=pb, g=g, yo=yo: e.tensor_copy(out=yo[:, :, g, :], in_=pb[:].rearrange("p (t c) -> p t c", c=16)), reads=[pk], writes=[yok])
                    yi += 1
                S.dma("sync", lambda e, kb=kb, yo=yo: e.dma_start(out=ytok_d[kb * 128:(kb + 1) * 128], in_=yo[:].rearrange("p t g c -> p t (g c)")), reads=[yok], writes=["Y_d"])
            S.barrier()
            S.flush(sst)


def emit_C(nc, S, sst, d, uid, n_exp, moe, last, xT_out=False):
    ffn = True
    convT_d = d["convT"]; ytok_d = d["ytok"]; gcT_d = d["gcT"]; gsT_d = d["gsT"]; x_d = d["x_tok"]
    lng_d = d["lng"]; lnb_d = d["lnb"]; bglu_d = d["bglu"]
    wcp_d = d["wcp"]; wglu_d = d["wglu"]; wsp_d = d["wsp"]; wout_d = d["wout"]; nfg_d = d["nfg"]
    wg_d = d["wg"]; wu_d = d["wu"]; wd_d = d["wd"]
    if moe:
        rw_d = d["rw"]; rb_d = d["rb"]
    if last:
        fg_d = d["fg"]
    out_d = d["out"]
    V = "vector"; A = "scalar"; T = "tensor"
    with contextlib.ExitStack() as st:
        def mk(stack):
            return (lambda name, shape, dt=F32: stack.enter_context(nc.sbuf_tensor("sb%d_" % uid + name, shape, dt)),
                    lambda name, shape, dt=F32: stack.enter_context(nc.psum_tensor("ps%d_" % uid + name, shape, dt)))
        sb, ps = mk(st)
        xm = sb("xm", [128, 16, D])
        ident = sb("ident", [128, 128])
        onesm = sb("onesm", [128, 128])
        S.op("gpsimd", lambda e: e.memset(ident[:], 1.0), writes=["ident"])
        S.op("gpsimd", lambda e: e.affine_select(out=ident[:], in_=ident[:], pattern=[[-1, 128]], compare_op=ALU.is_equal,
                                                 fill=0.0, base=0, channel_multiplier=1), reads=["ident"], writes=["ident"])
        S.op("gpsimd", lambda e: e.memset(onesm[:], 1.0 / 512.0), writes=["onesm"])

        def ldsm(sbf, name, d, shape):
            t = sbf(name, shape)
            S.dma("sync", lambda e: e.dma_start(out=t[:], in_=d), writes=[name])
            return t
        nfg = ldsm(sb, "nfg", nfg_d, [128, D])
        if last:
            pass
        if moe:
            rw = ldsm(sb, "rw", rw_d, [128, 8, 8]); rb = ldsm(sb, "rb", rb_d, [128, 8])
            gates = sb("gates", [128, 16, 8])

        with contextlib.ExitStack() as s1:
            sb1, ps1 = mk(s1)
            lng = ldsm(sb1, "lng", lng_d, [128, 4]); lnb = ldsm(sb1, "lnb", lnb_d, [128, 4]); bglu = ldsm(sb1, "bglu", bglu_d, [128, 4])
            wcp = sb1("wcp", [128, 4, D], BF16); wglu = sb1("wglu", [128, 4, 512], BF16)
            wsp = sb1("wsp", [128, 4, D], BF16); wout = sb1("wout", [128, 8, D], BF16)
            for t_, d_, nm in ((wcp, wcp_d, "wcp"), (wglu, wglu_d, "wglu"), (wsp, wsp_d, "wsp"), (wout, wout_d, "wout")):
                S.dma("gpsimd", lambda e, t_=t_, d_=d_: e.dma_start(out=t_[:], in_=d_.rearrange("(kb p) n -> p kb n", p=128)), writes=[nm])
            cv = sb1("cv", [128, 4, TT]); sq = sb1("sq", [128, 4, TT])
            yv = sb1("yv", [128, 4, TT]); ytk = sb1("ytk", [128, 4, TT]); ygb = sb1("ygb", [128, 4, TT], BF16)
            mean = sb1("mean", [128, TT]); var = sb1("var", [128, TT]); rstd = var
            ca = sb1("ca", [128, 4, TT], BF16)
            gl = sb1("gl", [128, 4, TT]); y2 = sb1("y2", [128, 4, TT], BF16)
            gcb = [sb1("gcb%d" % i, [128, TT], BF16) for i in range(4)]
            gsb = [sb1("gsb%d" % i, [128, TT], BF16) for i in range(4)]
            m1 = [sb1("m1_%d" % i, [128, TT]) for i in range(2)]
            m2 = [sb1("m2_%d" % i, [128, TT]) for i in range(2)]
            mg = sb1("mg", [128, 8, TT], BF16)
            xin = [sb1("xin%d" % i, [128, D]) for i in range(2)]
            pmean = ps1("pmean", [128, TT]); pex2 = ps1("pex2", [128, TT])
            pA = [ps1("pA%d" % i, [128, TT]) for i in range(2)]
            pB = [ps1("pB%d" % i, [128, TT]) for i in range(2)]
            pW = [ps1("pW%d" % i, [128, TT]) for i in range(2)]
            fi = 0
            xi = 0
            caL = [ca, sb1("ca_b", [128, 4, TT], BF16)]; y2L = [y2, sb1("y2_b", [128, 4, TT], BF16)]

            def front(t):
                nonlocal fi
                ca = caL[t % 2]; y2 = y2L[t % 2]; sfx = str(t % 2)
                ts_ = slice(t * TT, (t + 1) * TT)
                S.dma("sync", lambda e, ts_=ts_: e.dma_start(out=cv[:], in_=convT_d[:, :, ts_]), writes=["cv"])
                S.dma("sync", lambda e, t=t: e.dma_start(out=ytk[:], in_=ytok_d[t * TT:(t + 1) * TT, :].rearrange("(s p) c -> p s c", p=128)),
                      writes=["ytk"])
                S.op(A, lambda e: e.activation(out=sq[:], in_=cv[:], func=AF.Square), reads=["cv"], writes=["sq"])

                def mm_stats(e):
                    for kb in range(4):
                        e.matmul(pmean[:], lhsT=onesm[:], rhs=cv[:, kb, :], start=(kb == 0), stop=(kb == 3))
                    for kb in range(4):
                        r = e.matmul(pex2[:], lhsT=onesm[:], rhs=sq[:, kb, :], start=(kb == 0), stop=(kb == 3))
                    return r
                S.op(T, mm_stats, reads=["onesm", "cv", "sq"], writes=["pmean", "pex2"])
                S.op(V, lambda e: e.tensor_copy(out=mean[:], in_=pmean[:]), reads=["pmean"], writes=["mean"])
                S.op(V, lambda e: e.tensor_tensor(out=var[:], in0=mean[:], in1=mean[:], op=ALU.mult), reads=["mean"], writes=["var"])
                S.op(V, lambda e: e.tensor_tensor(out=var[:], in0=pex2[:], in1=var[:], op=ALU.subtract), reads=["pex2", "var"], writes=["var"])
                S.op(V, lambda e: e.tensor_scalar(out=var[:], in0=var[:], scalar1=LN_EPS, scalar2=None, op0=ALU.add), reads=["var"], writes=["var"])
                S.op(A, lambda e: e.sqrt(out=var[:], in_=var[:]), reads=["var"], writes=["var"])
                S.op(V, lambda e: e.reciprocal(out=rstd[:], in_=var[:]), reads=["var"], writes=["var"])
                for kb in range(4):
                    S.op(V, lambda e, kb=kb: e.tensor_tensor(out=cv[:, kb, :], in0=cv[:, kb, :], in1=mean[:], op=ALU.subtract),
                         reads=["cv", "mean"], writes=["cv"])
                    S.op(V, lambda e, kb=kb: e.tensor_tensor(out=cv[:, kb, :], in0=cv[:, kb, :], in1=rstd[:], op=ALU.mult),
                         reads=["cv", "var"], writes=["cv"])
                    S.op(A, lambda e, kb=kb: e.activation(out=ca[:, kb, :], in_=cv[:, kb, :], func=AF.Silu,
                                                         scale=lng[:, kb:kb + 1], bias=lnb[:, kb:kb + 1]),
                         reads=["cv", "lng", "lnb"], writes=["ca" + sfx])
                S.op(A, lambda e: e.activation(out=ytk[:], in_=ytk[:], func=AF.Gelu_apprx_tanh), reads=["ytk"], writes=["ytk"])
                for cb in range(4):
                    pbt = (pA + pB)[cb]; pbk = ("pA0", "pA1", "pB0", "pB1")[cb]

                    def try_(e, pbt=pbt, cb=cb):
                        for s_ in range(4):
                            r = e.transpose(pbt[:, s_ * 128:(s_ + 1) * 128], ytk[:, s_, cb * 128:(cb + 1) * 128], ident[:])
                        return r
                    S.op(T, try_, reads=["ytk", "ident"], writes=[pbk])
                    S.op(V, lambda e, pbt=pbt, cb=cb: e.tensor_copy(out=yv[:, cb, :], in_=pbt[:]), reads=[pbk], writes=["yv"])
                S.op(V, lambda e: e.tensor_copy(out=ygb[:], in_=yv[:]), reads=["yv"], writes=["ygb"])
                for fo in range(4):
                    pb = pA[fi % 2]; pk = "pA%d" % (fi % 2); fi += 1

                    def mm_glu(e, pb=pb, fo=fo):
                        for kb in range(4):
                            r = e.matmul(pb[:], lhsT=wglu[:, kb, fo * 128:(fo + 1) * 128], rhs=ygb[:, kb, :], start=(kb == 0), stop=(kb == 3))
                        return r
                    S.op(T, mm_glu, reads=["wglu", "ygb"], writes=[pk])
                    S.op(A, lambda e, pb=pb, fo=fo: e.activation(out=gl[:, fo, :], in_=pb[:], func=AF.Sigmoid, bias=bglu[:, fo:fo + 1]),
                         reads=[pk, "bglu"], writes=["gl"])
                    S.op(V, lambda e, fo=fo: e.tensor_tensor(out=y2[:, fo, :], in0=yv[:, fo, :], in1=gl[:, fo, :], op=ALU.mult),
                         reads=["yv", "gl"], writes=["y2" + sfx])

            def back(t):
                nonlocal fi, xi
                ca = caL[t % 2]; y2 = y2L[t % 2]; sfx = str(t % 2)
                ts_ = slice(t * TT, (t + 1) * TT)
                for fo in range(8):
                    pa = pA[fi % 2]; pak = "pA%d" % (fi % 2)
                    pb = pB[fi % 2]; pbk = "pB%d" % (fi % 2)
                    gc = gcb[fi % 4]; gck = "gcb%d" % (fi % 4)
                    gs = gsb[fi % 4]; gsk = "gsb%d" % (fi % 4)
                    ma = m1[fi % 2]; mak = "m1_%d" % (fi % 2)
                    mb_ = m2[fi % 2]; mbk = "m2_%d" % (fi % 2)
                    fi += 1
                    S.dma("sync", lambda e, gc=gc, fo=fo, ts_=ts_: e.dma_start(out=gc[:], in_=gcT_d[:, fo, ts_]), writes=[gck])
                    S.dma("sync", lambda e, gs=gs, fo=fo, ts_=ts_: e.dma_start(out=gs[:], in_=gsT_d[:, fo, ts_]), writes=[gsk])

                    def mm_cp(e, pa=pa, fo=fo):
                        for kb in range(4):
                            r = e.matmul(pa[:], lhsT=wcp[:, kb, fo * 128:(fo + 1) * 128], rhs=ca[:, kb, :], start=(kb == 0), stop=(kb == 3))
                        return r
                    S.op(T, mm_cp, reads=["wcp", "ca" + sfx], writes=[pak])

                    def mm_sp(e, pb=pb, fo=fo):
                        for kb in range(4):
                            r = e.matmul(pb[:], lhsT=wsp[:, kb, fo * 128:(fo + 1) * 128], rhs=y2[:, kb, :], start=(kb == 0), stop=(kb == 3))
                        return r
                    S.op(T, mm_sp, reads=["wsp", "y2" + sfx], writes=[pbk])
                    S.op(V, lambda e, pa=pa, gc=gc, ma=ma: e.tensor_tensor(out=ma[:], in0=pa[:], in1=gc[:], op=ALU.mult), reads=[pak, gck], writes=[mak])
                    S.op(V, lambda e, pb=pb, gs=gs, mb_=mb_: e.tensor_tensor(out=mb_[:], in0=pb[:], in1=gs[:], op=ALU.mult), reads=[pbk, gsk], writes=[mbk])
                    S.op(V, lambda e, ma=ma, mb_=mb_, fo=fo: e.tensor_tensor(out=mg[:, fo, :], in0=ma[:], in1=mb_[:], op=ALU.add),
                         reads=[mak, mbk], writes=["mg%d" % fo])
                for sub in range(TT // 128):
                    tt_ = t * (TT // 128) + sub
                    xb = xin[xi % 2]; xk = "xin%d" % (xi % 2); xi += 1
                    S.dma("sync", lambda e, xb=xb, tt_=tt_: e.dma_start(out=xb[:], in_=x_d[:, tt_, :]), writes=[xk])

                    def mm_wo(e, sub=sub):
                        for h in range(2):
                            for kb in range(8):
                                r = e.matmul(pW[h][:], lhsT=mg[:, kb, sub * 128:(sub + 1) * 128], rhs=wout[:, kb, h * 512:(h + 1) * 512],
                                             start=(kb == 0), stop=(kb == 7))
                        return r
                    S.op(T, mm_wo, reads=["wout"] + ["mg%d" % k for k in range(8)], writes=["pW0", "pW1"])
                    for h in range(2):
                        S.op(V, lambda e, h=h, xb=xb, tt_=tt_: e.tensor_tensor(out=xm[:, tt_, h * 512:(h + 1) * 512], in0=pW[h][:],
                                                                               in1=xb[:, h * 512:(h + 1) * 512], op=ALU.add),
                             reads=["pW%d" % h, xk], writes=["xm%d" % tt_])

            front(0)
            for t in range(NT // TT):
                if t + 1 < NT // TT:
                    front(t + 1)
                back(t)
            S.barrier()
            S.flush(sst)

        with contextlib.ExitStack() as s2:
            sb2, ps2 = mk(s2)
            HT = NT // 2
            NSUB = HT // 128
            h2T = sb2("h2T", [128, 8, NT], BF16)
            h2f = sb2("h2f", [128, D])
            ssq = sb2("ssq", [128, 1]); rs = sb2("rs", [128, 1])
            if ffn:
                act = sb2("act", [128, NFB, HT], BF16)
                wdb = sb2("wdb", [128, NFB, D], BF16)
            WC = 128
            NCH = DFF // WC
            wgb = [sb2("wgb%d" % i, [128, 8, WC], BF16) for i in range(2)]
            wub = [sb2("wub%d" % i, [128, 8, WC], BF16) for i in range(2)]
            sg = [sb2("sg%d" % i, [128, TT], BF16) for i in range(2)]
            ptr = ps2("ptr", [128, 8 * 128])
            pg = [ps2("pg%d" % i, [128, TT]) for i in range(2)]
            pu = [ps2("pu%d" % i, [128, TT]) for i in range(2)]
            pd = ps2("pd", [128, D])
            if not moe:
                ob_ap = h2f[:]; obk = "h2f"
            if moe:
                h2T32 = h2f[:].rearrange("p (k t) -> p k t", t=128)
                ob_ap = h2f[:]; obk = "h2f"
                lg = sb2("lg", [128, 8]); l2 = sb2("l2", [128, 8]); eq1 = sb2("eq1", [128, 8]); eq2 = sb2("eq2", [128, 8])
                mx1 = sb2("mx1", [128, 1]); mx2 = sb2("mx2", [128, 1]); dd = sb2("dd", [128, 1]); w1 = sb2("w1", [128, 1]); w2 = sb2("w2", [128, 1])
                g1 = sb2("g1", [128, 8])
            wi = 0
            gi = 0
            def P2(th):
                for sub in range(NSUB):
                    tt_ = th * NSUB + sub
                    xk = "xm%d" % tt_
                    S.op(A, lambda e, tt_=tt_: e.activation(out=ob_ap, in_=xm[:, tt_, :], func=AF.Square, accum_out=ssq[:]),
                         reads=[xk], writes=[obk, "ssq"])
                    S.op(V, lambda e: e.tensor_scalar(out=rs[:], in0=ssq[:], scalar1=1.0 / D, scalar2=RMS_EPS, op0=ALU.mult, op1=ALU.add),
                         reads=["ssq"], writes=["rs"])
                    S.op(A, lambda e: e.sqrt(out=rs[:], in_=rs[:]), reads=["rs"], writes=["rs"])
                    S.op(V, lambda e: e.reciprocal(out=rs[:], in_=rs[:]), reads=["rs"], writes=["rs"])
                    S.op(V, lambda e, tt_=tt_: e.scalar_tensor_tensor(out=h2f[:], in0=xm[:, tt_, :], scalar=rs[:, 0:1], in1=nfg[:],
                                                                      op0=ALU.mult, op1=ALU.mult),
                         reads=[xk, "rs", "nfg"], writes=["h2f"])

                    def tr8(e):
                        for kb in range(8):
                            r = e.transpose(ptr[:, kb * 128:(kb + 1) * 128], h2f[:, kb * 128:(kb + 1) * 128], ident[:])
                        return r
                    S.op(T, tr8, reads=["h2f", "ident"], writes=["ptr"])
                    S.op(V, lambda e, tt_=tt_: e.tensor_copy(out=h2T[:, :, tt_ * 128:(tt_ + 1) * 128], in_=ptr[:].rearrange("p (k t) -> p k t", t=128)),
                         reads=["ptr"], writes=["h2T_%d" % tt_])
                    if moe:
                        S.op(A, lambda e: e.copy(out=h2T32, in_=ptr[:].rearrange("p (k t) -> p k t", t=128)),
                             reads=["ptr", "h2T_%d" % tt_], writes=["h2f"])
                        if not ffn:
                            S.dma("sync", lambda e, tt_=tt_: e.dma_start(out=h2T_o[:, :, tt_ * 128:(tt_ + 1) * 128], in_=h2T32),
                                  reads=["h2f"], writes=["h2T_o"], is_output=last)

                        def mm_r(e):
                            for kb in range(8):
                                r = e.matmul(pg[0][:, 0:8], lhsT=h2T32[:, kb, :], rhs=rw[:, kb, :], start=(kb == 0), stop=(kb == 7))
                            return r
                        S.op(T, mm_r, reads=["h2f", "rw"], writes=["pg0"])
                        S.op(V, lambda e: e.tensor_tensor(out=lg[:], in0=pg[0][:, 0:8], in1=rb[:], op=ALU.add), reads=["pg0", "rb"], writes=["lg"])
                        S.op(V, lambda e: e.reduce_max(out=mx1[:], in_=lg[:], axis=AX.X), reads=["lg"], writes=["mx1"])
                        S.op(V, lambda e: e.tensor_scalar(out=eq1[:], in0=lg[:], scalar1=mx1[:, 0:1], scalar2=None, op0=ALU.is_equal),
                             reads=["lg", "mx1"], writes=["eq1"])
                        S.op(V, lambda e: e.scalar_tensor_tensor(out=l2[:], in0=eq1[:], scalar=-1e30, in1=lg[:], op0=ALU.mult, op1=ALU.add),
                             reads=["eq1", "lg"], writes=["l2"])
                        S.op(V, lambda e: e.reduce_max(out=mx2[:], in_=l2[:], axis=AX.X), reads=["l2"], writes=["mx2"])
                        S.op(V, lambda e: e.tensor_scalar(out=eq2[:], in0=l2[:], scalar1=mx2[:, 0:1], scalar2=None, op0=ALU.is_equal),
                             reads=["l2", "mx2"], writes=["eq2"])
                        S.op(V, lambda e: e.tensor_tensor(out=dd[:], in0=mx2[:], in1=mx1[:], op=ALU.subtract), reads=["mx1", "mx2"], writes=["dd"])
                        S.op(A, lambda e: e.activation(out=w2[:], in_=dd[:], func=AF.Sigmoid), reads=["dd"], writes=["w2"])
                        S.op(V, lambda e: e.tensor_scalar(out=w1[:], in0=w2[:], scalar1=-1.0, scalar2=1.0, op0=ALU.mult, op1=ALU.add),
                             reads=["w2"], writes=["w1"])
                        S.op(V, lambda e: e.tensor_scalar(out=g1[:], in0=eq1[:], scalar1=w1[:, 0:1], scalar2=None, op0=ALU.mult),
                             reads=["eq1", "w1"], writes=["g1"])
                        S.op(V, lambda e, tt_=tt_: e.scalar_tensor_tensor(out=gates[:, tt_, :], in0=eq2[:], scalar=w2[:, 0:1], in1=g1[:],
                                                                          op0=ALU.mult, op1=ALU.add),
                             reads=["eq2", "w2", "g1"], writes=["gates%d" % tt_])

            def P3(th, exs):
                nonlocal wi, gi
                H2K = ["h2T_%d" % (th * NSUB + s_) for s_ in range(NSUB)]
                for ex in exs:
                    for q in (range(2) if th == 0 else []):
                        S.dma("gpsimd", lambda e, ex=ex, q=q: e.dma_start(
                            out=wdb[:, q * 11:(q + 1) * 11, :],
                            in_=wd_d[ex].rearrange("(fb p) n -> p fb n", p=128)[:, q * 11:(q + 1) * 11, :]), writes=["wdb%d" % q])
                    for c in range(NCH):
                        wgt = wgb[wi % 2]; wgk = "wgb%d" % (wi % 2)
                        wut = wub[wi % 2]; wuk = "wub%d" % (wi % 2)
                        wi += 1
                        cs = slice(c * WC, (c + 1) * WC)
                        S.dma("gpsimd", lambda e, wgt=wgt, ex=ex, cs=cs: e.dma_start(
                            out=wgt[:], in_=wg_d[ex].rearrange("(kb p) n -> p kb n", p=128)[:, :, cs]), writes=[wgk])
                        S.dma("gpsimd", lambda e, wut=wut, ex=ex, cs=cs: e.dma_start(
                            out=wut[:], in_=wu_d[ex].rearrange("(kb p) n -> p kb n", p=128)[:, :, cs]), writes=[wuk])
                        for fl in range(WC // 128):
                            fb = c * (WC // 128) + fl
                            for tt2 in range(HT // TT):
                                pgb = pg[gi % 2]; pgk = "pg%d" % (gi % 2)
                                pub = pu[gi % 2]; puk = "pu%d" % (gi % 2)
                                sgb = sg[gi % 2]; sgk = "sg%d" % (gi % 2)
                                gi += 1
                                tsl = slice(tt2 * TT, (tt2 + 1) * TT); tslh = slice(th * HT + tt2 * TT, th * HT + (tt2 + 1) * TT)

                                def mm_g(e, pgb=pgb, wgt=wgt, fl=fl, tsl=tslh):
                                    for kb in range(8):
                                        r = e.matmul(pgb[:], lhsT=wgt[:, kb, fl * 128:(fl + 1) * 128], rhs=h2T[:, kb, tsl], start=(kb == 0), stop=(kb == 7))
                                    return r
                                S.op(T, mm_g, reads=[wgk] + H2K, writes=[pgk])

                                def mm_u(e, pub=pub, wut=wut, fl=fl, tsl=tslh):
                                    for kb in range(8):
                                        r = e.matmul(pub[:], lhsT=wut[:, kb, fl * 128:(fl + 1) * 128], rhs=h2T[:, kb, tsl], start=(kb == 0), stop=(kb == 7))
                                    return r
                                S.op(T, mm_u, reads=[wuk] + H2K, writes=[puk])
                                S.op(A, lambda e, pgb=pgb, sgb=sgb: e.activation(out=sgb[:], in_=pgb[:], func=AF.Silu), reads=[pgk], writes=[sgk])
                                S.op(V, lambda e, pub=pub, sgb=sgb, fb=fb, tsl=tsl: e.tensor_tensor(out=act[:, fb, tsl], in0=pub[:], in1=sgb[:], op=ALU.mult),
                                     reads=[puk, sgk], writes=["act%d" % fb])
                    ACTK = ["act%d" % fb for fb in range(NFB)]
                    for sub in range(NSUB):
                        tt_ = th * NSUB + sub

                        def mm_d(e, sub=sub):
                            for h in range(2):
                                for fb in range(NFB):
                                    r = e.matmul(pd[:, h * 512:(h + 1) * 512], lhsT=act[:, fb, sub * 128:(sub + 1) * 128],
                                                 rhs=wdb[:, fb, h * 512:(h + 1) * 512], start=(fb == 0), stop=(fb == NFB - 1))
                            return r
                        S.op(T, mm_d, reads=ACTK + ["wdb0", "wdb1"], writes=["pd"])
                        if moe:
                            S.op(V, lambda e, tt_=tt_, ex=ex: e.scalar_tensor_tensor(out=xm[:, tt_, :], in0=pd[:], scalar=gates[:, tt_, ex:ex + 1],
                                                                                     in1=xm[:, tt_, :], op0=ALU.mult, op1=ALU.add),
                                 reads=["pd", "gates%d" % tt_, "xm%d" % tt_], writes=["xm%d" % tt_])
                        else:
                            S.op(V, lambda e, tt_=tt_: e.tensor_tensor(out=xm[:, tt_, :], in0=pd[:], in1=xm[:, tt_, :], op=ALU.add),
                                 reads=["pd", "xm%d" % tt_], writes=["xm%d" % tt_])

            def P4(th):
                for sub in range(NSUB):
                    tt_ = th * NSUB + sub
                    xk = "xm%d" % tt_
                    if last:
                        S.op(A, lambda e, tt_=tt_: e.activation(out=h2f[:], in_=xm[:, tt_, :], func=AF.Square, accum_out=ssq[:]),
                             reads=[xk], writes=["h2f", "ssq"])
                        S.op(V, lambda e: e.tensor_scalar(out=rs[:], in0=ssq[:], scalar1=1.0 / D, scalar2=RMS_EPS, op0=ALU.mult, op1=ALU.add),
                             reads=["ssq"], writes=["rs"])
                        S.op(A, lambda e: e.sqrt(out=rs[:], in_=rs[:]), reads=["rs"], writes=["rs"])
                        S.op(V, lambda e: e.reciprocal(out=rs[:], in_=rs[:]), reads=["rs"], writes=["rs"])
                        S.op(V, lambda e, tt_=tt_: e.scalar_tensor_tensor(out=ob_ap, in0=xm[:, tt_, :], scalar=rs[:, 0:1], in1=nfg[:],
                                                                          op0=ALU.mult, op1=ALU.mult),
                             reads=[xk, "rs", "nfg"], writes=[obk])
                        S.dma("sync", lambda e, tt_=tt_: e.dma_start(out=out_d[:, tt_, :], in_=ob_ap), reads=[obk], writes=["out_d"], is_output=last)
                    else:
                        S.dma("sync", lambda e, tt_=tt_: e.dma_start(out=out_d[:, tt_, :], in_=xm[:, tt_, :]), reads=[xk], writes=["out_d"], is_output=last)
                        if xT_out:
                            def trx(e, tt_=tt_):
                                for kb in range(8):
                                    r = e.transpose(ptr[:, kb * 128:(kb + 1) * 128], xm[:, tt_, kb * 128:(kb + 1) * 128], ident[:])
                                return r
                            S.op(T, trx, reads=[xk, "ident"], writes=["ptr"])
                            S.op(V, lambda e: e.tensor_copy(out=h2f[:].rearrange("p (k t) -> p k t", t=128), in_=ptr[:].rearrange("p (k t) -> p k t", t=128)),
                                 reads=["ptr"], writes=["h2f"])
                            S.dma("sync", lambda e, tt_=tt_: e.dma_start(out=d["xT_out"][:, :, tt_ * 128:(tt_ + 1) * 128],
                                                                         in_=h2f[:].rearrange("p (k t) -> p k t", t=128)),
                                  reads=["h2f"], writes=["xT_scr"])

            for th in range(2):
                P2(th)
            for ex in range(n_exp):
                for th in range(2):
                    P3(th, [ex])
            if last:
                S.dma("sync", lambda e: e.dma_start(out=nfg[:], in_=fg_d), writes=["nfg"])
            for th in range(2):
                P4(th)
            S.barrier()
            S.flush(sst)


def _prep_Bp(p, chalf):
    gs = slice(chalf * G, (chalf + 1) * G)
    cs = slice(chalf * 256, (chalf + 1) * 256)
    dp = lambda a: np.ascontiguousarray(a[:, gs, :].transpose(0, 2, 1).reshape(128, G))
    ldt = np.ascontiguousarray(np.broadcast_to(p["ssm_log_dt"][:, gs][:, None, :], (2, 64, G)).reshape(128, G))
    bb = lambda a: np.ascontiguousarray(np.broadcast_to(a[gs].transpose(1, 0, 2)[None], (2, 64, G, 16)).reshape(128, G, 16))
    cc = lambda a: np.ascontiguousarray(a[:, gs].transpose(0, 3, 1, 2).reshape(128, G, 16))
    dsk = p["ssm_d"][cs].reshape(G, 16)
    dbc = np.ascontiguousarray(np.broadcast_to(dsk[None, :, None, :], (128, G, 8, 16)).reshape(128, G, 128))
    return {
        "dww": np.ascontiguousarray(p["conv_dw_w"][:, cs].T.reshape(2, 128, 31).transpose(1, 0, 2)),
        "dwb": np.ascontiguousarray(p["conv_dw_b"][cs].reshape(2, 128).T),
        "a_re": dp(p["ssm_a_re"]), "a_im": dp(p["ssm_a_im"]), "log_dt": ldt,
        "b_re": bb(p["ssm_b_re"]), "b_im": bb(p["ssm_b_im"]),
        "c_re": cc(p["ssm_c_re"]), "c_im": cc(p["ssm_c_im"]), "dbc": dbc,
    }


_BP_SHAPES = {"dww": [128, 2, 31], "dwb": [128, 2], "a_re": [128, G], "a_im": [128, G], "log_dt": [128, G],
              "b_re": [128, G, 16], "b_im": [128, G, 16], "c_re": [128, G, 16], "c_im": [128, G, 16], "dbc": [128, G, 128]}
_CP_SHAPES = {"lng": [128, 4], "lnb": [128, 4], "bglu": [128, 4], "wcp": [512, D], "wglu": [512, 512], "wsp": [512, D],
              "wout": [D, D], "nfg": [128, D]}


def build_mega(debug=False):
    nc = bass.Bass("TRN2", target_bir_lowering=False)
    din = lambda name, shape: nc.dram_tensor(name, shape, F32, kind="ExternalInput").ap()
    skind = "ExternalOutput" if debug else "Internal"
    scr = lambda name, shape, dt=F32: nc.dram_tensor(name, shape, dt, kind=skind).ap()
    LL = 4096
    xT0 = din("xT0", [128, 8, LL]); x_tok0 = din("x_tok0", [128, 32, D])
    mask_f = din("mask_f", [128, 128]); mask_b = din("mask_b", [128, 128])
    gcol = [din("gcol%d" % l, [128, 8]) for l in range(2)]
    w_in = [din("w_in%d" % l, [D, 3584]) for l in range(2)]
    bp = [[{k: din("B%d%d_%s" % (l, h, k), shp) for k, shp in _BP_SHAPES.items()} for h in range(2)] for l in range(2)]
    cp = [{k: din("C%d_%s" % (l, k), shp) for k, shp in _CP_SHAPES.items()} for l in range(2)]
    ffw = [{"wg": din("wg0", [1, D, DFF]), "wu": din("wu0", [1, D, DFF]), "wd": din("wd0", [1, DFF, D])},
           {"wg": din("wg1", [8, D, DFF]), "wu": din("wu1", [8, D, DFF]), "wd": din("wd1", [8, DFF, D])}]
    rw = din("rw", [128, 8, 8]); rb = din("rb", [128, 8]); fg = din("fg", [128, D])
    out = nc.dram_tensor("out", [128, 16, D], F32, kind="ExternalOutput").ap()
    vT = scr("s_vT", [128, 4, LL]); gT = scr("s_gT", [128, 4, LL]); zu = scr("s_zu", [LL, 512])
    gcT = scr("s_gcT", [128, 8, LL], BF16); gsT = scr("s_gsT", [128, 8, LL], BF16)
    convT = scr("s_convT", [128, 4, LL]); ytok = scr("s_ytok", [LL, 512])
    x_tok1 = scr("s_xtok1", [128, 32, D]); xT1 = scr("s_xT1", [128, 8, LL])
    S = Sched(nc)
    uid = [0]

    def nu():
        uid[0] += 1
        return uid[0]
    with contextlib.ExitStack() as sst:
        for l in range(2):
            xT = xT0 if l == 0 else xT1
            xtok = x_tok0 if l == 0 else x_tok1
            emit_A(nc, S, sst, {"xT": xT, "gcol": gcol[l], "w": w_in[l], "vT": vT, "gT": gT, "zu": zu, "gcT": gcT, "gsT": gsT},
                   nu(), 8, 8 if l == 0 else 4)
            for h in range(2):
                dB = dict(bp[l][h])
                dB.update({"zu": zu[:, h * 256:(h + 1) * 256], "vT": vT[:, h * 2:(h + 1) * 2, :], "gT": gT[:, h * 2:(h + 1) * 2, :],
                           "convT": convT[:, h * 2:(h + 1) * 2, :], "mask_f": mask_f, "mask_b": mask_b,
                           "ytok": ytok.rearrange("(k t) c -> k t c", t=8)[:, :, h * 256:(h + 1) * 256]})
                emit_B(nc, S, sst, dB, nu(), own_only=(l == 1))
            for hf in range(2 if l == 0 else 1):
                tk = slice(hf * NT, (hf + 1) * NT)
                dC = dict(cp[l]); dC.update(ffw[l])
                dC.update({"convT": convT[:, :, tk], "ytok": ytok[tk, :], "gcT": gcT[:, :, tk], "gsT": gsT[:, :, tk],
                           "x_tok": xtok[:, hf * 16:(hf + 1) * 16, :], "rw": rw, "rb": rb, "fg": fg})
                if l == 0:
                    dC["out"] = x_tok1[:, hf * 16:(hf + 1) * 16, :]
                    dC["xT_out"] = xT1[:, :, tk]
                    emit_C(nc, S, sst, dC, nu(), 1, False, False, xT_out=True)
                else:
                    dC["out"] = out
                    emit_C(nc, S, sst, dC, nu(), 8, True, True)
        S.finish()
        S.flush(sst)
    return nc


def prep_mega(core, inp):
    b, hf = core // 2, core % 2
    rv = (hf == 1)
    xs = inp["x"][b]
    if rv:
        xs = xs[::-1]
    s_idx = np.arange(128) // 16
    m = {
        "xT0": np.ascontiguousarray(xs.T.reshape(8, 128, 4096).transpose(1, 0, 2)),
        "x_tok0": np.ascontiguousarray(xs.reshape(32, 128, D).transpose(1, 0, 2)),
        "mask_f": (s_idx[None, :] >= s_idx[:, None]).astype(np.float32),
        "mask_b": (s_idx[None, :] <= s_idx[:, None]).astype(np.float32),
    }
    col = lambda v, nb: np.ascontiguousarray(v.reshape(nb, 128).T)
    rep = lambda v: np.ascontiguousarray(np.broadcast_to(v[None, :], (128, v.shape[0])))
    for l in range(2):
        m["gcol%d" % l] = col(inp["norm_mix_g"][l], 8)
        m["w_in%d" % l] = inp["w_in"][l]
        p = {k: inp[k][l] for k in ["conv_dw_w", "conv_dw_b", "ssm_a_re", "ssm_a_im", "ssm_log_dt", "ssm_b_re", "ssm_b_im",
                                     "ssm_c_re", "ssm_c_im", "ssm_d"]}
        if rv:
            p["conv_dw_w"] = p["conv_dw_w"][::-1]
            for k in ["ssm_a_re", "ssm_a_im", "ssm_log_dt", "ssm_c_re", "ssm_c_im"]:
                p[k] = p[k][::-1]
        for h in range(2):
            for k, v in _prep_Bp(p, h).items():
                m["B%d%d_%s" % (l, h, k)] = v
        m["C%d_lng" % l] = col(inp["conv_ln_g"][l], 4); m["C%d_lnb" % l] = col(inp["conv_ln_b"][l], 4)
        m["C%d_bglu" % l] = col(inp["ssm_b_glu"][l], 4)
        m["C%d_wcp" % l] = inp["w_conv_proj"][l]; m["C%d_wglu" % l] = inp["ssm_w_glu"][l]
        m["C%d_wsp" % l] = inp["w_ssm_proj"][l]; m["C%d_wout" % l] = inp["w_out"][l]
        m["C%d_nfg" % l] = rep(inp["norm_ffn_g"][l])
    m["wg0"] = inp["ffn_w_gate"]; m["wu0"] = inp["ffn_w_up"]; m["wd0"] = inp["ffn_w_down"]
    m["wg1"] = inp["moe_w_gate"][0]; m["wu1"] = inp["moe_w_up"][0]; m["wd1"] = inp["moe_w_down"][0]
    m["rw"] = np.ascontiguousarray(inp["router_w"][0].reshape(8, 128, 8).transpose(1, 0, 2))
    m["rb"] = rep(inp["router_b"][0]); m["fg"] = rep(inp["final_norm_g"])
    return m


_NC = {}


def kernel(**inputs):
    inp = {k: np.ascontiguousarray(np.asarray(v, dtype=np.float32)) for k, v in inputs.items()}
    if "mega" not in _NC:
        _NC["mega"] = build_mega()
    cores = list(range(8))
    res = run_bass_kernel_spmd(_NC["mega"], [prep_mega(c, inp) for c in cores], core_ids=cores)
    out = np.zeros((4, 4096, D), np.float32)
    for c in cores:
        b, hf = c // 2, c % 2
        o = res.results[c]["out"].transpose(1, 0, 2).reshape(NT, D)
        if hf == 0:
            out[b, 0:NT] = o
        else:
            out[b, NT:] = o[::-1]
    return out
```
